# Optimizing a Trainium2 kernel written in Bass

```python
import functools
import jax, jax.numpy as jnp
from jax import lax
import numpy as np

D_MODEL = 1024
BATCH = 4
SEQ = 4096
DEPTH = 2

GRID_W = 64
CTX_LEN = 256
MLSTM_HEADS = 8
MLSTM_DQK = 64
MLSTM_DV = 128
MLSTM_CHUNK = 64
GATE_SOFT_CAP = 15.0
RWKV_HEADS = 16
RWKV_N = 64
RWKV_DIM = RWKV_HEADS * RWKV_N
DECAY_LORA = 64
AAA_LORA = 64
GATE_LORA = 128
GN_EPS = 64e-5
D_FF = 2816
N_EXPERTS = 8
TOP_K = 2
D_FF_EXPERT = 3584
NORM_EPS = 1e-6

MLSTM_QK_W = MLSTM_HEADS * MLSTM_DQK
MLSTM_V_W = MLSTM_HEADS * MLSTM_DV
RWKV_IN_W = 3 * RWKV_DIM + 2 * DECAY_LORA + 2 * AAA_LORA + GATE_LORA
IN_SIZES = (MLSTM_QK_W, MLSTM_QK_W, MLSTM_V_W, MLSTM_V_W, 4 * MLSTM_HEADS, RWKV_IN_W, 2 * D_MODEL)
D_IN = sum(IN_SIZES)
RWKV_SIZES = (RWKV_DIM, RWKV_DIM, RWKV_DIM, DECAY_LORA, DECAY_LORA, AAA_LORA, AAA_LORA, GATE_LORA)
N_DENSE = (DEPTH + 1) // 2
N_MOE = DEPTH // 2

kernel_name = 'hybrid_mlstm_rwkv7_moe_prefix_dit'


def _split(a, sizes):
    idx = np.cumsum(sizes)[:-1].tolist()
    return jnp.split(a, idx, axis=-1)


def rmsnorm(x, g):
    xf = x.astype(jnp.float32)
    y = xf * lax.rsqrt(jnp.mean(xf * xf, axis=-1, keepdims=True) + NORM_EPS)
    return (y * g.astype(jnp.float32)).astype(x.dtype)


def modulate(h, shift, scale):
    return h * (1 + scale) + shift


def soft_cap(a):
    return GATE_SOFT_CAP * jnp.tanh(a / GATE_SOFT_CAP)


def heads(a, n_heads):
    B, T, _ = a.shape
    return a.reshape(B, T, n_heads, -1).transpose(0, 2, 1, 3)


def grid_shift(x):
    B, T, C = x.shape
    rows = T // GRID_W
    g = x.reshape(B, rows, GRID_W, C)
    q = C // 4
    left = jnp.pad(g[:, :, :-1, :q], ((0, 0), (0, 0), (1, 0), (0, 0)))
    right = jnp.pad(g[:, :, 1:, q:2 * q], ((0, 0), (0, 0), (0, 1), (0, 0)))
    up = jnp.pad(g[:, :-1, :, 2 * q:3 * q], ((0, 0), (1, 0), (0, 0), (0, 0)))
    down = jnp.pad(g[:, 1:, :, 3 * q:], ((0, 0), (0, 1), (0, 0), (0, 0)))
    return jnp.concatenate([left, right, up, down], axis=-1).reshape(B, T, C)


def seq_shift(x):
    h = x.shape[-1] // 2
    prev = jnp.pad(x[:, :-1, :h], ((0, 0), (1, 0), (0, 0)))
    nxt = jnp.pad(x[:, 1:, h:], ((0, 0), (0, 1), (0, 0)))
    return jnp.concatenate([prev, nxt], axis=-1)


def mlstm_chunkwise(q, k, v, i_pre, f_pre, state):
    B, H, T, dk = q.shape
    L = MLSTM_CHUNK
    nc = T // L
    q = q * (dk ** -0.5)
    logf = jax.nn.log_sigmoid(f_pre)
    causal = jnp.tril(jnp.ones((L, L), dtype=bool))

    def to_chunks(a):
        return jnp.moveaxis(a.reshape(B, H, nc, L, *a.shape[3:]), 2, 0)

    def step(carry, inp):
        C, n, m = carry
        qc, kc, vc, ic, lf = inp
        b = jnp.cumsum(lf, axis=-1)
        d = jnp.where(causal, b[..., :, None] - b[..., None, :] + ic[..., None, :], -jnp.inf)
        inter = b + m[..., None]
        m_t = jnp.maximum(inter, jnp.max(d, axis=-1))
        w_intra = jnp.exp(d - m_t[..., None])
        w_inter = jnp.exp(inter - m_t)
        s = jnp.einsum('bhtd,bhsd->bhts', qc, kc) * w_intra
        num = jnp.einsum('bhts,bhsv->bhtv', s, vc) + w_inter[..., None] * jnp.einsum('bhvd,bhtd->bhtv', C, qc)
        den = jnp.sum(s, axis=-1) + w_inter * jnp.einsum('bhd,bhtd->bht', n, qc)
        h = num / jnp.maximum(jnp.abs(den), jnp.exp(-m_t))[..., None]
        b_last = b[..., -1]
        w_src = b_last[..., None] - b + ic
        m_new = jnp.maximum(b_last + m, jnp.max(w_src, axis=-1))
        a_src = jnp.exp(w_src - m_new[..., None])
        a_old = jnp.exp(b_last + m - m_new)
        C = a_old[..., None, None] * C + jnp.einsum('bhs,bhsv,bhsd->bhvd', a_src, vc, kc)
        n = a_old[..., None] * n + jnp.einsum('bhs,bhsd->bhd', a_src, kc)
        return (C, n, m_new), h

    xs = (to_chunks(q), to_chunks(k), to_chunks(v), to_chunks(i_pre), to_chunks(logf))
    state, h = lax.scan(step, state, xs)
    h = jnp.moveaxis(h, 0, 2).reshape(B, H, T, -1)
    return h, state


def mlstm_mixer(q, k, v, gates, qc, kc, vc, gatesc, b_gate):
    def prep(q, k, v, gates):
        B, T, _ = q.shape
        pre = soft_cap(gates.astype(jnp.float32).reshape(B, T, 4, MLSTM_HEADS) + b_gate).transpose(2, 0, 3, 1)
        f32 = lambda a: a.astype(jnp.float32)
        return heads(f32(q), MLSTM_HEADS), heads(f32(k), MLSTM_HEADS), heads(f32(v), MLSTM_HEADS), pre

    ql, kl, vl, pl = prep(q, k, v, gates)
    qx, kx, vx, px = prep(qc, kc, vc, gatesc)
    B = q.shape[0]
    zero = (jnp.zeros((B, MLSTM_HEADS, MLSTM_DV, MLSTM_DQK), jnp.float32),
            jnp.zeros((B, MLSTM_HEADS, MLSTM_DQK), jnp.float32),
            jnp.zeros((B, MLSTM_HEADS), jnp.float32))
    fl = lambda a: jnp.flip(a, axis=2)
    hc_f, st_f = mlstm_chunkwise(qx, kx, vx, px[0], px[1], zero)
    h_f, _ = mlstm_chunkwise(ql, kl, vl, pl[0], pl[1], st_f)
    hc_b, st_b = mlstm_chunkwise(fl(qx), fl(kx), fl(vx), fl(px[2]), fl(px[3]), zero)
    h_b, _ = mlstm_chunkwise(fl(ql), fl(kl), fl(vl), fl(pl[2]), fl(pl[3]), st_b)
    return h_f + fl(h_b), hc_f + fl(hc_b)


def mlstm_out(h, o, g_norm):
    B, H, T, DV = h.shape
    h = h.transpose(0, 2, 1, 3)
    h = h * lax.rsqrt(jnp.mean(h * h, axis=-1, keepdims=True) + NORM_EPS)
    h = h.reshape(B, T, H * DV) * g_norm
    return (h * jax.nn.sigmoid(o.astype(jnp.float32))).astype(o.dtype)


def rwkv7_prepare(p, p_shift, mu, w0, w2, a0, a2, g2, k_k, k_a):
    B, T, _ = p.shape
    pf = p.astype(jnp.float32)
    xm = pf + (p_shift.astype(jnp.float32) - pf) * mu
    r, k, v, wd_f, wd_b, ad_f, ad_b, gd = _split(xm, RWKV_SIZES)
    hd = lambda a: a.reshape(B, T, RWKV_HEADS, RWKV_N)
    kk = hd(k * k_k)
    kk = kk / jnp.maximum(jnp.sqrt(jnp.sum(kk * kk, axis=-1, keepdims=True)), 1e-12)
    dirs = []
    for d, (wd, ad) in enumerate(((wd_f, ad_f), (wd_b, ad_b))):
        w = -jax.nn.softplus(-(w0[d] + jnp.tanh(wd) @ w2[d])) - 0.5
        decay = jnp.exp(-jnp.exp(w))
        a = jax.nn.sigmoid(a0[d] + ad @ a2[d])
        kd = k * (1 + (a - 1) * k_a)
        dirs.append((hd(decay), hd(a), hd(kd)))
    g = jax.nn.sigmoid(gd) @ g2
    return hd(r), hd(v), kk, g, dirs


def rwkv7_scan(r, w, k, v, kk, a, state, reverse):
    def step(S, inp):
        r_t, w_t, k_t, v_t, kk_t, a_t = inp
        sa = jnp.einsum('bhvk,bhk->bhv', S, kk_t)
        S = (S * w_t[:, :, None, :] - sa[..., None] * (kk_t * a_t)[:, :, None, :]
             + v_t[..., None] * k_t[:, :, None, :])
        return S, jnp.einsum('bhvk,bhk->bhv', S, r_t)
    xs = tuple(jnp.moveaxis(t, 1, 0) for t in (r, w, k, v, kk, a))
    state, y = lax.scan(step, state, xs, reverse=reverse)
    return jnp.moveaxis(y, 0, 1), state


def rwkv7_mixer(p, pc, need_ctx, mu, w0, w2, a0, a2, g2, k_k, k_a, r_k, ln_w, ln_b):
    prm = (mu, w0, w2, a0, a2, g2, k_k, k_a)
    lat = rwkv7_prepare(p, grid_shift(p), *prm)
    ctxp = rwkv7_prepare(pc, seq_shift(pc), *prm)
    zero = jnp.zeros((p.shape[0], RWKV_HEADS, RWKV_N, RWKV_N), jnp.float32)

    def run(prep, s_f, s_b):
        r, v, kk, g, ((dec_f, a_f, k_f), (dec_b, a_b, k_b)) = prep
        y_f, s_f = rwkv7_scan(r, dec_f, k_f, v, kk, a_f, s_f, False)
        y_b, s_b = rwkv7_scan(r, dec_b, k_b, v, kk, a_b, s_b, True)
        return y_f + y_b, s_f, s_b

    def finish(y, prep):
        r, v, kk, g, dirs = prep
        B, T = y.shape[:2]
        mean = jnp.mean(y, axis=-1, keepdims=True)
        var = jnp.mean(jnp.square(y - mean), axis=-1, keepdims=True)
        yn = ((y - mean) * lax.rsqrt(var + GN_EPS)).reshape(B, T, -1) * ln_w + ln_b
        bonus = sum(jnp.sum(r * kd * r_k, axis=-1, keepdims=True) * v for (_, _, kd) in dirs)
        return (yn + bonus.reshape(B, T, -1)) * g

    yc, s_f, s_b = run(ctxp, zero, zero)
    y, _, _ = run(lat, s_f, s_b)
    out = finish(y, lat).astype(p.dtype)
    out_c = finish(yc, ctxp).astype(pc.dtype) if need_ctx else None
    return out, out_c


def hybrid_mixer(h, hc, need_ctx, w_in, b_gate, g_mnorm, mu, w0, w2, a0, a2, g2, k_k, k_a, r_k,
                 ln_w, ln_b, w_pm, w_pr, w_out):
    q, k, v, o, gates, prw, mg = _split(h @ w_in, IN_SIZES)
    qc, kc, vc, oc, gatesc, prwc, mgc = _split(hc @ w_in, IN_SIZES)
    hm, hmc = mlstm_mixer(q, k, v, gates, qc, kc, vc, gatesc, b_gate)
    yr, yrc = rwkv7_mixer(prw, prwc, need_ctx, mu, w0, w2, a0, a2, g2, k_k, k_a, r_k, ln_w, ln_b)

    def merge(hm, o, yr, mg):
        ym = mlstm_out(hm, o, g_mnorm) @ w_pm
        yr = yr @ w_pr
        g_m, g_r = jnp.split(jax.nn.sigmoid(mg), 2, axis=-1)
        return (g_m * ym + g_r * yr) @ w_out

    y = merge(hm, o, yr, mg)
    yc = merge(hmc, oc, yrc, mgc) if need_ctx else None
    return y, yc


def swiglu(h, wg, wu, wd):
    return (jax.nn.silu(h @ wg) * (h @ wu)) @ wd


def moe_swiglu(h, w_router, wg, wu, wd):
    logits = (h @ w_router).astype(jnp.float32)
    top_v, top_i = lax.top_k(logits, TOP_K)
    probs = jax.nn.softmax(top_v, axis=-1)
    gates = jnp.einsum('btk,btke->bte', probs, jax.nn.one_hot(top_i, N_EXPERTS, dtype=jnp.float32)).astype(h.dtype)
    out = jnp.zeros_like(h)
    for e in range(N_EXPERTS):
        out = out + gates[..., e:e + 1] * swiglu(h, wg[e], wu[e], wd[e])
    return out


def setup_inputs(seed: int = 0) -> dict:
    key = jax.random.key(seed)
    ks = iter(jax.random.split(key, 40))
    nrm = lambda shape, s: s * jax.random.normal(next(ks), shape, jnp.float32)
    D, Dr, Dv = D_MODEL, RWKV_DIM, MLSTM_V_W
    gate_base = jnp.array([-2.0, 3.0, -2.0, 3.0], jnp.float32)[None, :, None]
    return {
        'x': nrm((BATCH, SEQ, D), 1.0),
        'c': nrm((BATCH, D), 1.0),
        'ctx': nrm((BATCH, CTX_LEN, D), 1.0),
        'c_ctx': nrm((D,), 1.0),
        'w_ada': nrm((DEPTH, D, 6 * D), 0.5 * D ** -0.5),
        'b_ada': nrm((DEPTH, 6 * D), 0.01),
        'g_norm_mix': 1.0 + nrm((DEPTH, D), 0.02),
        'g_norm_ffn': 1.0 + nrm((DEPTH, D), 0.02),
        'w_in': nrm((DEPTH, D, D_IN), D ** -0.5),
        'b_mlstm_gate': gate_base + nrm((DEPTH, 4, MLSTM_HEADS), 0.5),
        'g_mlstm_norm': 1.0 + nrm((DEPTH, Dv), 0.02),
        'mu_shift': jax.random.uniform(next(ks), (DEPTH, RWKV_IN_W), jnp.float32),
        'w0': jax.random.uniform(next(ks), (DEPTH, 2, Dr), jnp.float32, -5.5, -0.5),
        'w2': nrm((DEPTH, 2, DECAY_LORA, Dr), 0.1 * DECAY_LORA ** -0.5),
        'a0': nrm((DEPTH, 2, Dr), 0.3),
        'a2': nrm((DEPTH, 2, AAA_LORA, Dr), 0.5 * AAA_LORA ** -0.5),
        'g2': nrm((DEPTH, GATE_LORA, Dr), GATE_LORA ** -0.5),
        'k_k': 0.85 + nrm((DEPTH, Dr), 0.1),
        'k_a': 1.0 + nrm((DEPTH, Dr), 0.1),
        'r_k': nrm((DEPTH, RWKV_HEADS, RWKV_N), 0.1),
        'ln_w': 1.0 + nrm((DEPTH, Dr), 0.02),
        'ln_b': nrm((DEPTH, Dr), 0.01),
        'w_proj_mlstm': nrm((DEPTH, Dv, D), Dv ** -0.5),
        'w_proj_rwkv': nrm((DEPTH, Dr, D), Dr ** -0.5),
        'w_out': nrm((DEPTH, D, D), D ** -0.5),
        'w_ff_gate': nrm((N_DENSE, D, D_FF), D ** -0.5),
        'w_ff_up': nrm((N_DENSE, D, D_FF), D ** -0.5),
        'w_ff_down': nrm((N_DENSE, D_FF, D), D_FF ** -0.5),
        'w_router': nrm((N_MOE, D, N_EXPERTS), D ** -0.5),
        'w_exp_gate': nrm((N_MOE, N_EXPERTS, D, D_FF_EXPERT), D ** -0.5),
        'w_exp_up': nrm((N_MOE, N_EXPERTS, D, D_FF_EXPERT), D ** -0.5),
        'w_exp_down': nrm((N_MOE, N_EXPERTS, D_FF_EXPERT, D), D_FF_EXPERT ** -0.5),
        'g_final': 1.0 + nrm((D,), 0.02),
    }


def reference(x, c, ctx, c_ctx, w_ada, b_ada, g_norm_mix, g_norm_ffn, w_in, b_mlstm_gate, g_mlstm_norm,
              mu_shift, w0, w2, a0, a2, g2, k_k, k_a, r_k, ln_w, ln_b, w_proj_mlstm, w_proj_rwkv, w_out,
              w_ff_gate, w_ff_up, w_ff_down, w_router, w_exp_gate, w_exp_up, w_exp_down, g_final):
    xc = ctx
    s_lat = jax.nn.silu(c)
    s_ctx = jax.nn.silu(c_ctx)
    for l in range(DEPTH):
        need_ctx = l < DEPTH - 1
        mod = (s_lat @ w_ada[l] + b_ada[l])[:, None, :]
        mod_c = s_ctx @ w_ada[l] + b_ada[l]
        sh1, sc1, gt1, sh2, sc2, gt2 = jnp.split(mod, 6, axis=-1)
        sh1c, sc1c, gt1c, sh2c, sc2c, gt2c = jnp.split(mod_c, 6, axis=-1)
        h = modulate(rmsnorm(x, g_norm_mix[l]), sh1, sc1)
        hc = modulate(rmsnorm(xc, g_norm_mix[l]), sh1c, sc1c)
        y, yc = hybrid_mixer(h, hc, need_ctx, w_in[l], b_mlstm_gate[l], g_mlstm_norm[l], mu_shift[l],
                             w0[l], w2[l], a0[l], a2[l], g2[l], k_k[l], k_a[l], r_k[l], ln_w[l], ln_b[l],
                             w_proj_mlstm[l], w_proj_rwkv[l], w_out[l])
        x = x + gt1 * y
        i = l // 2
        if l % 2 == 0:
            ffn = functools.partial(swiglu, wg=w_ff_gate[i], wu=w_ff_up[i], wd=w_ff_down[i])
        else:
            ffn = functools.partial(moe_swiglu, w_router=w_router[i], wg=w_exp_gate[i], wu=w_exp_up[i],
                                    wd=w_exp_down[i])
        x = x + gt2 * ffn(modulate(rmsnorm(x, g_norm_ffn[l]), sh2, sc2))
        if need_ctx:
            xc = xc + gt1c * yc
            xc = xc + gt2c * ffn(modulate(rmsnorm(xc, g_norm_ffn[l]), sh2c, sc2c))
    return rmsnorm(x, g_final)
```

```python
import numpy as np
from contextlib import ExitStack
import concourse.bass as bass
import concourse.mybir as mybir
from concourse.bass_utils import run_bass_kernel_spmd

F32 = mybir.dt.float32
BF16 = mybir.dt.bfloat16
AF = mybir.ActivationFunctionType
ALU = mybir.AluOpType
AX = mybir.AxisListType

SAME_ENGINE_SYNC = True
E3_PHASE2 = True

D = 1024
NCTX = 256
NLAT = 4096
NT = NCTX + NLAT
DEPTH = 2
D_IN = 8608
D_FF = 2816
D_FFE = 3584
NEXP = 8
TILES = [(0, 256)] + [(256 + 512 * i, 512) for i in range(8)]
LAM = 0.6065306597126334


class TU:
    __slots__ = ("name", "w", "r")

    def __init__(self, name=""):
        self.name = name
        self.w = {}
        self.r = {}


class Eng:
    def __init__(self, name, sem):
        self.name = name
        self.sem = sem
        self.cnt = 0
        self.seen = {}
        self.prog = []


class FW:
    def __init__(self, nc, stack):
        self.nc = nc
        self.engs = {}
        for n in ("pe", "act", "dve", "pool", "sp"):
            sem = stack.enter_context(nc.semaphore("sem_" + n))
            self.engs[n] = Eng(n, sem)
        self.dma_sems = {}
        for q, k in (("sp", 24), ("pool", 12), ("act", 4)):
            lst = [[stack.enter_context(nc.semaphore(f"dq_{q}_{i}")), 0] for i in range(k)]
            self.dma_sems[q] = [lst, 0]
        self.n_instr = 0

    def _gather(self, eng, reads, writes):
        deps = {}
        for t in reads:
            for k, (s, v) in t.w.items():
                if deps.get(k, (None, 0))[1] < v:
                    deps[k] = (s, v)
        for t in writes:
            for d in (t.w, t.r):
                for k, (s, v) in d.items():
                    if deps.get(k, (None, 0))[1] < v:
                        deps[k] = (s, v)
        for k, (s, v) in deps.items():
            if k == id(eng.sem) and (eng.name in ("pe", "sp") or not SAME_ENGINE_SYNC):
                continue
            if eng.seen.get(k, 0) < v:
                eng.seen[k] = v
                eng.prog.append(lambda e, s=s, v=v: e.wait_ge(s, v))

    def _record(self, ev, reads, writes):
        k = id(ev[0])
        for t in reads:
            if t.r.get(k, (None, 0))[1] < ev[1]:
                t.r[k] = ev
        for t in writes:
            t.w = {k: ev}
            t.r = {}

    def op(self, engname, fn, reads=(), writes=(), **kw):
        eng = self.engs[engname]
        self._gather(eng, reads, writes)
        eng.cnt += 1
        ev = (eng.sem, eng.cnt)
        sem = eng.sem
        if isinstance(fn, str):
            eng.prog.append(lambda e, fn=fn, sem=sem, kw=kw: getattr(e, fn)(**kw).then_inc(sem, 1))
        else:
            eng.prog.append(lambda e, fn=fn, sem=sem: fn(e).then_inc(sem, 1))
        self._record(ev, reads, writes)
        self.n_instr += 1

    def dma(self, q, out_ap, in_ap, reads=(), writes=(), **kw):
        eng = self.engs[q]
        self._gather(eng, reads, writes)
        lst, idx = self.dma_sems[q]
        ent = lst[idx % len(lst)]
        self.dma_sems[q][1] = idx + 1
        sem, uses = ent
        if uses > 0 and eng.seen.get(id(sem), 0) < 16 * uses:
            eng.seen[id(sem)] = 16 * uses
            eng.prog.append(lambda e, s=sem, v=16 * uses: e.wait_ge(s, v))
        ent[1] = uses + 1
        ev = (sem, 16 * (uses + 1))
        eng.prog.append(lambda e, o=out_ap, i=in_ap, sem=sem, kw=kw: e.dma_start(out=o, in_=i, **kw).then_inc(sem, 16))
        self._record(ev, reads, writes)
        self.n_instr += 1

    def barrier(self):
        evs = []
        for n, e in self.engs.items():
            if e.cnt > 0:
                evs.append((e.sem, e.cnt))
        for q, (lst, _) in self.dma_sems.items():
            for sem, uses in lst:
                if uses > 0:
                    evs.append((sem, 16 * uses))
        for n, e in self.engs.items():
            for s, v in evs:
                if s is e.sem:
                    continue
                if e.seen.get(id(s), 0) < v:
                    e.seen[id(s)] = v
                    e.prog.append(lambda en, s=s, v=v: en.wait_ge(s, v))

    def finish(self):
        nc = self.nc
        with nc.Block() as block:
            @block.tensor
            def _(e):
                for f in self.engs["pe"].prog:
                    f(e)

            @block.scalar
            def _(e):
                for f in self.engs["act"].prog:
                    f(e)

            @block.vector
            def _(e):
                for f in self.engs["dve"].prog:
                    f(e)

            @block.gpsimd
            def _(e):
                for f in self.engs["pool"].prog:
                    f(e)

            @block.sync
            def _(e):
                for f in self.engs["sp"].prog:
                    f(e)


_UID = [0]


class Ring:
    def __init__(self, nc, stack, name, shape, dtype, n, psum=False):
        self.bufs = []
        for i in range(n):
            if psum:
                t = stack.enter_context(nc.psum_tensor(f"{name}{i}", shape, dtype))
            else:
                _UID[0] += 1
                t = stack.enter_context(nc.sbuf_tensor(f"{name}{i}_u{_UID[0]}", shape, dtype))
            self.bufs.append((t, TU(f"{name}{i}")))
        self.i = 0

    def next(self):
        b = self.bufs[self.i % len(self.bufs)]
        self.i += 1
        return b


class DT:
    def __init__(self, nc, name, shape, dtype, kind="Internal"):
        self.t = nc.dram_tensor(name, shape, dtype, kind=kind)
        self.ap = self.t.ap()
        self.tus = {}
        self.name = name

    def tu(self, t0=0, n=NT, key=None):
        out = []
        for c in range(t0 // 128, (t0 + n + 127) // 128):
            k = (key, c)
            if k not in self.tus:
                self.tus[k] = TU(f"{self.name}{k}")
            out.append(self.tus[k])
        return out


def build(debug=None, nlayers=DEPTH, stop=None):
    debug = debug or []
    nc = bass.Bass("TRN2", target_bir_lowering=False)

    def SBT(name, shape, dt):
        _UID[0] += 1
        return nc.sbuf_tensor(f"{name}_u{_UID[0]}", shape, dt)
    I = {}

    def inp(name, shape, dt=F32):
        I[name] = DT(nc, name, shape, dt, kind="ExternalInput")
        return I[name]

    inp("xT", [D, NT]); inp("cs", [D, 2]); inp("sel", [128, 2]); inp("cst", [128, 1024])
    inp("w_ada", [DEPTH, D, 6 * D]); inp("b_ada", [DEPTH, 6 * D])
    inp("g_norm_mix", [DEPTH, D]); inp("g_norm_ffn", [DEPTH, D])
    inp("w_in", [DEPTH, D, D_IN]); inp("b_mlstm_gate", [DEPTH, 4, 8]); inp("g_mlstm_norm", [DEPTH, D])
    inp("mu_shift", [DEPTH, 3456]); inp("w0", [DEPTH, 2, D]); inp("w2", [DEPTH, 2, 64, D])
    inp("a0", [DEPTH, 2, D]); inp("a2", [DEPTH, 2, 64, D]); inp("g2", [DEPTH, 128, D])
    inp("k_k", [DEPTH, D]); inp("k_a", [DEPTH, D]); inp("r_k", [DEPTH, D])
    inp("ln_w", [DEPTH, D]); inp("ln_b", [DEPTH, D])
    inp("w_proj_mlstm", [DEPTH, D, D]); inp("w_proj_rwkv", [DEPTH, D, D]); inp("w_out", [DEPTH, D, D])
    inp("w_ff_gate", [1, D, D_FF]); inp("w_ff_up", [1, D, D_FF]); inp("w_ff_down", [1, D_FF, D])
    inp("w_router", [1, D, NEXP]); inp("w_exp_gate", [1, NEXP, D, D_FFE]); inp("w_exp_up", [1, NEXP, D, D_FFE])
    inp("w_exp_down", [1, NEXP, D_FFE, D]); inp("g_final", [D])
    out = DT(nc, "out", [D, NLAT // 2], F32, kind="ExternalOutput")
    dbg = {}
    for name, shape, dt in debug:
        dbg[name] = DT(nc, "dbg_" + name, shape, dt, kind="ExternalOutput")

    S = {}

    def scr(name, shape, dt):
        if name in dbg:
            S[name] = dbg[name]
        else:
            S[name] = DT(nc, "s_" + name, shape, dt)
        return S[name]

    scr("xres", [D, NT], F32)
    scr("qT", [512, NT], BF16); scr("kT", [512, NT], BF16); scr("ktm", [NT, 512], BF16)
    scr("vtm", [NT, 1024], BF16); scr("otm", [NT, 1024], BF16); scr("gT", [32, NT], F32)
    scr("pT", [3456, NT], BF16); scr("mgT", [2048, NT], BF16)
    scr("hm", [2, NT, 1024], F32); scr("moT", [D, NT], BF16)
    scr("gsc", [2, 3, 8, NT], F32)

    with ExitStack() as st:
        fw = FW(nc, st)
        psall = st.enter_context(nc.psum_tensor("psall", [128, 4096], F32))

        class PSRing:
            def __init__(self):
                self.tus = [TU(f"psb{i}") for i in range(8)]
                self.i = 0

            n = 8

            def next(self):
                b = self.i % self.n
                self.i += 1
                return psall[:, b * 512:(b + 1) * 512], self.tus[b]

            def next2(self):
                if self.i % 2:
                    self.i += 1
                b = self.i % self.n
                self.i += 2
                return psall[:, b * 512:(b + 2) * 512].rearrange("p (h x) -> p h x", x=512), [self.tus[b], self.tus[b + 1]]

            def pair(self, b):
                return psall[:, b * 512:(b + 2) * 512].rearrange("p (h x) -> p h x", x=512), [self.tus[b], self.tus[b + 1]]
        PS = PSRing()
        cst = st.enter_context(SBT("cst_sb", [128, 1024], F32))
        cstb = st.enter_context(SBT("cstb_sb", [128, 1024], BF16))
        t_cst = TU("cst")
        fw.dma("sp", cst[:], I["cst"].ap, writes=[t_cst])
        fw.op("dve", lambda e: e.tensor_copy(out=cstb[:], in_=cst[:]), reads=[t_cst], writes=[t_cst])
        ident_f, ident_b = cst[:, 0:128], cstb[:, 0:128]
        m_le, m_lt, m_ge, m_gt = (cst[:, 128 * i:128 * (i + 1)] for i in range(1, 5))
        blk1_f, blk1_b = cst[:, 640:768], cstb[:, 640:768]
        ones_b = cstb[:, 768:896]
        ones_f = cst[:, 768:896]
        sel2_f = cst[:, 896:898]
        mods = st.enter_context(SBT("mods", [128, DEPTH, 48, 2], F32))
        t_mods = TU("mods")
        prm = st.enter_context(SBT("prm", [128, DEPTH, 6, 8, 2], F32))
        t_prm = TU("prm")
        sel_sb = st.enter_context(SBT("sel_sb", [128, 2], F32))
        fw.dma("sp", sel_sb[:], I["sel"].ap, writes=[t_cst])

        with ExitStack() as sa:
            s_sb = sa.enter_context(SBT("s_sb", [128, 8, 2], F32))
            bada = sa.enter_context(SBT("bada", [128, DEPTH, 48], F32))
            gn = sa.enter_context(SBT("gn", [128, DEPTH, 2, 8], F32))
            t_s, t_b, t_gn = TU("s"), TU("bada"), TU("gn")
            fw.dma("sp", s_sb[:], I["cs"].ap.rearrange("(c p) j -> p c j", p=128), writes=[t_s])
            fw.op("act", lambda e: e.activation(out=s_sb[:], in_=s_sb[:], func=AF.Silu), reads=[t_s], writes=[t_s])
            for l_ in range(DEPTH):
                fw.dma("sp", bada[:, l_, :], I["b_ada"].ap[l_].rearrange("(j p) -> p j", p=128), writes=[t_b], allow_slow_non_contiguous=True)
                fw.dma("sp", gn[:, l_, 0, :], I["g_norm_mix"].ap[l_].rearrange("(c p) -> p c", p=128), writes=[t_gn], allow_slow_non_contiguous=True)
                fw.dma("sp", gn[:, l_, 1, :], I["g_norm_ffn"].ap[l_].rearrange("(c p) -> p c", p=128), writes=[t_gn], allow_slow_non_contiguous=True)
            WA = Ring(nc, sa, "wa", [128, 8, 512], F32, 3)
            for l in range(nlayers):
                ps, t_ps = PS.next()
                for jg in range(12):
                    wt, t_w = WA.next()
                    fw.dma("sp", wt[:], I["w_ada"].ap[l, :, jg * 512:(jg + 1) * 512].rearrange("(c p) m -> p c m", p=128), writes=[t_w])
                    for jj in range(4):
                        j = jg * 4 + jj
                        for c in range(8):
                            fw.op("pe", lambda e, ps=ps, wt=wt, c=c, j=j, jj=jj: e.matmul(ps[:, 2 * j:2 * j + 2], lhsT=wt[:, c, jj * 128:(jj + 1) * 128], rhs=s_sb[:, c, :], start=(c == 0), stop=(c == 7)),
                                  reads=[t_w, t_s], writes=[t_ps])
                fw.op("dve", lambda e, ps=ps, l=l: e.tensor_tensor(out=mods[:, l], in0=ps[:, 0:96].rearrange("p (j t) -> p j t", t=2), in1=bada[:, l].unsqueeze(2).to_broadcast([128, 48, 2]), op=ALU.add),
                      reads=[t_ps, t_b], writes=[t_mods])
                for si in range(2):
                    o = 3 * si
                    fw.op("dve", lambda e, l=l, si=si, o=o: e.scalar_tensor_tensor(out=prm[:, l, o], in0=mods[:, l, (o + 1) * 8:(o + 2) * 8, :], scalar=1.0, in1=gn[:, l, si, :].unsqueeze(2).to_broadcast([128, 8, 2]), op0=ALU.add, op1=ALU.mult),
                          reads=[t_mods, t_gn], writes=[t_prm])
                    fw.op("dve", lambda e, l=l, o=o: e.tensor_copy(out=prm[:, l, o + 1], in_=mods[:, l, o * 8:(o + 1) * 8, :]), reads=[t_mods], writes=[t_prm])
                    fw.op("dve", lambda e, l=l, o=o: e.tensor_copy(out=prm[:, l, o + 2], in_=mods[:, l, (o + 2) * 8:(o + 3) * 8, :]), reads=[t_mods], writes=[t_prm])
            fw.barrier()
        if "mods" in dbg:
            fw.dma("sp", dbg["mods"].ap, mods[:].rearrange("p l j t -> p (l j t)"), reads=[t_mods], writes=dbg["mods"].tu())

        def norm_mod(sx, xsrc_ap, src_tus, t0, n, l, si, h_out, h_tus, work, h32=None):
            xt, t_xt = work["x"].next()
            sq, t_sq = work["sq"].next()
            rs, t_rs = work["rs"].next()
            if callable(xsrc_ap):
                xsrc_ap(xt, t_xt)
            else:
                fw.dma("sp", xt[:, :, :n], xsrc_ap[:, t0:t0 + n].rearrange("(c p) t -> p c t", p=128), reads=src_tus, writes=[t_xt])
            fw.op("act", lambda e: e.activation(out=sq[:, :, :n], in_=xt[:, :, :n], func=AF.Square), reads=[t_xt], writes=[t_sq])
            ps, t_ps = PS.next()
            for c in range(8):
                fw.op("pe", lambda e, c=c: e.matmul(ps[:, :n], lhsT=ones_b, rhs=sq[:, c, :n], start=(c == 0), stop=(c == 7)), reads=[t_sq, t_cst], writes=[t_ps])
            fw.op("act", lambda e: e.activation(out=rs[:, :n], in_=ps[:, :n], func=AF.Sqrt, scale=1.0 / D, bias=1e-6), reads=[t_ps], writes=[t_rs])
            fw.op("dve", lambda e: e.reciprocal(out=rs[:, :n], in_=rs[:, :n]), reads=[t_rs], writes=[t_rs])
            fw.op("dve", lambda e: e.tensor_tensor(out=xt[:, :, :n], in0=xt[:, :, :n], in1=rs[:, :n].unsqueeze(1).to_broadcast([128, 8, n]), op=ALU.mult), reads=[t_rs, t_xt], writes=[t_xt])
            j = 1 if t0 < NCTX else 0
            o = 3 * si
            for c in range(8):
                if c % 2 == 0:
                    fw.op("dve", lambda e, c=c: e.tensor_scalar(out=h_out[:, c, :], in0=xt[:, c, :n], scalar1=prm[:, l, o, c, j:j + 1], scalar2=prm[:, l, o + 1, c, j:j + 1], op0=ALU.mult, op1=ALU.add),
                          reads=[t_xt, t_prm], writes=h_tus)
                else:
                    fw.op("act", lambda e, c=c: e.activation(out=h_out[:, c, :], in_=xt[:, c, :n], func=AF.Identity, scale=prm[:, l, o, c, j:j + 1], bias=prm[:, l, o + 1, c, j:j + 1]),
                          reads=[t_xt, t_prm], writes=h_tus)
                if h32 is not None:
                    if c % 2 == 1:
                        fw.op("dve", lambda e, c=c: e.tensor_scalar(out=h32[0][:, c, :n], in0=xt[:, c, :n], scalar1=prm[:, l, o, c, j:j + 1], scalar2=prm[:, l, o + 1, c, j:j + 1], op0=ALU.mult, op1=ALU.add),
                              reads=[t_xt, t_prm], writes=[h32[1]])
                    else:
                        fw.op("act", lambda e, c=c: e.activation(out=h32[0][:, c, :n], in_=xt[:, c, :n], func=AF.Identity, scale=prm[:, l, o, c, j:j + 1], bias=prm[:, l, o + 1, c, j:j + 1]),
                              reads=[t_xt, t_prm], writes=[h32[1]])

        for l in range(nlayers):
            xsrc = I["xT"] if l == 0 else S["xres"]
            with ExitStack() as sb:
                hT = sb.enter_context(SBT("hT", [128, 8, NT], BF16))
                t_h = [TU(f"h{i}") for i in range(len(TILES))]
                work = {"x": Ring(nc, sb, "nx", [128, 8, 512], F32, 2), "sq": Ring(nc, sb, "nsq", [128, 8, 512], BF16, 1), "rs": Ring(nc, sb, "nrs", [128, 512], F32, 2)}
                for ti, (t0, n) in enumerate(TILES):
                    norm_mod(sb, xsrc.ap, xsrc.tu(t0, n), t0, n, l, 0, hT[:, :, t0:t0 + n], [t_h[ti]], work)
                if l == 0 and "hT" in dbg:
                    for ti, (t0, n) in enumerate(TILES):
                        fw.dma("sp", dbg["hT"].ap[:, t0:t0 + n].rearrange("(c p) t -> p c t", p=128), hT[:, :, t0:t0 + n], reads=[t_h[ti]], writes=dbg["hT"].tu(t0, n))
                WS = Ring(nc, sb, "ws", [128, 8, 512], F32, 2)
                WB = Ring(nc, sb, "wb", [128, 8, 512], BF16, 2)
                EV = Ring(nc, sb, "ev", [128, 512], BF16, 4)
                EVF = Ring(nc, sb, "evf", [128, 512], F32, 2)
                groups = [(0, 512, "fq", None), (512, 512, "fk", None), (512, 512, "tk", None),
                          (1024, 512, "tv", 0), (1536, 512, "tv", 512), (2048, 512, "to", 0), (2560, 512, "to", 512),
                          (3072, 32, "fg", None)]
                for i in range(7):
                    w = 512 if i < 6 else 384
                    groups.append((3104 + 512 * i, w, "fp", 512 * i))
                for i in range(4):
                    groups.append((6560 + 512 * i, 512, "fm", 512 * i))
                evi = 0
                for (c0, w, kind, dst) in groups:
                    ws, t_ws = WS.next()
                    wb, t_wb = WB.next()
                    fw.dma("sp", ws[:, :, :w], I["w_in"].ap[l, :, c0:c0 + w].rearrange("(c p) m -> p c m", p=128), writes=[t_ws])
                    fw.op("dve", lambda e, ws=ws, wb=wb, w=w: e.tensor_copy(out=wb[:, :, :w], in_=ws[:, :, :w]), reads=[t_ws], writes=[t_wb])
                    for ti, (t0, n) in enumerate(TILES):
                        if kind[0] == "f":
                            for mb in range((w + 127) // 128):
                                mw = min(128, w - mb * 128)
                                ps, t_ps = PS.next()
                                for c in range(8):
                                    fw.op("pe", lambda e, ps=ps, wb=wb, c=c, mb=mb, mw=mw, t0=t0, n=n: e.matmul(ps[:mw, :n], lhsT=wb[:, c, mb * 128:mb * 128 + mw], rhs=hT[:, c, t0:t0 + n], start=(c == 0), stop=(c == 7)),
                                          reads=[t_wb, t_h[ti]], writes=[t_ps])
                                evi += 1
                                if kind == "fg":
                                    ev, t_ev = EVF.next()
                                    fw.op("dve", lambda e, ev=ev, ps=ps, mw=mw, n=n: e.tensor_copy(out=ev[:mw, :n], in_=ps[:mw, :n]), reads=[t_ps], writes=[t_ev])
                                    fw.dma("pool", S["gT"].ap[:, t0:t0 + n], ev[:mw, :n], reads=[t_ev], writes=S["gT"].tu(t0, n))
                                    continue
                                ev, t_ev = EV.next()
                                if kind == "fm":
                                    fw.op("act", lambda e, ev=ev, ps=ps, mw=mw, n=n: e.activation(out=ev[:mw, :n], in_=ps[:mw, :n], func=AF.Sigmoid), reads=[t_ps], writes=[t_ev])
                                elif kind == "fq":
                                    fw.op("act", lambda e, ev=ev, ps=ps, mw=mw, n=n: e.activation(out=ev[:mw, :n], in_=ps[:mw, :n], func=AF.Copy, scale=0.125), reads=[t_ps], writes=[t_ev])
                                elif evi % 2 == 0:
                                    fw.op("act", lambda e, ev=ev, ps=ps, mw=mw, n=n: e.activation(out=ev[:mw, :n], in_=ps[:mw, :n], func=AF.Copy), reads=[t_ps], writes=[t_ev])
                                else:
                                    fw.op("dve", lambda e, ev=ev, ps=ps, mw=mw, n=n: e.tensor_copy(out=ev[:mw, :n], in_=ps[:mw, :n]), reads=[t_ps], writes=[t_ev])
                                dname = {"fq": "qT", "fk": "kT", "fp": "pT", "fm": "mgT"}[kind]
                                r0 = (dst or 0) + mb * 128
                                fw.dma("pool", S[dname].ap[r0:r0 + mw, t0:t0 + n], ev[:mw, :n], reads=[t_ev], writes=S[dname].tu(t0, n, key=r0))
                        else:
                            for sub in range(n // 128):
                                ts = t0 + sub * 128
                                ps, t_ps = PS.next()
                                for c in range(8):
                                    fw.op("pe", lambda e, ps=ps, wb=wb, c=c, ts=ts, w=w: e.matmul(ps[:, :w], lhsT=hT[:, c, ts:ts + 128], rhs=wb[:, c, :w], start=(c == 0), stop=(c == 7)),
                                          reads=[t_wb, t_h[ti]], writes=[t_ps])
                                ev, t_ev = EV.next()
                                evi += 1
                                if kind == "to":
                                    fw.op("act", lambda e, ev=ev, ps=ps: e.activation(out=ev[:], in_=ps[:], func=AF.Sigmoid), reads=[t_ps], writes=[t_ev])
                                elif evi % 2 == 0:
                                    fw.op("act", lambda e, ev=ev, ps=ps: e.activation(out=ev[:], in_=ps[:], func=AF.Copy), reads=[t_ps], writes=[t_ev])
                                else:
                                    fw.op("dve", lambda e, ev=ev, ps=ps: e.tensor_copy(out=ev[:], in_=ps[:]), reads=[t_ps], writes=[t_ev])
                                dname = {"tk": "ktm", "tv": "vtm", "to": "otm"}[kind]
                                d0 = dst or 0
                                fw.dma("pool", S[dname].ap[ts:ts + 128, d0:d0 + w], ev[:, :w], reads=[t_ev], writes=S[dname].tu(ts, 128, key=d0))
                fw.barrier()
            if stop == "C":
                break
            NCH = NT // 64
            with ExitStack() as sd:
                etm = sd.enter_context(SBT("etm", [64, 2, NCH, 8], F32))
                ctm = sd.enter_context(SBT("ctm", [64, 2, NCH, 8], F32))
                omb = sd.enter_context(SBT("omb", [64, 2, NCH, 8], F32))
                t_etm, t_ctm, t_omb = TU("etm"), TU("ctm"), TU("omb")
                orders = [list(range(NCH)), [3, 2, 1, 0] + list(range(NCH - 1, 3, -1))]
                with ExitStack() as sd1:
                    bg = sd1.enter_context(SBT("bg", [8, 4], F32))
                    rmask = sd1.enter_context(SBT("rmask", [8, NT], F32))
                    t_bg, t_rm = TU("bg"), TU("rm")
                    fw.dma("sp", bg[:], I["b_mlstm_gate"].ap[l].rearrange("j h -> h j"), writes=[t_bg], allow_slow_non_contiguous=True)
                    fw.op("dve", lambda e: e.tensor_scalar(out=bg[:], in0=bg[:], scalar1=1.0 / 15.0, scalar2=None, op0=ALU.mult), reads=[t_bg], writes=[t_bg])
                    fw.op("pool", lambda e: e.memset(rmask[:], 1.0), writes=[t_rm])
                    fw.op("pool", lambda e: e.memset(rmask[:].rearrange("p (c l) -> p c l", l=64)[:, :, 0:1], 0.0), writes=[t_rm])
                    for d in range(2):
                        It = sd1.enter_context(SBT(f"It{d}", [8, NT], F32))
                        Ft = sd1.enter_context(SBT(f"Ft{d}", [8, NT], F32))
                        Cs = sd1.enter_context(SBT(f"Cs{d}", [8, NT], F32))
                        Bn = sd1.enter_context(SBT(f"Bn{d}", [8, NT], F32))
                        sm_ = sd1.enter_context(SBT(f"gsm{d}", [8, 8 * NCH], F32))
                        t_i, t_f, t_c, t_b, t_s = TU("It"), TU("Ft"), TU("Cs"), TU("Bn"), TU("gsm")
                        tot = sm_[:, 0:NCH]; G = sm_[:, NCH:2 * NCH]; Mc = sm_[:, 2 * NCH:3 * NCH]; mm_ = sm_[:, 3 * NCH:4 * NCH + 1]; om = sm_[:, 5 * NCH:6 * NCH]
                        fw.dma("sp", It[:], S["gT"].ap[(2 * d) * 8:(2 * d) * 8 + 8, :], reads=S["gT"].tu(), writes=[t_i])
                        fw.dma("sp", Ft[:], S["gT"].ap[(2 * d + 1) * 8:(2 * d + 1) * 8 + 8, :], reads=S["gT"].tu(), writes=[t_f])
                        fw.op("act", lambda e, It=It, d=d: e.activation(out=It[:], in_=It[:], func=AF.Tanh, scale=1.0 / 15.0, bias=bg[:, 2 * d:2 * d + 1]), reads=[t_i, t_bg], writes=[t_i])
                        fw.op("act", lambda e, Ft=Ft, d=d: e.activation(out=Ft[:], in_=Ft[:], func=AF.Tanh, scale=1.0 / 15.0, bias=bg[:, 2 * d + 1:2 * d + 2]), reads=[t_f, t_bg], writes=[t_f])
                        fw.op("act", lambda e, Ft=Ft: e.activation(out=Ft[:], in_=Ft[:], func=AF.Exp, scale=-15.0), reads=[t_f], writes=[t_f])
                        fw.op("act", lambda e, Ft=Ft: e.activation(out=Ft[:], in_=Ft[:], func=AF.Ln, bias=1.0), reads=[t_f], writes=[t_f])
                        fw.op("dve", lambda e, Cs=Cs, Ft=Ft: e.tensor_tensor_scan(out=Cs[:], data0=rmask[:], data1=Ft[:], initial=0.0, op0=ALU.mult, op1=ALU.add), reads=[t_rm, t_f], writes=[t_c])
                        cs3 = Cs[:].rearrange("p (c l) -> p c l", l=64)
                        fw.op("dve", lambda e, tot=tot, cs3=cs3: e.tensor_copy(out=tot.unsqueeze(2), in_=cs3[:, :, 63:64]), reads=[t_c], writes=[t_s])
                        if d == 0:
                            fw.op("dve", lambda e, Bn=Bn, Cs=Cs: e.tensor_copy(out=Bn[:], in_=Cs[:]), reads=[t_c], writes=[t_b])
                        else:
                            fw.op("dve", lambda e, Bn=Bn, Cs=Cs, Ft=Ft: e.tensor_tensor(out=Bn[:], in0=Ft[:], in1=Cs[:], op=ALU.subtract), reads=[t_c, t_f], writes=[t_b])
                            bn3 = Bn[:].rearrange("p (c l) -> p c l", l=64)
                            fw.op("dve", lambda e, bn3=bn3, tot=tot: e.tensor_tensor(out=bn3, in0=bn3, in1=tot.unsqueeze(2).to_broadcast([8, NCH, 64]), op=ALU.add), reads=[t_b, t_s], writes=[t_b])
                        fw.op("dve", lambda e, It=It, Bn=Bn: e.scalar_tensor_tensor(out=It[:], in0=It[:], scalar=15.0, in1=Bn[:], op0=ALU.mult, op1=ALU.add), reads=[t_i, t_b], writes=[t_i])
                        it3 = It[:].rearrange("p (c l) -> p c l", l=64)
                        fw.op("dve", lambda e, G=G, it3=it3: e.tensor_reduce(out=G, in_=it3, axis=AX.X, op=ALU.max), reads=[t_i], writes=[t_s])
                        fw.op("dve", lambda e, mm_=mm_: e.memset(mm_[:, 0:1], 0.0), writes=[t_s])
                        for i, c in enumerate(orders[d]):
                            fw.op("dve", lambda e, i=i, c=c, Mc=Mc, mm_=mm_, G=G: e.tensor_tensor(out=Mc[:, c:c + 1], in0=mm_[:, i:i + 1], in1=G[:, c:c + 1], op=ALU.max), reads=[t_s], writes=[t_s])
                            fw.op("dve", lambda e, i=i, c=c, Mc=Mc, mm_=mm_, om=om: e.tensor_tensor(out=om[:, c:c + 1], in0=mm_[:, i:i + 1], in1=Mc[:, c:c + 1], op=ALU.subtract), reads=[t_s], writes=[t_s])
                            fw.op("dve", lambda e, i=i, c=c, Mc=Mc, mm_=mm_, tot=tot: e.tensor_tensor(out=mm_[:, i + 1:i + 2], in0=Mc[:, c:c + 1], in1=tot[:, c:c + 1], op=ALU.subtract), reads=[t_s], writes=[t_s])
                        fw.op("act", lambda e, om=om: e.activation(out=om, in_=om, func=AF.Exp), reads=[t_s], writes=[t_s])
                        mcb = Mc.unsqueeze(2).to_broadcast([8, NCH, 64])
                        fw.op("dve", lambda e, it3=it3, mcb=mcb: e.tensor_tensor(out=it3, in0=it3, in1=mcb, op=ALU.subtract), reads=[t_i, t_s], writes=[t_i])
                        fw.op("act", lambda e, It=It: e.activation(out=It[:], in_=It[:], func=AF.Exp), reads=[t_i], writes=[t_i])
                        bn3 = Bn[:].rearrange("p (c l) -> p c l", l=64)
                        fw.op("dve", lambda e, bn3=bn3, mcb=mcb: e.tensor_tensor(out=bn3, in0=bn3, in1=mcb, op=ALU.subtract), reads=[t_b, t_s], writes=[t_b])
                        fw.op("act", lambda e, Bn=Bn: e.activation(out=Bn[:], in_=Bn[:], func=AF.Exp), reads=[t_b], writes=[t_b])
                        for (src, t_src, dst, t_dst) in ((It, t_i, etm, t_etm), (Bn, t_b, ctm, t_ctm)):
                            for half in range(2):
                                ps, t_ps = PS.next()
                                for cc in range(NCH // 2):
                                    c = half * (NCH // 2) + cc
                                    fw.op("pe", lambda e, ps=ps, src=src, c=c, cc=cc: e.matmul(ps[0:64, cc * 8:cc * 8 + 8], lhsT=src[:, c * 64:(c + 1) * 64], rhs=ident_f[0:8, 0:8], start=True, stop=True),
                                          reads=[t_src, t_cst], writes=[t_ps])
                                fw.op("dve", lambda e, ps=ps, dst=dst, half=half, d=d: e.tensor_copy(out=dst[:, d, half * (NCH // 2):(half + 1) * (NCH // 2), :], in_=ps[0:64, 0:(NCH // 2) * 8].rearrange("p (c h) -> p c h", h=8)),
                                      reads=[t_ps], writes=[t_dst])
                        X = sd1.enter_context(SBT(f"omx{d}", [8, NCH, 8], F32))
                        t_x = TU("omx")
                        fw.op("dve", lambda e, X=X, om=om: e.tensor_tensor(out=X[:], in0=om.unsqueeze(2).to_broadcast([8, NCH, 8]), in1=ident_f[0:8, 0:8].unsqueeze(1).to_broadcast([8, NCH, 8]), op=ALU.mult), reads=[t_s, t_cst], writes=[t_x])
                        for half in range(2):
                            ps, t_ps = PS.next()
                            hw = (NCH // 2) * 8
                            fw.op("pe", lambda e, ps=ps, X=X, half=half, hw=hw: e.matmul(ps[0:64, 0:hw], lhsT=ones_f[0:8, 0:64], rhs=X[:].rearrange("p c h -> p (c h)")[:, half * hw:(half + 1) * hw], start=True, stop=True), reads=[t_x, t_cst], writes=[t_ps])
                            fw.op("dve", lambda e, ps=ps, half=half, hw=hw, d=d: e.tensor_copy(out=omb[:, d, half * (NCH // 2):(half + 1) * (NCH // 2), :], in_=ps[0:64, 0:hw].rearrange("p (c h) -> p c h", h=8)), reads=[t_ps], writes=[t_omb])
                    fw.barrier()
                for nm_, tl_, tt_ in (("etm", etm, t_etm), ("ctm", ctm, t_ctm), ("omb", omb, t_omb)):
                    if nm_ in dbg:
                        fw.dma("sp", dbg[nm_].ap, tl_[:].rearrange("p d c h -> p (d c h)"), reads=[tt_], writes=dbg[nm_].tu())
                with ExitStack() as sd2:
                    QC = Ring(nc, sd2, "qc", [64, 8, 64], BF16, 4)
                    KC = Ring(nc, sd2, "kc", [64, 8, 64], BF16, 4)
                    KM = Ring(nc, sd2, "km", [64, 8, 64], BF16, 4)
                    VA = Ring(nc, sd2, "va", [64, 8, 130], BF16, 4)
                    SMF = Ring(nc, sd2, "smf", [64, 8, 64], F32, 4)
                    SM = Ring(nc, sd2, "sm", [64, 8, 64], BF16, 4)
                    KP = Ring(nc, sd2, "kp", [64, 8, 64], BF16, 4)
                    HO = Ring(nc, sd2, "ho", [64, 8, 128], F32, 4)
                    DEN = Ring(nc, sd2, "den", [64, 8], F32, 4)
                    for (va, t_va) in VA.bufs:
                        fw.op("pool", lambda e, va=va: e.memset(va[:, :, 128:130], 1.0), writes=[t_va])
                    st_d = []
                    for d in range(2):
                        C32 = sd2.enter_context(SBT(f"C32_{d}", [64, 8, 129], F32))
                        Cb = sd2.enter_context(SBT(f"Cb_{d}", [64, 8, 129], BF16))
                        t_C, t_Cb = TU("C32"), TU("Cb")
                        fw.op("pool", lambda e, C32=C32: e.memset(C32[:], 0.0), writes=[t_C])
                        fw.op("pool", lambda e, Cb=Cb: e.memset(Cb[:], 0.0), writes=[t_Cb])
                        mask = (m_le if d == 0 else m_ge)[0:64, 0:64]
                        st_d.append((C32, Cb, t_C, t_Cb, mask))
                    for i in range(NCH):
                        for d in range(2):
                            C32, Cb, t_C, t_Cb, mask = st_d[d]
                            order = orders[d]
                            c = order[i]
                            t0 = c * 64
                            qc, t_qc = QC.next(); kc, t_kc = KC.next(); km, t_km = KM.next(); va, t_va = VA.next()
                            fw.dma("sp", qc[:], S["qT"].ap[:, t0:t0 + 64].rearrange("(h d) t -> d h t", d=64), reads=[x for r0 in range(0, 512, 128) for x in S["qT"].tu(t0, 64, key=r0)], writes=[t_qc])
                            fw.dma("sp", kc[:], S["kT"].ap[:, t0:t0 + 64].rearrange("(h d) t -> d h t", d=64), reads=[x for r0 in range(0, 512, 128) for x in S["kT"].tu(t0, 64, key=r0)], writes=[t_kc])
                            fw.dma("sp", km[:].rearrange("p h d -> p (h d)"), S["ktm"].ap[t0:t0 + 64, :], reads=S["ktm"].tu(t0, 64, key=0), writes=[t_km])
                            fw.dma("sp", va[:, :, 0:128], S["vtm"].ap[t0:t0 + 64, :].rearrange("t (h v) -> t h v", v=128), reads=S["vtm"].tu(t0, 64, key=0) + S["vtm"].tu(t0, 64, key=512), writes=[t_va])
                            ps1, t_ps1 = PS.next()
                            for h in range(8):
                                fw.op("pe", lambda e, ps1=ps1, kc=kc, qc=qc, h=h: e.matmul(ps1[0:64, h * 64:(h + 1) * 64], lhsT=kc[:, h, :], rhs=qc[:, h, :], start=True, stop=True), reads=[t_kc, t_qc], writes=[t_ps1])
                            smf, t_smf = SMF.next(); sm, t_sm = SM.next(); kp, t_kp = KP.next()
                            eb = etm[:, d, c, :].unsqueeze(2).to_broadcast([64, 8, 64])
                            fw.op("dve", lambda e, smf=smf, ps1=ps1, eb=eb: e.tensor_tensor(out=smf[:], in0=ps1[0:64, :].rearrange("p (h t) -> p h t", t=64), in1=eb, op=ALU.mult), reads=[t_ps1, t_etm], writes=[t_smf])
                            fw.op("dve", lambda e, smf=smf, sm=sm, mask=mask: e.tensor_tensor(out=sm[:], in0=smf[:], in1=mask.unsqueeze(1).to_broadcast([64, 8, 64]), op=ALU.mult), reads=[t_smf, t_cst], writes=[t_sm])
                            fw.op("dve", lambda e, kp=kp, km=km, eb=eb: e.tensor_tensor(out=kp[:], in0=km[:], in1=eb, op=ALU.mult), reads=[t_km, t_etm], writes=[t_kp])
                            groups3 = [(0, 3), (3, 3), (6, 2)]
                            psn = [PS.next() for _ in range(3)]
                            for gi, (h0, nh) in enumerate(groups3):
                                for j in range(nh):
                                    h = h0 + j
                                    fw.op("pe", lambda e, p=psn[gi][0], sm=sm, va=va, h=h, j=j: e.matmul(p[0:64, j * 129:(j + 1) * 129], lhsT=sm[:, h, :], rhs=va[:, h, 0:129], start=True, stop=False), reads=[t_sm, t_va], writes=[psn[gi][1]])
                                    fw.op("pe", lambda e, p=psn[gi][0], qc=qc, Cb=Cb, h=h, j=j: e.matmul(p[0:64, j * 129:(j + 1) * 129], lhsT=qc[:, h, :], rhs=Cb[:, h, :], start=False, stop=True), reads=[t_qc, t_Cb], writes=[psn[gi][1]])
                            ho, t_ho = HO.next(); den, t_den = DEN.next()
                            for gi, (h0, nh) in enumerate(groups3):
                                pv = psn[gi][0][0:64, 0:nh * 129].rearrange("p (h n) -> p h n", n=129)
                                fw.op("act", lambda e, den=den, pv=pv, h0=h0, nh=nh: e.activation(out=den[:, h0:h0 + nh].unsqueeze(2), in_=pv[:, :, 128:129], func=AF.Abs), reads=[psn[gi][1]], writes=[t_den])
                            fw.op("dve", lambda e, den=den, d=d, c=c: e.tensor_tensor(out=den[:], in0=den[:], in1=ctm[:, d, c, :], op=ALU.max), reads=[t_den, t_ctm], writes=[t_den])
                            fw.op("dve", lambda e, den=den: e.reciprocal(out=den[:], in_=den[:]), reads=[t_den], writes=[t_den])
                            for gi, (h0, nh) in enumerate(groups3):
                                pv = psn[gi][0][0:64, 0:nh * 129].rearrange("p (h n) -> p h n", n=129)
                                fw.op("dve", lambda e, ho=ho, den=den, pv=pv, h0=h0, nh=nh: e.tensor_tensor(out=ho[:, h0:h0 + nh, :], in0=pv[:, :, 0:128], in1=den[:, h0:h0 + nh].unsqueeze(2).to_broadcast([64, nh, 128]), op=ALU.mult), reads=[psn[gi][1], t_den], writes=[t_ho])
                            fw.dma("pool", S["hm"].ap[d, t0:t0 + 64, :], ho[:].rearrange("p h v -> p (h v)"), reads=[t_ho], writes=S["hm"].tu(t0, 64, key=d))
                            if i + 1 < len(order):
                                psc = [PS.next() for _ in range(3)]
                                for gi, (h0, nh) in enumerate(groups3):
                                    for j in range(nh):
                                        h = h0 + j
                                        fw.op("pe", lambda e, p=psc[gi][0], kp=kp, va=va, h=h, j=j: e.matmul(p[0:64, j * 129:(j + 1) * 129], lhsT=kp[:, h, :], rhs=va[:, h, 0:129], start=True, stop=True), reads=[t_kp, t_va], writes=[psc[gi][1]])
                                for gi, (h0, nh) in enumerate(groups3):
                                    pv = psc[gi][0][0:64, 0:nh * 129].rearrange("p (h n) -> p h n", n=129)
                                    fw.op("dve", lambda e, C32=C32, pv=pv, h0=h0, nh=nh: e.tensor_tensor(out=C32[:, h0:h0 + nh, :], in0=pv, in1=C32[:, h0:h0 + nh, :], op=ALU.add), reads=[psc[gi][1], t_C], writes=[t_C])
                                cn = order[i + 1]
                                fw.op("dve", lambda e, C32=C32, cn=cn, d=d: e.tensor_tensor(out=C32[:], in0=C32[:], in1=omb[:, d, cn, :].unsqueeze(2).to_broadcast([64, 8, 129]), op=ALU.mult), reads=[t_C, t_omb], writes=[t_C])
                                fw.op("act", lambda e, C32=C32, Cb=Cb: e.activation(out=Cb[:], in_=C32[:], func=AF.Copy), reads=[t_C], writes=[t_Cb])
                    fw.barrier()
                with ExitStack() as sd3:
                    gmb = sd3.enter_context(SBT("gmb", [128, 1024], F32))
                    t_gmb = TU("gmb")
                    fw.dma("sp", gmb[:], I["g_mlstm_norm"].ap[l].partition_broadcast(128), writes=[t_gmb])
                    HF = Ring(nc, sd3, "hf", [128, 1024], F32, 2)
                    HB = Ring(nc, sd3, "hb", [128, 1024], F32, 2)
                    SO = Ring(nc, sd3, "so", [128, 1024], BF16, 2)
                    SQ = Ring(nc, sd3, "hsq", [128, 1024], F32, 1)
                    SS = Ring(nc, sd3, "hss", [128, 8], F32, 2)
                    MO = Ring(nc, sd3, "mo", [128, 1024], BF16, 2)
                    MT = Ring(nc, sd3, "mt", [128, 8, 128], BF16, 2)
                    for tt in range(NT // 128):
                        t0 = tt * 128
                        hf, t_hf = HF.next(); hb, t_hb = HB.next(); so, t_so = SO.next(); sq, t_sq = SQ.next(); ss, t_ss = SS.next(); mo, t_mo = MO.next(); mt, t_mt = MT.next()
                        fw.dma("sp", hf[:], S["hm"].ap[0, t0:t0 + 128, :], reads=S["hm"].tu(t0, 128, key=0), writes=[t_hf])
                        fw.dma("sp", hb[:], S["hm"].ap[1, t0:t0 + 128, :], reads=S["hm"].tu(t0, 128, key=1), writes=[t_hb])
                        fw.dma("sp", so[:], S["otm"].ap[t0:t0 + 128, :], reads=S["otm"].tu(t0, 128, key=0) + S["otm"].tu(t0, 128, key=512), writes=[t_so])
                        fw.op("dve", lambda e, hf=hf, hb=hb: e.tensor_tensor(out=hf[:], in0=hf[:], in1=hb[:], op=ALU.add), reads=[t_hf, t_hb], writes=[t_hf])
                        fw.op("act", lambda e, sq=sq, hf=hf: e.activation(out=sq[:], in_=hf[:], func=AF.Square), reads=[t_hf], writes=[t_sq])
                        fw.op("dve", lambda e, ss=ss, sq=sq: e.tensor_reduce(out=ss[:], in_=sq[:].rearrange("p (h v) -> p h v", v=128), axis=AX.X, op=ALU.add), reads=[t_sq], writes=[t_ss])
                        fw.op("act", lambda e, ss=ss: e.activation(out=ss[:], in_=ss[:], func=AF.Sqrt, scale=1.0 / 128, bias=1e-6), reads=[t_ss], writes=[t_ss])
                        fw.op("dve", lambda e, ss=ss: e.reciprocal(out=ss[:], in_=ss[:]), reads=[t_ss], writes=[t_ss])
                        hf3 = hf[:].rearrange("p (h v) -> p h v", v=128)
                        fw.op("dve", lambda e, hf3=hf3, ss=ss: e.tensor_tensor(out=hf3, in0=hf3, in1=ss[:].unsqueeze(2).to_broadcast([128, 8, 128]), op=ALU.mult), reads=[t_hf, t_ss], writes=[t_hf])
                        fw.op("pool", lambda e, hf=hf: e.tensor_tensor(out=hf[:], in0=hf[:], in1=gmb[:], op=ALU.mult), reads=[t_hf, t_gmb], writes=[t_hf])
                        fw.op("dve", lambda e, hf=hf, so=so, mo=mo: e.tensor_tensor(out=mo[:], in0=hf[:], in1=so[:], op=ALU.mult), reads=[t_hf, t_so], writes=[t_mo])
                        if "motm" in dbg:
                            fw.dma("pool", dbg["motm"].ap[t0:t0 + 128, :], mo[:], reads=[t_mo], writes=dbg["motm"].tu(t0, 128))
                        for hb2 in range(2):
                            ps, t_ps = PS.next()
                            for j in range(4):
                                cb = hb2 * 4 + j
                                fw.op("pe", lambda e, ps=ps, mo=mo, cb=cb, j=j: e.matmul(ps[:, j * 128:(j + 1) * 128], lhsT=mo[:, cb * 128:(cb + 1) * 128], rhs=ident_b, start=True, stop=True), reads=[t_mo, t_cst], writes=[t_ps])
                            fw.op("act", lambda e, ps=ps, mt=mt, hb2=hb2: e.activation(out=mt[:, hb2 * 4:(hb2 + 1) * 4, :], in_=ps[:].rearrange("p (c t) -> p c t", t=128), func=AF.Copy), reads=[t_ps], writes=[t_mt])
                        fw.dma("pool", S["moT"].ap[:, t0:t0 + 128].rearrange("(c p) t -> p c t", p=128), mt[:], reads=[t_mt], writes=S["moT"].tu(t0, 128))
                    fw.barrier()
            if stop == "D":
                break
            NC2 = NT // 128

            def TT(eng, out, in0, in1, op, reads, writes):
                fw.op(eng, "tensor_tensor", reads, writes, out=out, in0=in0, in1=in1, op=op)

            def TS(eng, out, in0, s1, s2, op0, op1, reads, writes):
                if op1 is None:
                    fw.op(eng, "tensor_scalar", reads, writes, out=out, in0=in0, scalar1=s1, scalar2=None, op0=op0)
                else:
                    fw.op(eng, "tensor_scalar", reads, writes, out=out, in0=in0, scalar1=s1, scalar2=s2, op0=op0, op1=op1)

            def STT(eng, out, in0, scalar, in1, op0, op1, reads, writes):
                fw.op(eng, "scalar_tensor_tensor", reads, writes, out=out, in0=in0, scalar=scalar, in1=in1, op0=op0, op1=op1)

            def ACT(out, in_, func, reads, writes, **kw):
                fw.op("act", "activation", reads, writes, out=out, in_=in_, func=func, **kw)

            def MM(out, lhsT, rhs, reads, writes, start=True, stop=True):
                fw.op("pe", "matmul", reads, writes, out=out, lhsT=lhsT, rhs=rhs, start=start, stop=stop)

            if l == 0:
                for nm_ in ("abT", "rbT", "bbT", "kbT"):
                    scr(nm_, [2, D, NT], BF16)
                scr("Khat", [2, NT, D], BF16); scr("Bhat", [2, NT, D], BF16); scr("v_tm", [NT, D], BF16)
                scr("gdT", [128, NT], BF16); scr("yr", [2, NT, D], F32)
                scr("Tinv", [2, NC2, 128, 16, 128], BF16); scr("roT", [D, NT], BF16)
            with ExitStack() as se:
                plb = se.enter_context(SBT("plb", [128, 2, 8, NC2], F32))
                bon = se.enter_context(SBT("bon", [128, NC2, 16], F32))
                t_plb, t_bon = TU("plb"), TU("bon")
                with ExitStack() as se1:
                    def colload(name, src1d, ncol):
                        t = se1.enter_context(SBT(name, [128, ncol], F32))
                        tu_ = TU(name)
                        fw.dma("sp", t[:], src1d.rearrange("(j p) -> p j", p=128), writes=[tu_], allow_slow_non_contiguous=True)
                        return t, tu_
                    mu, t_mu = colload("mu_sb", I["mu_shift"].ap[l], 27)
                    a1 = se1.enter_context(SBT("a1_sb", [128, 27], F32))
                    TS("dve", a1[:], mu[:], -1.0, 1.0, ALU.mult, ALU.add, [t_mu], [t_mu])
                    kk_c, t_kkc = colload("kk_c", I["k_k"].ap[l], 8)
                    ka_c, t_kac = colload("ka_c", I["k_a"].ap[l], 8)
                    nka_c = se1.enter_context(SBT("nka_c", [128, 8], F32))
                    TS("dve", nka_c[:], ka_c[:], -1.0, None, ALU.mult, None, [t_kac], [t_kac])
                    rk_c, t_rkc = colload("rk_c", I["r_k"].ap[l], 8)
                    w0_c = [colload(f"w0_c{d}", I["w0"].ap[l, d], 8) for d in range(2)]
                    a0_c = [colload(f"a0_c{d}", I["a0"].ap[l, d], 8) for d in range(2)]
                    w2b = se1.enter_context(SBT("w2b", [128, D], BF16))
                    a2b = se1.enter_context(SBT("a2b", [128, D], BF16))
                    t_w2, t_a2 = TU("w2"), TU("a2")
                    with ExitStack() as se0:
                        w2f = se0.enter_context(SBT("w2f", [128, D], F32))
                        a2f = se0.enter_context(SBT("a2f", [128, D], F32))
                        fw.dma("sp", w2f[:], I["w2"].ap[l].rearrange("d k c -> (d k) c"), writes=[t_w2])
                        fw.dma("sp", a2f[:], I["a2"].ap[l].rearrange("d k c -> (d k) c"), writes=[t_a2])
                        fw.op("dve", "tensor_copy", [t_w2], [t_w2], out=w2b[:], in_=w2f[:])
                        fw.op("dve", "tensor_copy", [t_a2], [t_a2], out=a2b[:], in_=a2f[:])
                        fw.barrier()
                    rmask2 = se1.enter_context(SBT("rmask2", [128, NT // 2], F32))
                    t_rm2 = TU("rm2")
                    fw.op("pool", "memset", [], [t_rm2], ap=rmask2[:], constant=1.0)
                    fw.op("pool", "memset", [], [t_rm2], ap=rmask2[:].rearrange("p (c l) -> p c l", l=128)[:, :, 0:1], constant=0.0)
                    NP = NT // 2
                    Pt = se1.enter_context(SBT("Pt", [128, NT], BF16)); t_P = TU("Pt")
                    SH = se1.enter_context(SBT("SH", [128, NP], F32)); t_SH = TU("SH")
                    wdT = se1.enter_context(SBT("wdT", [128, NT], BF16)); t_wd = TU("wdT")
                    adT = se1.enter_context(SBT("adT", [128, NT], BF16)); t_ad = TU("adT")
                    XV = se1.enter_context(SBT("XV", [128, NP], BF16)); t_XV = TU("XV")
                    XR = se1.enter_context(SBT("XR", [128, NP], F32)); t_XR = TU("XR")
                    XK = se1.enter_context(SBT("XK", [128, NP], F32)); t_XK = TU("XK")
                    KK = se1.enter_context(SBT("KK", [128, NP], F32)); t_KK = TU("KK")
                    LW = se1.enter_context(SBT("LW", [128, NP], F32)); t_LW = TU("LW")
                    CL = se1.enter_context(SBT("CL", [128, NP], F32)); t_CL = TU("CL")
                    AA = se1.enter_context(SBT("AA", [128, NP], F32)); t_AA = TU("AA")
                    KT = se1.enter_context(SBT("KT", [128, NP], F32)); t_KT = TU("KT")
                    KS = se1.enter_context(SBT("KS", [128, NP], F32)); t_KS = TU("KS")
                    EE = se1.enter_context(SBT("EE", [128, NP], F32)); t_EE = TU("EE")
                    E2 = se1.enter_context(SBT("E2", [128, NP], F32)); t_E2 = TU("E2")
                    TOT = se1.enter_context(SBT("TOT", [128, NC2 // 2], F32)); t_TOT = TU("TOT")
                    OUT = Ring(nc, se1, "rout", [128, NP], BF16, 3)
                    RN = Ring(nc, se1, "rn", [128, 512], F32, 2)
                    TRB = Ring(nc, se1, "trb", [128, 4, 128], BF16, 3)

                    def shift_lerp(j, p0, dst, t_dst, eng="pool"):
                        rngs = []
                        a = 0
                        while a < 128:
                            qd = (j * 128 + a) // 864
                            b = min(128, (qd + 1) * 864 - j * 128)
                            while a < b:
                                mx = {0: 128, 32: 32, 64: 64, 96: 32}[a]
                                e_ = min(b, a + mx)
                                rngs.append((a, e_, qd))
                                a = e_
                        for (a, b, qd) in rngs:
                            muc = mu[a:b, j:j + 1]
                            if p0 == 0:
                                off = -1 if qd < 2 else 1
                                lo, hi = (1, NCTX) if off < 0 else (0, NCTX - 1)
                                TS(eng, SH[a:b, lo:hi], Pt[a:b, lo + off:hi + off], muc, None, ALU.mult, None, [t_P, t_mu], [t_SH])
                                z = 0 if off < 0 else NCTX - 1
                                fw.op(eng, "memset", [], [t_SH], ap=SH[a:b, z:z + 1], constant=0.0)
                            off = (-1, 1, -64, 64)[qd]
                            lo = max(p0, NCTX); hi = p0 + NP
                            if off > 0:
                                hi = min(hi, NT - off)
                            ACT(SH[a:b, lo - p0:hi - p0], Pt[a:b, lo + off:hi + off], AF.Copy, [t_P, t_mu], [t_SH], scale=muc)
                            l0 = max(p0, NCTX) - p0
                            lat = SH[a:b, l0:NP].rearrange("p (r c) -> p r c", c=64)
                            if qd == 0:
                                fw.op(eng, "memset", [], [t_SH], ap=lat[:, :, 0:1], constant=0.0)
                            elif qd == 1:
                                fw.op(eng, "memset", [], [t_SH], ap=lat[:, :, 63:64], constant=0.0)
                            elif qd == 2 and p0 == 0:
                                fw.op(eng, "memset", [], [t_SH], ap=SH[a:b, l0:l0 + 64], constant=0.0)
                            elif qd == 3 and p0 + NP == NT:
                                fw.op(eng, "memset", [], [t_SH], ap=SH[a:b, NP - 64:NP], constant=0.0)
                        STT("dve", dst, Pt[:, p0:p0 + NP], a1[:, j:j + 1], SH[:], ALU.mult, ALU.add, [t_P, t_SH, t_mu], [t_dst])

                    def transpose_store(src, t_src, dram_ap_fn, tus_fn, p0):
                        for g in range(0, NP // 128, 4):
                            ng = min(4, NP // 128 - g)
                            ps, t_ps = PS.next()
                            for jj in range(ng):
                                MM(ps[:, jj * 128:(jj + 1) * 128], src[:, (g + jj) * 128:(g + jj + 1) * 128], ident_b, [t_src, t_cst], [t_ps])
                            tb, t_tb = TRB.next()
                            ACT(tb[:, 0:ng, :], ps[:, 0:ng * 128].rearrange("p (j c) -> p j c", c=128), AF.Copy, [t_ps], [t_tb])
                            for jj in range(ng):
                                t0 = p0 + (g + jj) * 128
                                fw.dma("sp", dram_ap_fn(t0), tb[:, jj, :], reads=[t_tb], writes=tus_fn(t0))

                    for j, (dstT, t_d, func) in ((24, (wdT, t_wd, AF.Tanh)), (25, (adT, t_ad, AF.Copy)), (26, (None, None, AF.Sigmoid))):
                        fw.dma("sp", Pt[:], S["pT"].ap[j * 128:(j + 1) * 128, :], reads=[x for r0 in range(3072, 3456, 128) for x in S["pT"].tu(key=r0)], writes=[t_P])
                        for p0 in (0, NP):
                            shift_lerp(j, p0, EE[:], t_EE)
                            if dstT is not None:
                                ACT(dstT[:, p0:p0 + NP], EE[:], func, [t_EE], [t_d])
                            else:
                                ob, t_ob = OUT.next()
                                ACT(ob[:], EE[:], func, [t_EE], [t_ob])
                                fw.dma("sp", S["gdT"].ap[:, p0:p0 + NP], ob[:], reads=[t_ob], writes=S["gdT"].tu(p0, NP))
                    for cb in range(8):
                        for p0 in (0, NP):
                            pi = p0 // NP
                            ntile = [(q0, min(512, NP - q0)) for q0 in range(0, NP, 512)]
                            fw.dma("sp", Pt[:], S["pT"].ap[(16 + cb) * 128:(17 + cb) * 128, :], reads=[x for r0 in range(0, 3456, 128) for x in S["pT"].tu(key=r0)], writes=[t_P])
                            shift_lerp(16 + cb, p0, XV[:], t_XV)
                            transpose_store(XV, t_XV, lambda t0, cb=cb: S["v_tm"].ap[t0:t0 + 128, cb * 128:(cb + 1) * 128], lambda t0, cb=cb: S["v_tm"].tu(t0, 128, key=cb), p0)
                            fw.dma("sp", Pt[:], S["pT"].ap[cb * 128:(cb + 1) * 128, :], reads=S["pT"].tu(key=0), writes=[t_P])
                            shift_lerp(cb, p0, XR[:], t_XR)
                            fw.dma("sp", Pt[:], S["pT"].ap[(8 + cb) * 128:(9 + cb) * 128, :], reads=S["pT"].tu(key=0), writes=[t_P])
                            shift_lerp(8 + cb, p0, XK[:], t_XK)
                            ACT(KK[:], XK[:], AF.Copy, [t_XK, t_kkc], [t_KK], scale=kk_c[:, cb:cb + 1])
                            ob, t_ob = OUT.next()
                            ACT(ob[:], KK[:], AF.Square, [t_KK], [t_ob])
                            for (q0, qn) in ntile:
                                ps, t_ps = PS.next()
                                MM(ps[:, :qn], blk1_b, ob[:, q0:q0 + qn], [t_ob, t_cst], [t_ps])
                                rn, t_rn = RN.next()
                                ACT(rn[:, :qn], ps[:, :qn], AF.Sqrt, [t_ps], [t_rn])
                                TS("dve", rn[:, :qn], rn[:, :qn], 1e-12, None, ALU.max, None, [t_rn], [t_rn])
                                fw.op("dve", "reciprocal", [t_rn], [t_rn], out=rn[:, :qn], in_=rn[:, :qn])
                                TT("dve", KK[:, q0:q0 + qn], KK[:, q0:q0 + qn], rn[:, :qn], ALU.mult, [t_KK, t_rn], [t_KK])
                            for d in range(2):
                                w0c, t_w0c = w0_c[d]; a0c, t_a0c = a0_c[d]
                                for (q0, qn) in ntile:
                                    ps, t_ps = PS.next()
                                    MM(ps[:, :qn], w2b[d * 64:(d + 1) * 64, cb * 128:(cb + 1) * 128], wdT[d * 64:(d + 1) * 64, p0 + q0:p0 + q0 + qn], [t_w2, t_wd], [t_ps])
                                    ACT(LW[:, q0:q0 + qn], ps[:, :qn], AF.Sigmoid, [t_ps, t_w0c], [t_LW], bias=w0c[:, cb:cb + 1])
                                    ps, t_ps = PS.next()
                                    MM(ps[:, :qn], a2b[d * 64:(d + 1) * 64, cb * 128:(cb + 1) * 128], adT[d * 64:(d + 1) * 64, p0 + q0:p0 + q0 + qn], [t_a2, t_ad], [t_ps])
                                    ACT(AA[:, q0:q0 + qn], ps[:, :qn], AF.Sigmoid, [t_ps, t_a0c], [t_AA], bias=a0c[:, cb:cb + 1])
                                fw.op("dve", "tensor_tensor_scan", [t_rm2, t_LW], [t_CL], out=CL[:], data0=rmask2[:], data1=LW[:], initial=0.0, op0=ALU.mult, op1=ALU.add)
                                cl3 = CL[:].rearrange("p (c l) -> p c l", l=128)
                                fw.op("dve", "tensor_copy", [t_CL], [t_TOT], out=TOT[:].unsqueeze(2), in_=cl3[:, :, 127:128])
                                totb = TOT[:].unsqueeze(2).to_broadcast([128, NC2 // 2, 128])
                                if d == 1:
                                    TT("dve", CL[:], LW[:], CL[:], ALU.subtract, [t_LW, t_CL], [t_CL])
                                    TT("dve", cl3, cl3, totb, ALU.add, [t_CL, t_TOT], [t_CL])
                                ACT(plb[:, d, cb, pi * (NC2 // 2):(pi + 1) * (NC2 // 2)], TOT[:], AF.Exp, [t_TOT], [t_plb], scale=-LAM)
                                ACT(EE[:], AA[:], AF.Identity, [t_AA, t_kac], [t_EE], scale=ka_c[:, cb:cb + 1], bias=nka_c[:, cb:cb + 1])
                                STT("dve", KT[:], EE[:], 1.0, XK[:], ALU.add, ALU.mult, [t_EE, t_XK], [t_KT])
                                if d == 0:
                                    ACT(KS[:], KT[:], AF.Copy, [t_KT], [t_KS])
                                else:
                                    TT("pool", KS[:], KS[:], KT[:], ALU.add, [t_KT, t_KS], [t_KS])
                                TT("dve", EE[:], CL[:], LW[:], ALU.subtract, [t_CL, t_LW], [t_EE])
                                ACT(EE[:], EE[:], AF.Exp, [t_EE], [t_EE], scale=-LAM)
                                ob, t_ob = OUT.next()
                                TT("dve", ob[:], KK[:], EE[:], ALU.mult, [t_KK, t_EE], [t_ob])
                                fw.dma("sp", S["abT"].ap[d, cb * 128:(cb + 1) * 128, p0:p0 + NP], ob[:], reads=[t_ob], writes=S["abT"].tu(p0, NP, key=(d, cb)))
                                ACT(EE[:], CL[:], AF.Exp, [t_CL], [t_EE], scale=-LAM)
                                ob, t_ob = OUT.next()
                                TT("dve", ob[:], XR[:], EE[:], ALU.mult, [t_XR, t_EE], [t_ob])
                                fw.dma("sp", S["rbT"].ap[d, cb * 128:(cb + 1) * 128, p0:p0 + NP], ob[:], reads=[t_ob], writes=S["rbT"].tu(p0, NP, key=(d, cb)))
                                TT("pool", LW[:], KK[:], AA[:], ALU.mult, [t_KK, t_AA], [t_LW])
                                ACT(EE[:], CL[:], AF.Exp, [t_CL], [t_EE], scale=LAM)
                                ob, t_ob = OUT.next()
                                TT("dve", ob[:], LW[:], EE[:], ALU.mult, [t_LW, t_EE], [t_ob])
                                fw.dma("sp", S["bbT"].ap[d, cb * 128:(cb + 1) * 128, p0:p0 + NP], ob[:], reads=[t_ob], writes=S["bbT"].tu(p0, NP, key=(d, cb)))
                                transpose_store(ob, t_ob, lambda t0, cb=cb, d=d: S["Bhat"].ap[d, t0:t0 + 128, cb * 128:(cb + 1) * 128], lambda t0, cb=cb, d=d: S["Bhat"].tu(t0, 128, key=(d, cb)), p0)
                                ob, t_ob = OUT.next()
                                TT("dve", ob[:], KT[:], EE[:], ALU.mult, [t_KT, t_EE], [t_ob])
                                fw.dma("sp", S["kbT"].ap[d, cb * 128:(cb + 1) * 128, p0:p0 + NP], ob[:], reads=[t_ob], writes=S["kbT"].tu(p0, NP, key=(d, cb)))
                                transpose_store(ob, t_ob, lambda t0, cb=cb, d=d: S["Khat"].ap[d, t0:t0 + 128, cb * 128:(cb + 1) * 128], lambda t0, cb=cb, d=d: S["Khat"].tu(t0, 128, key=(d, cb)), p0)
                            STT("dve", EE[:], XR[:], rk_c[:, cb:cb + 1], KS[:], ALU.mult, ALU.mult, [t_XR, t_KS, t_rkc], [t_EE])
                            ps, t_ps = PS.next()
                            for tt in range(NP // 128):
                                MM(ps[:, 2 * tt:2 * tt + 2], EE[:, tt * 128:(tt + 1) * 128], sel2_f, [t_EE, t_cst], [t_ps])
                            fw.op("dve", "tensor_copy", [t_ps], [t_bon], out=bon[:, pi * (NP // 128):(pi + 1) * (NP // 128), 2 * cb:2 * cb + 2], in_=ps[:, 0:2 * (NP // 128)].rearrange("p (t h) -> p t h", h=2))
                    fw.barrier()
                if stop == "E1":
                    for nm_, tl_, tt_ in (("plb", plb, t_plb), ("bon", bon, t_bon)):
                        if nm_ in dbg:
                            fw.dma("sp", dbg[nm_].ap, tl_[:].rearrange("p a b c -> p (a b c)") if nm_ == "plb" else tl_[:].rearrange("p a b -> p (a b)"), reads=[tt_], writes=dbg[nm_].tu())
                    break
                with ExitStack() as se2:
                    AB = Ring(nc, se2, "e2ab", [128, 8, 128], BF16, 2)
                    BB = Ring(nc, se2, "e2bb", [128, 8, 128], BF16, 2)
                    YP = Ring(nc, se2, "e2yp", [128, 2, 256], BF16, 12)
                    YT = Ring(nc, se2, "e2yt", [128, 2, 128], BF16, 12)
                    TO = Ring(nc, se2, "e2to", [128, 16, 128], BF16, 2)
                    bank = lambda b: (psall[:, b * 512:(b + 1) * 512], PS.tus[b])
                    evi2 = 0
                    for d in range(2):
                        mk_s = m_lt if d == 0 else m_gt
                        mk_t = m_gt if d == 0 else m_lt
                        for c in range(NC2):
                            t0 = c * 128
                            ab, t_ab = AB.next(); bb, t_bb = BB.next(); to, t_to = TO.next()
                            fw.dma("sp", ab[:], S["abT"].ap[d, :, t0:t0 + 128].rearrange("(cb p) t -> p cb t", p=128), reads=[x for cb_ in range(8) for x in S["abT"].tu(t0, 128, key=(d, cb_))], writes=[t_ab])
                            fw.dma("sp", bb[:], S["bbT"].ap[d, :, t0:t0 + 128].rearrange("(cb p) t -> p cb t", p=128), reads=[x for cb_ in range(8) for x in S["bbT"].tu(t0, 128, key=(d, cb_))], writes=[t_bb])
                            for g in range(2):
                                chains = []
                                for k in range(4):
                                    cb = 4 * g + k
                                    yp, t_yp = YP.next(); yt, t_yt = YT.next()
                                    ps, t_ps = PS.pair(2 * k)
                                    for hh in range(2):
                                        pb = hh * 64
                                        MM(ps[:, hh, 0:128], bb[pb:pb + 64, cb, :], ab[pb:pb + 64, cb, :], [t_ab, t_bb], [t_ps[hh]])
                                        MM(ps[:, hh, 128:256], ab[pb:pb + 64, cb, :], bb[pb:pb + 64, cb, :], [t_ab, t_bb], [t_ps[hh]])
                                    STT("dve", yp[:, :, 0:128], ps[:, :, 0:128], -1.0, mk_s.unsqueeze(1).to_broadcast([128, 2, 128]), ALU.mult, ALU.mult, t_ps + [t_cst], [t_yp])
                                    STT("dve", yt[:], ps[:, :, 128:256], -1.0, mk_t.unsqueeze(1).to_broadcast([128, 2, 128]), ALU.mult, ALU.mult, t_ps + [t_cst], [t_yt])
                                    chains.append([cb, yp, t_yp, yt, t_yt])
                                for lev in range(7):
                                    last = lev == 6
                                    nxt = []
                                    for k in range(4):
                                        cb, yp, t_yp, yt, t_yt = chains[k]
                                        pa, t_pa = bank(2 * k)
                                        pbb, t_pbb = bank(2 * k + 1)
                                        for hh in range(2):
                                            if lev == 0:
                                                MM(pa[:, hh * 256:hh * 256 + 128], yt[:, hh, :], yp[:, hh, 0:128], [t_yt, t_yp], [t_pa])
                                                MM(pa[:, hh * 256 + 128:(hh + 1) * 256], yt[:, hh, :], ident_b, [t_yt, t_cst], [t_pa])
                                                MM(pbb[:, hh * 128:(hh + 1) * 128], yp[:, hh, 0:128], yt[:, hh, :], [t_yt, t_yp], [t_pbb])
                                            elif not last:
                                                MM(pa[:, hh * 256:(hh + 1) * 256], yt[:, hh, :], yp[:, hh, :], [t_yt, t_yp], [t_pa])
                                                MM(pbb[:, hh * 128:(hh + 1) * 128], yp[:, hh, 0:128], yt[:, hh, :], [t_yt, t_yp], [t_pbb])
                                            else:
                                                MM(pa[:, hh * 256 + 128:(hh + 1) * 256], yt[:, hh, :], yp[:, hh, 128:256], [t_yt, t_yp], [t_pa])
                                    for k in range(4):
                                        cb, yp, t_yp, yt, t_yt = chains[k]
                                        pa, t_pa = bank(2 * k)
                                        pbb, t_pbb = bank(2 * k + 1)
                                        pav = pa.rearrange("p (h x) -> p h x", x=256)
                                        if not last:
                                            ypn, t_ypn = YP.next(); ytn, t_ytn = YT.next()
                                            ACT(ypn[:, :, 0:128], pav[:, :, 0:128], AF.Copy, [t_pa], [t_ypn])
                                            if lev == 0:
                                                TT("dve", ypn[:, :, 128:256], pav[:, :, 128:256], ident_b.unsqueeze(1).to_broadcast([128, 2, 128]), ALU.add, [t_pa, t_cst], [t_ypn])
                                            else:
                                                TT("dve", ypn[:, :, 128:256], pav[:, :, 128:256], yp[:, :, 128:256], ALU.add, [t_pa, t_yp], [t_ypn])
                                            evi2 += 1
                                            if evi2 % 2 == 0:
                                                ACT(ytn[:], pbb[:, 0:256].rearrange("p (h x) -> p h x", x=128), AF.Copy, [t_pbb], [t_ytn])
                                            else:
                                                fw.op("dve", "tensor_copy", [t_pbb], [t_ytn], out=ytn[:], in_=pbb[:, 0:256].rearrange("p (h x) -> p h x", x=128))
                                            chains[k] = [cb, ypn, t_ypn, ytn, t_ytn]
                                        else:
                                            TT("dve", to[:, 2 * cb:2 * cb + 2, :], pav[:, :, 128:256], yp[:, :, 128:256], ALU.add, [t_pa, t_yp], [t_to])
                            fw.dma("pool", S["Tinv"].ap[d, c], to[:], reads=[t_to], writes=S["Tinv"].tu(t0, 128, key=d))
                    fw.barrier()
                if stop == "E2":
                    break
                with ExitStack() as se3:
                    AR = Ring(nc, se3, "e3ar", [128, 8, 2, 128], BF16, 3)
                    BB3 = Ring(nc, se3, "e3bb", [128, 8, 128], BF16, 3)
                    KB3 = Ring(nc, se3, "e3kb", [128, 8, 128], BF16, 3)
                    V3 = Ring(nc, se3, "e3v", [128, D], BF16, 3)
                    KH3 = Ring(nc, se3, "e3kh", [128, D], BF16, 3)
                    BH3 = Ring(nc, se3, "e3bh", [128, D], BF16, 3)
                    TI3 = Ring(nc, se3, "e3ti", [128, 16, 128], BF16, 3)
                    AM = Ring(nc, se3, "e3am", [128, 384], BF16, 8)
                    WB3 = Ring(nc, se3, "e3w", [128, 64], BF16, 8)
                    UN3 = Ring(nc, se3, "e3u", [128, 64], BF16, 8)
                    YO3 = Ring(nc, se3, "e3yo", [128, D], F32, 3)
                    orders2 = [list(range(NC2)), [1, 0] + list(range(NC2 - 1, 1, -1))]
                    bank = lambda b: (psall[:, b * 512:(b + 1) * 512], PS.tus[b])
                    st3 = []
                    for d in range(2):
                        S32 = se3.enter_context(SBT(f"S32_{d}", [128, 8, 64], F32))
                        Sb = se3.enter_context(SBT(f"Sb_{d}", [128, 8, 64], BF16))
                        t_S = [TU(f"S32_{cb}") for cb in range(8)]
                        t_Sb = [TU(f"Sb_{cb}") for cb in range(8)]
                        fw.op("pool", "memset", [], t_S, ap=S32[:], constant=0.0)
                        fw.op("pool", "memset", [], t_Sb, ap=Sb[:], constant=0.0)
                        st3.append((S32, Sb, t_S, t_Sb))
                    psa_i = 0
                    STMP = [(se3.enter_context(SBT(f"stmp{d_}", [128, 8, 64], F32)), TU(f"stmp{d_}")) for d_ in range(2)]
                    for i in range(NC2):
                        cx = []
                        for d in range(2):
                            c = orders2[d][i]
                            t0 = c * 128
                            ar, t_ar = AR.next(); bb, t_bb = BB3.next(); kb, t_kb = KB3.next(); vv, t_vv = V3.next(); kh, t_kh = KH3.next(); bh, t_bh = BH3.next(); ti, t_ti = TI3.next()
                            rd = lambda nm: [x for cb_ in range(8) for x in S[nm].tu(t0, 128, key=(d, cb_))]
                            fw.dma("sp", ar[:, :, 0, :], S["abT"].ap[d, :, t0:t0 + 128].rearrange("(cb p) t -> p cb t", p=128), reads=rd("abT"), writes=[t_ar])
                            fw.dma("sp", ar[:, :, 1, :], S["rbT"].ap[d, :, t0:t0 + 128].rearrange("(cb p) t -> p cb t", p=128), reads=rd("rbT"), writes=[t_ar])
                            fw.dma("sp", bb[:], S["bbT"].ap[d, :, t0:t0 + 128].rearrange("(cb p) t -> p cb t", p=128), reads=rd("bbT"), writes=[t_bb])
                            fw.dma("sp", kb[:], S["kbT"].ap[d, :, t0:t0 + 128].rearrange("(cb p) t -> p cb t", p=128), reads=rd("kbT"), writes=[t_kb])
                            fw.dma("sp", vv[:], S["v_tm"].ap[t0:t0 + 128, :], reads=[x for cb_ in range(8) for x in S["v_tm"].tu(t0, 128, key=cb_)], writes=[t_vv])
                            fw.dma("sp", kh[:], S["Khat"].ap[d, t0:t0 + 128, :], reads=rd("Khat"), writes=[t_kh])
                            fw.dma("sp", bh[:], S["Bhat"].ap[d, t0:t0 + 128, :], reads=rd("Bhat"), writes=[t_bh])
                            fw.dma("sp", ti[:], S["Tinv"].ap[d, c], reads=S["Tinv"].tu(t0, 128, key=d), writes=[t_ti])
                            yo, t_yo = YO3.next()
                            stmp, t_stmp = STMP[d]
                            S32_, _, t_S_, _ = st3[d]
                            fw.op("pool", "tensor_tensor", t_S_ + [t_plb], [t_stmp], out=stmp[:], in0=S32_[:], in1=plb[:, d, :, c:c + 1].to_broadcast([128, 8, 64]), op=ALU.mult)
                            cx.append(dict(d=d, c=c, t0=t0, stmp=stmp, t_stmp=t_stmp, ar=ar, t_ar=t_ar, bb=bb, t_bb=t_bb, kb=kb, t_kb=t_kb, vv=vv, t_vv=t_vv, kh=kh, t_kh=t_kh, bh=bh, t_bh=t_bh, ti=ti, t_ti=t_ti, yo=yo, t_yo=t_yo,
                                           mk_st=(m_lt if d == 0 else m_gt), mk_in=(m_le if d == 0 else m_ge), ams={}))

                        def front(x, hd_):
                            nonlocal_i = [0]
                            cb_, hh_ = hd_ // 2, hd_ % 2
                            pb_ = hh_ * 64
                            psa, t_psa = bank(6 + (front.k % 2))
                            front.k += 1
                            MM(psa[:, 0:256], x["kb"][pb_:pb_ + 64, cb_, :], x["ar"][pb_:pb_ + 64, cb_, :, :].rearrange("p a t -> p (a t)"), [x["t_kb"], x["t_ar"]], [t_psa])
                            MM(psa[:, 256:384], x["bb"][pb_:pb_ + 64, cb_, :], x["ar"][pb_:pb_ + 64, cb_, 1, :], [x["t_bb"], x["t_ar"]], [t_psa])
                            am_, t_am_ = AM.next()
                            TT("dve", am_[:, 0:128], psa[:, 0:128], x["mk_st"], ALU.mult, [t_psa, t_cst], [t_am_])
                            TT("dve", am_[:, 128:384].rearrange("p (a t) -> p a t", t=128), psa[:, 128:384].rearrange("p (a t) -> p a t", t=128), x["mk_in"].unsqueeze(1).to_broadcast([128, 2, 128]), ALU.mult, [t_psa, t_cst], [t_am_])
                            x["ams"][hd_] = (am_, t_am_)
                        front.k = 0
                        for x in cx:
                            front(x, 0)
                        for cb in range(8):
                            for hh in range(2):
                                hd = 2 * cb + hh
                                pb = hh * 64
                                hs = slice(hd * 64, (hd + 1) * 64)
                                if hd + 1 < 16:
                                    for x in cx:
                                        front(x, hd + 1)
                                loc = []
                                for x in cx:
                                    d = x["d"]
                                    S32, Sb, t_S, t_Sb = st3[d]
                                    am, t_am = x["ams"][hd]
                                    psw, t_psw = bank(2 * d + (hd % 2))
                                    MM(psw[:, 0:64], am[:, 0:128], x["vv"][:, hs], [t_am, x["t_vv"]], [t_psw], start=True, stop=False)
                                    MM(psw[:, 0:64], x["ar"][pb:pb + 64, cb, 0, :], Sb[pb:pb + 64, cb, :], [x["t_ar"], t_Sb[cb]], [t_psw], start=False, stop=True)
                                    loc.append((x, d, S32, Sb, t_S, t_Sb, am, t_am, psw, t_psw))
                                wbs = []
                                for (x, d, S32, Sb, t_S, t_Sb, am, t_am, psw, t_psw) in loc:
                                    wb_, t_wb_ = WB3.next()
                                    ACT(wb_[:], psw[:, 0:64], AF.Copy, [t_psw], [t_wb_])
                                    wbs.append((wb_, t_wb_))
                                for k_, (x, d, S32, Sb, t_S, t_Sb, am, t_am, psw, t_psw) in enumerate(loc):
                                    MM(psw[:, 64:128], x["ti"][:, hd, :], wbs[k_][0][:], [x["t_ti"], wbs[k_][1]], [t_psw])
                                uns = []
                                for (x, d, S32, Sb, t_S, t_Sb, am, t_am, psw, t_psw) in loc:
                                    un, t_un = UN3.next()
                                    ACT(un[:], psw[:, 64:128], AF.Copy, [t_psw], [t_un], scale=-1.0)
                                    uns.append((un, t_un))
                                for k_, (x, d, S32, Sb, t_S, t_Sb, am, t_am, psw, t_psw) in enumerate(loc):
                                    un, t_un = uns[k_]
                                    psS, t_psS = bank(4 + d)
                                    MM(psw[:, 128:192], am[:, 128:256], x["vv"][:, hs], [t_am, x["t_vv"]], [t_psw], start=True, stop=False)
                                    MM(psw[:, 128:192], am[:, 256:384], un[:], [t_am, t_un], [t_psw], start=False, stop=False)
                                    MM(psw[:, 128:192], x["ar"][pb:pb + 64, cb, 1, :], Sb[pb:pb + 64, cb, :], [x["t_ar"], t_Sb[cb]], [t_psw], start=False, stop=True)
                                    MM(psS[pb:pb + 64, 0:64], x["kh"][:, hs], x["vv"][:, hs], [x["t_kh"], x["t_vv"]], [t_psS], start=True, stop=False)
                                    MM(psS[pb:pb + 64, 0:64], x["bh"][:, hs], un[:], [x["t_bh"], t_un], [t_psS], start=False, stop=True)
                                for (x, d, S32, Sb, t_S, t_Sb, am, t_am, psw, t_psw) in loc:
                                    ACT(x["yo"][:, hs], psw[:, 128:192], AF.Copy, [t_psw], [x["t_yo"]])
                                if hh == 1:
                                    for (x, d, S32, Sb, t_S, t_Sb, am, t_am, psw, t_psw) in loc:
                                        psS, t_psS = bank(4 + d)
                                        c = x["c"]
                                        STT("dve", S32[:, cb, :], psS[:, 0:64], plb[:, d, cb, c:c + 1], x["stmp"][:, cb, :], ALU.mult, ALU.add, [t_S[cb], t_psS, t_plb, x["t_stmp"]], [t_S[cb]])
                                        ACT(Sb[:, cb, :], S32[:, cb, :], AF.Copy, [t_S[cb]], [t_Sb[cb]])
                        for x in cx:
                            fw.dma("pool", S["yr"].ap[x["d"], x["t0"]:x["t0"] + 128, :], x["yo"][:], reads=[x["t_yo"]], writes=S["yr"].tu(x["t0"], 128, key=x["d"]))
                    fw.barrier()
                if stop == "E3":
                    break
                with ExitStack() as se4:
                    def bc_load(name, src1d):
                        t = se4.enter_context(SBT(name, [128, D], F32))
                        tu_ = TU(name)
                        fw.dma("sp", t[:], src1d.partition_broadcast(128), writes=[tu_])
                        return t, tu_
                    lnw, t_lnw = bc_load("lnw_bc", I["ln_w"].ap[l])
                    lnb, t_lnb = bc_load("lnb_bc", I["ln_b"].ap[l])
                    g2b = se4.enter_context(SBT("g2b", [128, D], BF16)); t_g2 = TU("g2b")
                    with ExitStack() as se40:
                        g2f = se40.enter_context(SBT("g2f", [128, D], F32))
                        fw.dma("sp", g2f[:], I["g2"].ap[l], writes=[t_g2])
                        fw.op("dve", "tensor_copy", [t_g2], [t_g2], out=g2b[:], in_=g2f[:])
                        fw.barrier()
                    YF = Ring(nc, se4, "yf", [128, D], F32, 2)
                    YB = Ring(nc, se4, "yb", [128, D], F32, 2)
                    VT = Ring(nc, se4, "vt4", [128, D], BF16, 2)
                    GD = Ring(nc, se4, "gd4", [128, 128], BF16, 2)
                    SQ4 = Ring(nc, se4, "sq4", [128, D], F32, 1)
                    ST4 = Ring(nc, se4, "st4", [128, 2, 16], F32, 2)
                    RO = Ring(nc, se4, "ro4", [128, D], BF16, 2)
                    RT = Ring(nc, se4, "rt4", [128, 8, 128], BF16, 2)
                    for tt in range(NC2):
                        t0 = tt * 128
                        yf, t_yf = YF.next(); yb, t_yb = YB.next(); vt, t_vt = VT.next(); gd, t_gd = GD.next(); sq, t_sq = SQ4.next(); st4, t_st = ST4.next(); ro, t_ro = RO.next(); rt, t_rt = RT.next()
                        fw.dma("sp", yf[:], S["yr"].ap[0, t0:t0 + 128, :], reads=S["yr"].tu(t0, 128, key=0), writes=[t_yf])
                        fw.dma("sp", yb[:], S["yr"].ap[1, t0:t0 + 128, :], reads=S["yr"].tu(t0, 128, key=1), writes=[t_yb])
                        fw.dma("sp", vt[:], S["v_tm"].ap[t0:t0 + 128, :], reads=[x for cb_ in range(8) for x in S["v_tm"].tu(t0, 128, key=cb_)], writes=[t_vt])
                        fw.dma("sp", gd[:], S["gdT"].ap[:, t0:t0 + 128], reads=S["gdT"].tu(t0, 128), writes=[t_gd])
                        TT("dve", yf[:], yf[:], yb[:], ALU.add, [t_yf, t_yb], [t_yf])
                        y3 = yf[:].rearrange("p (h v) -> p h v", v=64)
                        fw.op("dve", "tensor_reduce", [t_yf], [t_st], out=st4[:, 0, :], in_=y3, axis=AX.X, op=ALU.add)
                        TS("dve", st4[:, 0, :], st4[:, 0, :], 1.0 / 64, None, ALU.mult, None, [t_st], [t_st])
                        TT("dve", y3, y3, st4[:, 0, :].unsqueeze(2).to_broadcast([128, 16, 64]), ALU.subtract, [t_yf, t_st], [t_yf])
                        ACT(sq[:], yf[:], AF.Square, [t_yf], [t_sq])
                        fw.op("dve", "tensor_reduce", [t_sq], [t_st], out=st4[:, 1, :], in_=sq[:].rearrange("p (h v) -> p h v", v=64), axis=AX.X, op=ALU.add)
                        ACT(st4[:, 1, :], st4[:, 1, :], AF.Sqrt, [t_st], [t_st], scale=1.0 / 64, bias=64e-5)
                        fw.op("dve", "reciprocal", [t_st], [t_st], out=st4[:, 1, :], in_=st4[:, 1, :])
                        TT("dve", y3, y3, st4[:, 1, :].unsqueeze(2).to_broadcast([128, 16, 64]), ALU.mult, [t_yf, t_st], [t_yf])
                        TT("pool", yf[:], yf[:], lnw[:], ALU.mult, [t_yf, t_lnw], [t_yf])
                        TT("pool", yf[:], yf[:], lnb[:], ALU.add, [t_yf, t_lnb], [t_yf])
                        TT("dve", sq[:].rearrange("p (h v) -> p h v", v=64), vt[:].rearrange("p (h v) -> p h v", v=64), bon[:, tt, :].unsqueeze(2).to_broadcast([128, 16, 64]), ALU.mult, [t_vt, t_bon], [t_sq])
                        TT("dve", yf[:], yf[:], sq[:], ALU.add, [t_yf, t_sq], [t_yf])
                        pg, t_pg = PS.next2()
                        for hf_ in range(2):
                            MM(pg[:, hf_, :], gd[:], g2b[:, hf_ * 512:(hf_ + 1) * 512], [t_gd, t_g2], [t_pg[hf_]])
                        TT("dve", ro[:].rearrange("p (a x) -> p a x", x=512), yf[:].rearrange("p (a x) -> p a x", x=512), pg, ALU.mult, [t_yf] + t_pg, [t_ro])
                        if "rotm" in dbg:
                            fw.dma("pool", dbg["rotm"].ap[t0:t0 + 128, :], ro[:], reads=[t_ro], writes=dbg["rotm"].tu(t0, 128))
                        for hb2 in range(2):
                            ps, t_ps = PS.next()
                            for j in range(4):
                                cb = hb2 * 4 + j
                                MM(ps[:, j * 128:(j + 1) * 128], ro[:, cb * 128:(cb + 1) * 128], ident_b, [t_ro, t_cst], [t_ps])
                            ACT(rt[:, hb2 * 4:(hb2 + 1) * 4, :], ps[:].rearrange("p (c t) -> p c t", t=128), AF.Copy, [t_ps], [t_rt])
                        fw.dma("pool", S["roT"].ap[:, t0:t0 + 128].rearrange("(c p) t -> p c t", p=128), rt[:], reads=[t_rt], writes=S["roT"].tu(t0, 128))
                    fw.barrier()
            if stop == "E4":
                break
            with ExitStack() as sf:
                wts = {}
                for nm_ in ("w_proj_mlstm", "w_proj_rwkv", "w_out"):
                    wts[nm_] = (sf.enter_context(SBT("f_" + nm_, [128, 8, D], BF16)), TU(nm_))
                with ExitStack() as sf0:
                    WST = Ring(nc, sf0, "fwst", [128, 8, 512], F32, 2)
                    for nm_ in ("w_proj_mlstm", "w_proj_rwkv", "w_out"):
                        for hf_ in range(2):
                            wst, t_wst = WST.next()
                            fw.dma("sp", wst[:], I[nm_].ap[l, :, hf_ * 512:(hf_ + 1) * 512].rearrange("(c p) m -> p c m", p=128), writes=[t_wst])
                            if hf_ == 0:
                                fw.op("dve", "tensor_copy", [t_wst], [wts[nm_][1]], out=wts[nm_][0][:, :, hf_ * 512:(hf_ + 1) * 512], in_=wst[:])
                            else:
                                ACT(wts[nm_][0][:, :, hf_ * 512:(hf_ + 1) * 512], wst[:], AF.Copy, [t_wst], [wts[nm_][1]])
                    fw.barrier()
                MO_ = Ring(nc, sf, "f_mo", [128, 8, 512], BF16, 2)
                RO_ = Ring(nc, sf, "f_ro", [128, 8, 512], BF16, 2)
                GM_ = Ring(nc, sf, "f_gm", [128, 8, 512], BF16, 1)
                GR_ = Ring(nc, sf, "f_gr", [128, 8, 512], BF16, 1)
                ZT_ = Ring(nc, sf, "f_zt", [128, 512], F32, 2)
                ZB_ = Ring(nc, sf, "f_zb", [128, 8, 512], BF16, 1)
                XT_ = Ring(nc, sf, "f_xt", [128, 8, 512], F32, 1)
                for ti, (t0, n) in enumerate(TILES):
                    if l == DEPTH - 1 and t0 < NCTX:
                        continue
                    j = 1 if t0 < NCTX else 0
                    mo_, t_mo_ = MO_.next(); ro_, t_ro_ = RO_.next(); gm_, t_gm_ = GM_.next(); gr_, t_gr_ = GR_.next(); zb_, t_zb_ = ZB_.next(); xt_, t_xt_ = XT_.next()
                    fw.dma("sp", mo_[:, :, :n], S["moT"].ap[:, t0:t0 + n].rearrange("(c p) t -> p c t", p=128), reads=S["moT"].tu(t0, n), writes=[t_mo_])
                    fw.dma("sp", ro_[:, :, :n], S["roT"].ap[:, t0:t0 + n].rearrange("(c p) t -> p c t", p=128), reads=S["roT"].tu(t0, n), writes=[t_ro_])
                    fw.dma("sp", gm_[:, :, :n], S["mgT"].ap[0:D, t0:t0 + n].rearrange("(c p) t -> p c t", p=128), reads=[x for r0 in range(0, 2048, 128) for x in S["mgT"].tu(t0, n, key=r0)], writes=[t_gm_])
                    fw.dma("sp", gr_[:, :, :n], S["mgT"].ap[D:2 * D, t0:t0 + n].rearrange("(c p) t -> p c t", p=128), reads=[x for r0 in range(0, 2048, 128) for x in S["mgT"].tu(t0, n, key=r0)], writes=[t_gr_])
                    fw.dma("sp", xt_[:, :, :n], xsrc.ap[:, t0:t0 + n].rearrange("(c p) t -> p c t", p=128), reads=xsrc.tu(t0, n), writes=[t_xt_])
                    for m in range(8):
                        pm, t_pm = PS.next(); pr, t_pr = PS.next()
                        for c in range(8):
                            MM(pm[:, :n], wts["w_proj_mlstm"][0][:, c, m * 128:(m + 1) * 128], mo_[:, c, :n], [wts["w_proj_mlstm"][1], t_mo_], [t_pm], start=(c == 0), stop=(c == 7))
                        for c in range(8):
                            MM(pr[:, :n], wts["w_proj_rwkv"][0][:, c, m * 128:(m + 1) * 128], ro_[:, c, :n], [wts["w_proj_rwkv"][1], t_ro_], [t_pr], start=(c == 0), stop=(c == 7))
                        zt_, t_zt_ = ZT_.next()
                        TT("dve", zt_[:, :n], pm[:, :n], gm_[:, m, :n], ALU.mult, [t_pm, t_gm_], [t_zt_])
                        TT("dve", zb_[:, m, :n], pr[:, :n], gr_[:, m, :n], ALU.mult, [t_pr, t_gr_], [t_zb_])
                        TT("pool", zb_[:, m, :n], zb_[:, m, :n], zt_[:, :n], ALU.add, [t_zb_, t_zt_], [t_zb_])
                    for m in range(8):
                        py_, t_py_ = PS.next()
                        for c in range(8):
                            MM(py_[:, :n], wts["w_out"][0][:, c, m * 128:(m + 1) * 128], zb_[:, c, :n], [wts["w_out"][1], t_zb_], [t_py_], start=(c == 0), stop=(c == 7))
                        STT("dve", xt_[:, m, :n], py_[:, :n], prm[:, l, 2, m, j:j + 1], xt_[:, m, :n], ALU.mult, ALU.add, [t_py_, t_xt_, t_prm], [t_xt_])
                    fw.dma("pool", S["xres"].ap[:, t0:t0 + n].rearrange("(c p) t -> p c t", p=128), xt_[:, :, :n], reads=[t_xt_], writes=S["xres"].tu(t0, n))
                fw.barrier()
            if stop == "F":
                break
            moe = (l % 2 == 1)
            with ExitStack() as sg:
                work = {"x": Ring(nc, sg, "gx", [128, 8, 512], F32, 1 if moe else 2), "sq": Ring(nc, sg, "gsq", [128, 8, 512], BF16, 1), "rs": Ring(nc, sg, "grs", [128, 512], F32, 2)}
                FP = 256
                H2 = Ring(nc, sg, "gh2", [128, 8, 512], BF16, 1)
                YA = Ring(nc, sg, "gya", [128, 8, 512], F32, 1)
                WGS = Ring(nc, sg, "gwgs", [128, 8, FP], F32, 3)
                WG = Ring(nc, sg, "gwg", [128, 8, FP], BF16, 2)
                WU = Ring(nc, sg, "gwu", [128, 8, FP], BF16, 2)
                WD = Ring(nc, sg, "gwd", [128, FP // 128, D], BF16, 2)
                ACTB = Ring(nc, sg, "gact", [128, FP // 128, 512], BF16, 2)
                SIL = Ring(nc, sg, "gsil", [128, 512], F32, 3)
                XB = Ring(nc, sg, "gxb", [128, 8, 512], F32, 1)
                if moe:
                    H32 = Ring(nc, sg, "gh32", [128, 8, 512], F32, 1)
                    wr = sg.enter_context(SBT("wr_sb", [128, 8, NEXP], F32)); t_wr = TU("wr")
                    fw.dma("sp", wr[:], I["w_router"].ap[0].rearrange("(c p) e -> p c e", p=128), writes=[t_wr])
                    SEL = sg.enter_context(SBT("SEL", [8, NEXP, 128], F32)); t_SEL = TU("SEL")
                    fw.op("dve", "tensor_copy", [t_cst], [t_SEL], out=SEL[:], in_=ident_f[0:8, 0:8].unsqueeze(2).to_broadcast([8, NEXP, 128]))
                    GTM = Ring(nc, sg, "gtm", [128, 4, 24], F32, 2)
                    GTT = Ring(nc, sg, "gtt", [8, 512], F32, 2)
                    GBC = Ring(nc, sg, "gbc", [128, 512], F32, 2)
                    gfin = sg.enter_context(SBT("gfin", [128, 8], F32)); t_gfin = TU("gfin")
                    fw.dma("sp", gfin[:], I["g_final"].ap.rearrange("(c p) -> p c", p=128), writes=[t_gfin], allow_slow_non_contiguous=True)
                if moe:
                    tiles_g = [(256 + 512 * i, 512, i) for i in range(4)]
                else:
                    tiles_g = [(t0, n, None) for (t0, n) in TILES]
                dff = D_FFE if moe else D_FF
                for (t0, n, hi) in tiles_g:
                    h2, t_h2 = H2.next(); ya, t_ya = YA.next()
                    xkeep = {}
                    if moe:
                        xb, t_xb = XB.next()
                        t1 = t0 + 2048

                        def loader(xt, t_xt, t0=t0, t1=t1, n=n, xb=xb, t_xb=t_xb):
                            fw.dma("sp", xt[:, :, :n], S["xres"].ap[:, t0:t0 + n].rearrange("(c p) t -> p c t", p=128), reads=S["xres"].tu(t0, n), writes=[t_xt])
                            fw.dma("sp", xb[:, :, :n], S["xres"].ap[:, t1:t1 + n].rearrange("(c p) t -> p c t", p=128), reads=S["xres"].tu(t1, n), writes=[t_xb])
                            TS("pool", xt[:, :, :n], xt[:, :, :n], sel_sb[:, 0:1], None, ALU.mult, None, [t_xt, t_cst], [t_xt])
                            STT("dve", xt[:, :, :n], xb[:, :, :n], sel_sb[:, 1:2], xt[:, :, :n], ALU.mult, ALU.add, [t_xb, t_xt, t_cst], [t_xt])
                            fw.op("pool", "tensor_copy", [t_xt], [t_xb], out=xb[:, :, :n], in_=xt[:, :, :n])
                        h32, t_h32 = H32.next()
                        norm_mod(sg, loader, None, t0, n, l, 1, h2[:, :, :n], [t_h2], work, h32=(h32, t_h32))
                    else:
                        norm_mod(sg, S["xres"].ap, S["xres"].tu(t0, n), t0, n, l, 1, h2[:, :, :n], [t_h2], work)
                    fw.op("pool", "memset", [], [t_ya], ap=ya[:, :, :n], constant=0.0)
                    gbcs = [None] * NEXP
                    if moe:
                        gtm, t_gtm = GTM.next(); gtt, t_gtt = GTT.next()
                        pl_, t_pl = PS.next()
                        for sub in range(4):
                            for c in range(8):
                                MM(pl_[:, sub * 8:sub * 8 + 8], h32[:, c, sub * 128:(sub + 1) * 128], wr[:, c, :], [t_h32, t_wr], [t_pl], start=(c == 0), stop=(c == 7))
                        lg = gtm[:, :, 0:8]; eq1 = gtm[:, :, 8:16]; eq2 = gtm[:, :, 16:24]
                        fw.op("dve", "tensor_copy", [t_pl], [t_gtm], out=lg, in_=pl_[:, 0:32].rearrange("p (s e) -> p s e", e=8))
                        mx, t_mx = SIL.next()
                        m1 = mx[:, 0:4]; m2 = mx[:, 4:8]; p1 = mx[:, 8:12]; p2 = mx[:, 12:16]
                        fw.op("dve", "tensor_reduce", [t_gtm], [t_mx], out=m1, in_=lg, axis=AX.X, op=ALU.max)
                        TT("dve", eq1, lg, m1.unsqueeze(2).to_broadcast([128, 4, 8]), ALU.is_equal, [t_gtm, t_mx], [t_gtm])
                        STT("dve", eq2, eq1, -1e30, lg, ALU.mult, ALU.add, [t_gtm], [t_gtm])
                        fw.op("dve", "tensor_reduce", [t_gtm], [t_mx], out=m2, in_=eq2, axis=AX.X, op=ALU.max)
                        TT("dve", eq2, eq2, m2.unsqueeze(2).to_broadcast([128, 4, 8]), ALU.is_equal, [t_gtm, t_mx], [t_gtm])
                        TT("dve", p1, m2, m1, ALU.subtract, [t_mx], [t_mx])
                        ACT(p1, p1, AF.Exp, [t_mx], [t_mx])
                        TS("dve", p1, p1, 1.0, None, ALU.add, None, [t_mx], [t_mx])
                        fw.op("dve", "reciprocal", [t_mx], [t_mx], out=p1, in_=p1)
                        TS("dve", p2, p1, -1.0, 1.0, ALU.mult, ALU.add, [t_mx], [t_mx])
                        TT("dve", eq1, eq1, p1.unsqueeze(2).to_broadcast([128, 4, 8]), ALU.mult, [t_gtm, t_mx], [t_gtm])
                        TT("dve", eq2, eq2, p2.unsqueeze(2).to_broadcast([128, 4, 8]), ALU.mult, [t_gtm, t_mx], [t_gtm])
                        TT("dve", eq1, eq1, eq2, ALU.add, [t_gtm], [t_gtm])
                        pt_, t_pt = PS.next()
                        for sub in range(4):
                            MM(pt_[0:8, sub * 128:(sub + 1) * 128], gtm[:, sub, 8:16], ident_f, [t_gtm, t_cst], [t_pt])
                        fw.op("dve", "tensor_copy", [t_pt], [t_gtt], out=gtt[:], in_=pt_[0:8, :])
                        if "gates" in dbg:
                            fw.dma("pool", dbg["gates"].ap[:, hi * 512:(hi + 1) * 512], gtt[:], reads=[t_gtt], writes=dbg["gates"].tu(hi * 512, 512))
                    for ex in range(NEXP if moe else 1):
                        if moe:
                            gbc, t_gbc = GBC.next()
                            pb_, t_pb = PS.next()
                            MM(pb_[:, :n], SEL[:, ex, :], gtt[:, :n], [t_SEL, t_gtt], [t_pb])
                            fw.op("dve", "tensor_copy", [t_pb], [t_gbc], out=gbc[:, :n], in_=pb_[:, :n])
                            wg_ap = I["w_exp_gate"].ap[0, ex]; wu_ap = I["w_exp_up"].ap[0, ex]; wd_ap = I["w_exp_down"].ap[0, ex]
                        else:
                            wg_ap = I["w_ff_gate"].ap[0]; wu_ap = I["w_ff_up"].ap[0]; wd_ap = I["w_ff_down"].ap[0]
                        for f0 in range(0, dff, FP):
                            fwid = min(FP, dff - f0)
                            nj = fwid // 128
                            wg, t_wg = WG.next(); wu, t_wu = WU.next(); wd, t_wd = WD.next()
                            st1, t_st1 = WGS.next()
                            fw.dma("sp", st1[:, :, :fwid], wg_ap[:, f0:f0 + fwid].rearrange("(c p) m -> p c m", p=128), writes=[t_st1])
                            fw.op("dve", "tensor_copy", [t_st1], [t_wg], out=wg[:, :, :fwid], in_=st1[:, :, :fwid])
                            st2, t_st2 = WGS.next()
                            fw.dma("sp", st2[:, :, :fwid], wu_ap[:, f0:f0 + fwid].rearrange("(c p) m -> p c m", p=128), writes=[t_st2])
                            fw.op("act", "activation", [t_st2], [t_wu], out=wu[:, :, :fwid], in_=st2[:, :, :fwid], func=AF.Copy)
                            st3, t_st3 = WGS.next()
                            st3v = st3[:].rearrange("p c m -> p (c m)").rearrange("p (j m) -> p j m", m=D)
                            fw.dma("sp", st3v[:, :nj, :], wd_ap[f0:f0 + fwid, :].rearrange("(j p) m -> p j m", p=128), writes=[t_st3])
                            ACT(wd[:, :nj, :], st3v[:, :nj, :], AF.Copy, [t_st3], [t_wd])
                            ab_, t_ab_ = ACTB.next()
                            for jb in range(nj):
                                pg_, t_pg_ = PS.next(); pu_, t_pu_ = PS.next()
                                for c in range(8):
                                    MM(pg_[:, :n], wg[:, c, jb * 128:(jb + 1) * 128], h2[:, c, :n], [t_wg, t_h2], [t_pg_], start=(c == 0), stop=(c == 7))
                                for c in range(8):
                                    MM(pu_[:, :n], wu[:, c, jb * 128:(jb + 1) * 128], h2[:, c, :n], [t_wu, t_h2], [t_pu_], start=(c == 0), stop=(c == 7))
                                sil, t_sil = SIL.next()
                                ACT(sil[:, :n], pg_[:, :n], AF.Silu, [t_pg_], [t_sil])
                                if moe:
                                    TT("dve", sil[:, :n], sil[:, :n], gbc[:, :n], ALU.mult, [t_sil, t_gbc], [t_sil])
                                TT("dve", ab_[:, jb, :n], pu_[:, :n], sil[:, :n], ALU.mult, [t_pu_, t_sil], [t_ab_])
                            for m in range(8):
                                pd_, t_pd_ = PS.next()
                                for jb in range(nj):
                                    MM(pd_[:, :n], wd[:, jb, m * 128:(m + 1) * 128], ab_[:, jb, :n], [t_wd, t_ab_], [t_pd_], start=(jb == 0), stop=(jb == nj - 1))
                                TT("dve", ya[:, m, :n], pd_[:, :n], ya[:, m, :n], ALU.add, [t_pd_, t_ya], [t_ya])
                    j = 1 if t0 < NCTX else 0
                    if moe:
                        xs, t_xs = xb, t_xb
                    else:
                        xs, t_xs = work["x"].next()
                        fw.dma("sp", xs[:, :, :n], S["xres"].ap[:, t0:t0 + n].rearrange("(c p) t -> p c t", p=128), reads=S["xres"].tu(t0, n), writes=[t_xs])
                    for m in range(8):
                        STT("dve", xs[:, m, :n], ya[:, m, :n], prm[:, l, 5, m, j:j + 1], xs[:, m, :n], ALU.mult, ALU.add, [t_ya, t_xs, t_prm], [t_xs])
                    if not moe:
                        fw.dma("pool", S["xres"].ap[:, t0:t0 + n].rearrange("(c p) t -> p c t", p=128), xs[:, :, :n], reads=[t_xs], writes=S["xres"].tu(t0, n))
                    else:
                        sq, t_sq = work["sq"].next(); rs, t_rs = work["rs"].next()
                        ACT(sq[:, :, :n], xs[:, :, :n], AF.Square, [t_xs], [t_sq])
                        ps, t_ps = PS.next()
                        for c in range(8):
                            MM(ps[:, :n], ones_b, sq[:, c, :n], [t_sq, t_cst], [t_ps], start=(c == 0), stop=(c == 7))
                        ACT(rs[:, :n], ps[:, :n], AF.Sqrt, [t_ps], [t_rs], scale=1.0 / D, bias=1e-6)
                        fw.op("dve", "reciprocal", [t_rs], [t_rs], out=rs[:, :n], in_=rs[:, :n])
                        TT("dve", xs[:, :, :n], xs[:, :, :n], rs[:, :n].unsqueeze(1).to_broadcast([128, 8, n]), ALU.mult, [t_rs, t_xs], [t_xs])
                        TT("pool", xs[:, :, :n], xs[:, :, :n], gfin[:].unsqueeze(2).to_broadcast([128, 8, n]), ALU.mult, [t_xs, t_gfin], [t_xs])
                        fw.dma("pool", out.ap[:, hi * 512:(hi + 1) * 512].rearrange("(c p) t -> p c t", p=128), xs[:, :, :n], reads=[t_xs], writes=out.tu(hi * 512, 512))
                fw.barrier()

        fw.barrier()
        fw.finish()
        nc._fw_counts = {n: e.cnt for n, e in fw.engs.items()}
        nc._fw_counts["dma"] = {q: sum(u for _, u in lst) for q, (lst, _) in fw.dma_sems.items()}
    return nc


def make_consts():
    c = np.zeros((128, 1024), np.float32)
    p = np.arange(128)[:, None]
    f = np.arange(128)[None, :]
    c[:, 0:128] = (p == f)
    c[:, 128:256] = (p <= f)
    c[:, 256:384] = (p < f)
    c[:, 384:512] = (p >= f)
    c[:, 512:640] = (p > f)
    c[:, 640:768] = (p // 64 == f // 64)
    c[:, 768:896] = 1.0
    c[:, 896:898] = (p // 64 == np.arange(2)[None, :])
    return c


def make_in_maps(inputs):
    f32 = lambda a: np.ascontiguousarray(np.asarray(a, dtype=np.float32))
    shared = {k: f32(inputs[k]) for k in ["w_ada", "b_ada", "g_norm_mix", "g_norm_ffn", "w_in", "b_mlstm_gate", "g_mlstm_norm", "mu_shift", "w0", "w2",
                                          "a0", "a2", "g2", "k_k", "k_a", "ln_w", "ln_b", "w_proj_mlstm", "w_proj_rwkv", "w_out", "w_ff_gate", "w_ff_up",
                                          "w_ff_down", "w_router", "w_exp_gate", "w_exp_up", "w_exp_down", "g_final"]}
    shared["r_k"] = f32(inputs["r_k"]).reshape(DEPTH, D)
    shared["cst"] = make_consts()
    x = f32(inputs["x"]); ctx = f32(inputs["ctx"]); c = f32(inputs["c"]); c_ctx = f32(inputs["c_ctx"])
    maps = []
    for core in range(8):
        b, half = core // 2, core % 2
        m = dict(shared)
        m["xT"] = np.ascontiguousarray(np.concatenate([ctx[b], x[b]], axis=0).T)
        m["cs"] = np.ascontiguousarray(np.stack([c[b], c_ctx], axis=1))
        sel = np.zeros((128, 2), np.float32)
        sel[:, half] = 1.0
        m["sel"] = sel
        maps.append(m)
    return maps


_NC_CACHE = {}


def kernel(**inputs):
    if "nc" not in _NC_CACHE:
        _NC_CACHE["nc"] = build()
    nc = _NC_CACHE["nc"]
    maps = make_in_maps(inputs)
    res = run_bass_kernel_spmd(nc, maps, core_ids=list(range(8)))
    outp = np.zeros((4, NLAT, D), np.float32)
    for core in range(8):
        b, half = core // 2, core % 2
        outp[b, half * 2048:(half + 1) * 2048, :] = res.results[core]["out"].T
    return outp
```

```python
import numpy as np
from contextlib import ExitStack
import concourse.bass as bass
import concourse.mybir as mybir
from concourse.bass_utils import run_bass_kernel_spmd

F32 = mybir.dt.float32
BF16 = mybir.dt.bfloat16
AF = mybir.ActivationFunctionType
ALU = mybir.AluOpType
AX = mybir.AxisListType

SAME_ENGINE_SYNC = True
E3_PHASE2 = True

D = 1024
NCTX = 256
NLAT = 4096
NT = NCTX + NLAT
DEPTH = 2
D_IN = 8608
D_FF = 2816
D_FFE = 3584
NEXP = 8
TILES = [(0, 256)] + [(256 + 512 * i, 512) for i in range(8)]
LAM = 0.6065306597126334


class TU:
    __slots__ = ("name", "w", "r")

    def __init__(self, name=""):
        self.name = name
        self.w = {}
        self.r = {}


class Eng:
    def __init__(self, name, sem):
        self.name = name
        self.sem = sem
        self.cnt = 0
        self.seen = {}
        self.prog = []


class FW:
    def __init__(self, nc, stack):
        self.nc = nc
        self.engs = {}
        for n in ("pe", "act", "dve", "pool", "sp"):
            sem = stack.enter_context(nc.semaphore("sem_" + n))
            self.engs[n] = Eng(n, sem)
        self.dma_sems = {}
        for q, k in (("sp", 24), ("pool", 12), ("act", 4)):
            lst = [[stack.enter_context(nc.semaphore(f"dq_{q}_{i}")), 0] for i in range(k)]
            self.dma_sems[q] = [lst, 0]
        self.n_instr = 0

    def _gather(self, eng, reads, writes):
        deps = {}
        for t in reads:
            for k, (s, v) in t.w.items():
                if deps.get(k, (None, 0))[1] < v:
                    deps[k] = (s, v)
        for t in writes:
            for d in (t.w, t.r):
                for k, (s, v) in d.items():
                    if deps.get(k, (None, 0))[1] < v:
                        deps[k] = (s, v)
        for k, (s, v) in deps.items():
            if k == id(eng.sem) and (eng.name in ("pe", "sp") or not SAME_ENGINE_SYNC):
                continue
            if eng.seen.get(k, 0) < v:
                eng.seen[k] = v
                eng.prog.append(lambda e, s=s, v=v: e.wait_ge(s, v))

    def _record(self, ev, reads, writes):
        k = id(ev[0])
        for t in reads:
            if t.r.get(k, (None, 0))[1] < ev[1]:
                t.r[k] = ev
        for t in writes:
            t.w = {k: ev}
            t.r = {}

    def op(self, engname, fn, reads=(), writes=(), **kw):
        eng = self.engs[engname]
        self._gather(eng, reads, writes)
        eng.cnt += 1
        ev = (eng.sem, eng.cnt)
        sem = eng.sem
        if isinstance(fn, str):
            eng.prog.append(lambda e, fn=fn, sem=sem, kw=kw: getattr(e, fn)(**kw).then_inc(sem, 1))
        else:
            eng.prog.append(lambda e, fn=fn, sem=sem: fn(e).then_inc(sem, 1))
        self._record(ev, reads, writes)
        self.n_instr += 1

    def dma(self, q, out_ap, in_ap, reads=(), writes=(), **kw):
        eng = self.engs[q]
        self._gather(eng, reads, writes)
        lst, idx = self.dma_sems[q]
        ent = lst[idx % len(lst)]
        self.dma_sems[q][1] = idx + 1
        sem, uses = ent
        if uses > 0 and eng.seen.get(id(sem), 0) < 16 * uses:
            eng.seen[id(sem)] = 16 * uses
            eng.prog.append(lambda e, s=sem, v=16 * uses: e.wait_ge(s, v))
        ent[1] = uses + 1
        ev = (sem, 16 * (uses + 1))
        eng.prog.append(lambda e, o=out_ap, i=in_ap, sem=sem, kw=kw: e.dma_start(out=o, in_=i, **kw).then_inc(sem, 16))
        self._record(ev, reads, writes)
        self.n_instr += 1

    def barrier(self):
        evs = []
        for n, e in self.engs.items():
            if e.cnt > 0:
                evs.append((e.sem, e.cnt))
        for q, (lst, _) in self.dma_sems.items():
            for sem, uses in lst:
                if uses > 0:
                    evs.append((sem, 16 * uses))
        for n, e in self.engs.items():
            for s, v in evs:
                if s is e.sem:
                    continue
                if e.seen.get(id(s), 0) < v:
                    e.seen[id(s)] = v
                    e.prog.append(lambda en, s=s, v=v: en.wait_ge(s, v))

    def finish(self):
        nc = self.nc
        with nc.Block() as block:
            @block.tensor
            def _(e):
                for f in self.engs["pe"].prog:
                    f(e)

            @block.scalar
            def _(e):
                for f in self.engs["act"].prog:
                    f(e)

            @block.vector
            def _(e):
                for f in self.engs["dve"].prog:
                    f(e)

            @block.gpsimd
            def _(e):
                for f in self.engs["pool"].prog:
                    f(e)

            @block.sync
            def _(e):
                for f in self.engs["sp"].prog:
                    f(e)


_UID = [0]


class Ring:
    def __init__(self, nc, stack, name, shape, dtype, n, psum=False):
        self.bufs = []
        for i in range(n):
            if psum:
                t = stack.enter_context(nc.psum_tensor(f"{name}{i}", shape, dtype))
            else:
                _UID[0] += 1
                t = stack.enter_context(nc.sbuf_tensor(f"{name}{i}_u{_UID[0]}", shape, dtype))
            self.bufs.append((t, TU(f"{name}{i}")))
        self.i = 0

    def next(self):
        b = self.bufs[self.i % len(self.bufs)]
        self.i += 1
        return b


class DT:
    def __init__(self, nc, name, shape, dtype, kind="Internal"):
        self.t = nc.dram_tensor(name, shape, dtype, kind=kind)
        self.ap = self.t.ap()
        self.tus = {}
        self.name = name

    def tu(self, t0=0, n=NT, key=None):
        out = []
        for c in range(t0 // 128, (t0 + n + 127) // 128):
            k = (key, c)
            if k not in self.tus:
                self.tus[k] = TU(f"{self.name}{k}")
            out.append(self.tus[k])
        return out


def build(debug=None, nlayers=DEPTH, stop=None):
    debug = debug or []
    nc = bass.Bass("TRN2", target_bir_lowering=False)

    def SBT(name, shape, dt):
        _UID[0] += 1
        return nc.sbuf_tensor(f"{name}_u{_UID[0]}", shape, dt)
    I = {}

    def inp(name, shape, dt=F32):
        I[name] = DT(nc, name, shape, dt, kind="ExternalInput")
        return I[name]

    inp("xT", [D, NT]); inp("cs", [D, 2]); inp("sel", [128, 2]); inp("cst", [128, 1024])
    inp("w_ada", [DEPTH, D, 6 * D]); inp("b_ada", [DEPTH, 6 * D])
    inp("g_norm_mix", [DEPTH, D]); inp("g_norm_ffn", [DEPTH, D])
    inp("w_in", [DEPTH, D, D_IN]); inp("b_mlstm_gate", [DEPTH, 4, 8]); inp("g_mlstm_norm", [DEPTH, D])
    inp("mu_shift", [DEPTH, 3456]); inp("w0", [DEPTH, 2, D]); inp("w2", [DEPTH, 2, 64, D])
    inp("a0", [DEPTH, 2, D]); inp("a2", [DEPTH, 2, 64, D]); inp("g2", [DEPTH, 128, D])
    inp("k_k", [DEPTH, D]); inp("k_a", [DEPTH, D]); inp("r_k", [DEPTH, D])
    inp("ln_w", [DEPTH, D]); inp("ln_b", [DEPTH, D])
    inp("w_proj_mlstm", [DEPTH, D, D]); inp("w_proj_rwkv", [DEPTH, D, D]); inp("w_out", [DEPTH, D, D])
    inp("w_ff_gate", [1, D, D_FF]); inp("w_ff_up", [1, D, D_FF]); inp("w_ff_down", [1, D_FF, D])
    inp("w_router", [1, D, NEXP]); inp("w_exp_gate", [1, NEXP, D, D_FFE]); inp("w_exp_up", [1, NEXP, D, D_FFE])
    inp("w_exp_down", [1, NEXP, D_FFE, D]); inp("g_final", [D])
    out = DT(nc, "out", [D, NLAT // 2], F32, kind="ExternalOutput")
    dbg = {}
    for name, shape, dt in debug:
        dbg[name] = DT(nc, "dbg_" + name, shape, dt, kind="ExternalOutput")

    S = {}

    def scr(name, shape, dt):
        if name in dbg:
            S[name] = dbg[name]
        else:
            S[name] = DT(nc, "s_" + name, shape, dt)
        return S[name]

    scr("xres", [D, NT], F32)
    scr("qT", [512, NT], BF16); scr("kT", [512, NT], BF16); scr("ktm", [NT, 512], BF16)
    scr("vtm", [NT, 1024], BF16); scr("otm", [NT, 1024], BF16); scr("gT", [32, NT], F32)
    scr("pT", [3456, NT], BF16); scr("mgT", [2048, NT], BF16)
    scr("hm", [2, NT, 1024], F32); scr("moT", [D, NT], BF16)
    scr("gsc", [2, 3, 8, NT], F32)

    with ExitStack() as st:
        fw = FW(nc, st)
        psall = st.enter_context(nc.psum_tensor("psall", [128, 4096], F32))

        class PSRing:
            def __init__(self):
                self.tus = [TU(f"psb{i}") for i in range(8)]
                self.i = 0

            n = 8

            def next(self):
                b = self.i % self.n
                self.i += 1
                return psall[:, b * 512:(b + 1) * 512], self.tus[b]

            def next2(self):
                if self.i % 2:
                    self.i += 1
                b = self.i % self.n
                self.i += 2
                return psall[:, b * 512:(b + 2) * 512].rearrange("p (h x) -> p h x", x=512), [self.tus[b], self.tus[b + 1]]

            def pair(self, b):
                return psall[:, b * 512:(b + 2) * 512].rearrange("p (h x) -> p h x", x=512), [self.tus[b], self.tus[b + 1]]
        PS = PSRing()
        cst = st.enter_context(SBT("cst_sb", [128, 1024], F32))
        cstb = st.enter_context(SBT("cstb_sb", [128, 1024], BF16))
        t_cst = TU("cst")
        fw.dma("sp", cst[:], I["cst"].ap, writes=[t_cst])
        fw.op("dve", lambda e: e.tensor_copy(out=cstb[:], in_=cst[:]), reads=[t_cst], writes=[t_cst])
        ident_f, ident_b = cst[:, 0:128], cstb[:, 0:128]
        m_le, m_lt, m_ge, m_gt = (cst[:, 128 * i:128 * (i + 1)] for i in range(1, 5))
        blk1_f, blk1_b = cst[:, 640:768], cstb[:, 640:768]
        ones_b = cstb[:, 768:896]
        ones_f = cst[:, 768:896]
        sel2_f = cst[:, 896:898]
        mods = st.enter_context(SBT("mods", [128, DEPTH, 48, 2], F32))
        t_mods = TU("mods")
        prm = st.enter_context(SBT("prm", [128, DEPTH, 6, 8, 2], F32))
        t_prm = TU("prm")
        sel_sb = st.enter_context(SBT("sel_sb", [128, 2], F32))
        fw.dma("sp", sel_sb[:], I["sel"].ap, writes=[t_cst])

        with ExitStack() as sa:
            s_sb = sa.enter_context(SBT("s_sb", [128, 8, 2], F32))
            bada = sa.enter_context(SBT("bada", [128, DEPTH, 48], F32))
            gn = sa.enter_context(SBT("gn", [128, DEPTH, 2, 8], F32))
            t_s, t_b, t_gn = TU("s"), TU("bada"), TU("gn")
            fw.dma("sp", s_sb[:], I["cs"].ap.rearrange("(c p) j -> p c j", p=128), writes=[t_s])
            fw.op("act", lambda e: e.activation(out=s_sb[:], in_=s_sb[:], func=AF.Silu), reads=[t_s], writes=[t_s])
            for l_ in range(DEPTH):
                fw.dma("sp", bada[:, l_, :], I["b_ada"].ap[l_].rearrange("(j p) -> p j", p=128), writes=[t_b], allow_slow_non_contiguous=True)
                fw.dma("sp", gn[:, l_, 0, :], I["g_norm_mix"].ap[l_].rearrange("(c p) -> p c", p=128), writes=[t_gn], allow_slow_non_contiguous=True)
                fw.dma("sp", gn[:, l_, 1, :], I["g_norm_ffn"].ap[l_].rearrange("(c p) -> p c", p=128), writes=[t_gn], allow_slow_non_contiguous=True)
            WA = Ring(nc, sa, "wa", [128, 8, 512], F32, 3)
            for l in range(nlayers):
                ps, t_ps = PS.next()
                for jg in range(12):
                    wt, t_w = WA.next()
                    fw.dma("sp", wt[:], I["w_ada"].ap[l, :, jg * 512:(jg + 1) * 512].rearrange("(c p) m -> p c m", p=128), writes=[t_w])
                    for jj in range(4):
                        j = jg * 4 + jj
                        for c in range(8):
                            fw.op("pe", lambda e, ps=ps, wt=wt, c=c, j=j, jj=jj: e.matmul(ps[:, 2 * j:2 * j + 2], lhsT=wt[:, c, jj * 128:(jj + 1) * 128], rhs=s_sb[:, c, :], start=(c == 0), stop=(c == 7)),
                                  reads=[t_w, t_s], writes=[t_ps])
                fw.op("dve", lambda e, ps=ps, l=l: e.tensor_tensor(out=mods[:, l], in0=ps[:, 0:96].rearrange("p (j t) -> p j t", t=2), in1=bada[:, l].unsqueeze(2).to_broadcast([128, 48, 2]), op=ALU.add),
                      reads=[t_ps, t_b], writes=[t_mods])
                for si in range(2):
                    o = 3 * si
                    fw.op("dve", lambda e, l=l, si=si, o=o: e.scalar_tensor_tensor(out=prm[:, l, o], in0=mods[:, l, (o + 1) * 8:(o + 2) * 8, :], scalar=1.0, in1=gn[:, l, si, :].unsqueeze(2).to_broadcast([128, 8, 2]), op0=ALU.add, op1=ALU.mult),
                          reads=[t_mods, t_gn], writes=[t_prm])
                    fw.op("dve", lambda e, l=l, o=o: e.tensor_copy(out=prm[:, l, o + 1], in_=mods[:, l, o * 8:(o + 1) * 8, :]), reads=[t_mods], writes=[t_prm])
                    fw.op("dve", lambda e, l=l, o=o: e.tensor_copy(out=prm[:, l, o + 2], in_=mods[:, l, (o + 2) * 8:(o + 3) * 8, :]), reads=[t_mods], writes=[t_prm])
            fw.barrier()
        if "mods" in dbg:
            fw.dma("sp", dbg["mods"].ap, mods[:].rearrange("p l j t -> p (l j t)"), reads=[t_mods], writes=dbg["mods"].tu())

        def norm_mod(sx, xsrc_ap, src_tus, t0, n, l, si, h_out, h_tus, work, h32=None):
            xt, t_xt = work["x"].next()
            sq, t_sq = work["sq"].next()
            rs, t_rs = work["rs"].next()
            if callable(xsrc_ap):
                xsrc_ap(xt, t_xt)
            else:
                fw.dma("sp", xt[:, :, :n], xsrc_ap[:, t0:t0 + n].rearrange("(c p) t -> p c t", p=128), reads=src_tus, writes=[t_xt])
            fw.op("act", lambda e: e.activation(out=sq[:, :, :n], in_=xt[:, :, :n], func=AF.Square), reads=[t_xt], writes=[t_sq])
            ps, t_ps = PS.next()
            for c in range(8):
                fw.op("pe", lambda e, c=c: e.matmul(ps[:, :n], lhsT=ones_b, rhs=sq[:, c, :n], start=(c == 0), stop=(c == 7)), reads=[t_sq, t_cst], writes=[t_ps])
            fw.op("act", lambda e: e.activation(out=rs[:, :n], in_=ps[:, :n], func=AF.Sqrt, scale=1.0 / D, bias=1e-6), reads=[t_ps], writes=[t_rs])
            fw.op("dve", lambda e: e.reciprocal(out=rs[:, :n], in_=rs[:, :n]), reads=[t_rs], writes=[t_rs])
            fw.op("dve", lambda e: e.tensor_tensor(out=xt[:, :, :n], in0=xt[:, :, :n], in1=rs[:, :n].unsqueeze(1).to_broadcast([128, 8, n]), op=ALU.mult), reads=[t_rs, t_xt], writes=[t_xt])
            j = 1 if t0 < NCTX else 0
            o = 3 * si
            for c in range(8):
                if c % 2 == 0:
                    fw.op("dve", lambda e, c=c: e.tensor_scalar(out=h_out[:, c, :], in0=xt[:, c, :n], scalar1=prm[:, l, o, c, j:j + 1], scalar2=prm[:, l, o + 1, c, j:j + 1], op0=ALU.mult, op1=ALU.add),
                          reads=[t_xt, t_prm], writes=[h_tus[0]])
                else:
                    fw.op("act", lambda e, c=c: e.activation(out=h_out[:, c, :], in_=xt[:, c, :n], func=AF.Identity, scale=prm[:, l, o, c, j:j + 1], bias=prm[:, l, o + 1, c, j:j + 1]),
                          reads=[t_xt, t_prm], writes=[h_tus[-1]])
                if h32 is not None:
                    if c % 2 == 1:
                        fw.op("dve", lambda e, c=c: e.tensor_scalar(out=h32[0][:, c, :n], in0=xt[:, c, :n], scalar1=prm[:, l, o, c, j:j + 1], scalar2=prm[:, l, o + 1, c, j:j + 1], op0=ALU.mult, op1=ALU.add),
                              reads=[t_xt, t_prm], writes=[h32[1]])
                    else:
                        fw.op("act", lambda e, c=c: e.activation(out=h32[0][:, c, :n], in_=xt[:, c, :n], func=AF.Identity, scale=prm[:, l, o, c, j:j + 1], bias=prm[:, l, o + 1, c, j:j + 1]),
                              reads=[t_xt, t_prm], writes=[h32[1]])

        for l in range(nlayers):
            xsrc = I["xT"] if l == 0 else S["xres"]
            with ExitStack() as sb:
                hT = sb.enter_context(SBT("hT", [128, 8, NT], BF16))
                t_h = [[TU(f"h{i}a"), TU(f"h{i}b")] for i in range(len(TILES))]
                work = {"x": Ring(nc, sb, "nx", [128, 8, 512], F32, 2), "sq": Ring(nc, sb, "nsq", [128, 8, 512], BF16, 1), "rs": Ring(nc, sb, "nrs", [128, 512], F32, 2)}
                for ti, (t0, n) in enumerate(TILES):
                    norm_mod(sb, xsrc.ap, xsrc.tu(t0, n), t0, n, l, 0, hT[:, :, t0:t0 + n], t_h[ti], work)
                if l == 0 and "hT" in dbg:
                    for ti, (t0, n) in enumerate(TILES):
                        fw.dma("sp", dbg["hT"].ap[:, t0:t0 + n].rearrange("(c p) t -> p c t", p=128), hT[:, :, t0:t0 + n], reads=t_h[ti], writes=dbg["hT"].tu(t0, n))
                WS = Ring(nc, sb, "ws", [128, 8, 512], F32, 2)
                WB = Ring(nc, sb, "wb", [128, 8, 512], BF16, 2)
                EV = Ring(nc, sb, "ev", [128, 512], BF16, 4)
                EVF = Ring(nc, sb, "evf", [128, 512], F32, 2)
                groups = [(0, 512, "fq", None), (512, 512, "fk", None), (512, 512, "tk", None),
                          (1024, 512, "tv", 0), (1536, 512, "tv", 512), (2048, 512, "to", 0), (2560, 512, "to", 512),
                          (3072, 32, "fg", None)]
                for i in range(7):
                    w = 512 if i < 6 else 384
                    groups.append((3104 + 512 * i, w, "fp", 512 * i))
                for i in range(4):
                    groups.append((6560 + 512 * i, 512, "fm", 512 * i))
                evi = 0
                for (c0, w, kind, dst) in groups:
                    ws, t_ws = WS.next()
                    wb, t_wb = WB.next()
                    fw.dma("sp", ws[:, :, :w], I["w_in"].ap[l, :, c0:c0 + w].rearrange("(c p) m -> p c m", p=128), writes=[t_ws])
                    fw.op("dve", lambda e, ws=ws, wb=wb, w=w: e.tensor_copy(out=wb[:, :, :w], in_=ws[:, :, :w]), reads=[t_ws], writes=[t_wb])
                    for ti, (t0, n) in enumerate(TILES):
                        if kind[0] == "f":
                            for mb in range((w + 127) // 128):
                                mw = min(128, w - mb * 128)
                                ps, t_ps = PS.next()
                                for c in range(8):
                                    fw.op("pe", lambda e, ps=ps, wb=wb, c=c, mb=mb, mw=mw, t0=t0, n=n: e.matmul(ps[:mw, :n], lhsT=wb[:, c, mb * 128:mb * 128 + mw], rhs=hT[:, c, t0:t0 + n], start=(c == 0), stop=(c == 7)),
                                          reads=[t_wb] + t_h[ti], writes=[t_ps])
                                evi += 1
                                if kind == "fg":
                                    ev, t_ev = EVF.next()
                                    fw.op("dve", lambda e, ev=ev, ps=ps, mw=mw, n=n: e.tensor_copy(out=ev[:mw, :n], in_=ps[:mw, :n]), reads=[t_ps], writes=[t_ev])
                                    fw.dma("pool", S["gT"].ap[:, t0:t0 + n], ev[:mw, :n], reads=[t_ev], writes=S["gT"].tu(t0, n))
                                    continue
                                ev, t_ev = EV.next()
                                if kind == "fm":
                                    fw.op("act", lambda e, ev=ev, ps=ps, mw=mw, n=n: e.activation(out=ev[:mw, :n], in_=ps[:mw, :n], func=AF.Sigmoid), reads=[t_ps], writes=[t_ev])
                                elif kind == "fq":
                                    fw.op("act", lambda e, ev=ev, ps=ps, mw=mw, n=n: e.activation(out=ev[:mw, :n], in_=ps[:mw, :n], func=AF.Copy, scale=0.125), reads=[t_ps], writes=[t_ev])
                                elif evi % 2 == 0:
                                    fw.op("act", lambda e, ev=ev, ps=ps, mw=mw, n=n: e.activation(out=ev[:mw, :n], in_=ps[:mw, :n], func=AF.Copy), reads=[t_ps], writes=[t_ev])
                                else:
                                    fw.op("dve", lambda e, ev=ev, ps=ps, mw=mw, n=n: e.tensor_copy(out=ev[:mw, :n], in_=ps[:mw, :n]), reads=[t_ps], writes=[t_ev])
                                dname = {"fq": "qT", "fk": "kT", "fp": "pT", "fm": "mgT"}[kind]
                                r0 = (dst or 0) + mb * 128
                                fw.dma("pool", S[dname].ap[r0:r0 + mw, t0:t0 + n], ev[:mw, :n], reads=[t_ev], writes=S[dname].tu(t0, n, key=r0))
                        else:
                            for sub in range(n // 128):
                                ts = t0 + sub * 128
                                ps, t_ps = PS.next()
                                for c in range(8):
                                    fw.op("pe", lambda e, ps=ps, wb=wb, c=c, ts=ts, w=w: e.matmul(ps[:, :w], lhsT=hT[:, c, ts:ts + 128], rhs=wb[:, c, :w], start=(c == 0), stop=(c == 7)),
                                          reads=[t_wb] + t_h[ti], writes=[t_ps])
                                ev, t_ev = EV.next()
                                evi += 1
                                if kind == "to":
                                    fw.op("act", lambda e, ev=ev, ps=ps: e.activation(out=ev[:], in_=ps[:], func=AF.Sigmoid), reads=[t_ps], writes=[t_ev])
                                elif evi % 2 == 0:
                                    fw.op("act", lambda e, ev=ev, ps=ps: e.activation(out=ev[:], in_=ps[:], func=AF.Copy), reads=[t_ps], writes=[t_ev])
                                else:
                                    fw.op("dve", lambda e, ev=ev, ps=ps: e.tensor_copy(out=ev[:], in_=ps[:]), reads=[t_ps], writes=[t_ev])
                                dname = {"tk": "ktm", "tv": "vtm", "to": "otm"}[kind]
                                d0 = dst or 0
                                fw.dma("pool", S[dname].ap[ts:ts + 128, d0:d0 + w], ev[:, :w], reads=[t_ev], writes=S[dname].tu(ts, 128, key=d0))
                fw.barrier()
            if stop == "C":
                break
            NCH = NT // 64
            with ExitStack() as sd:
                etm = sd.enter_context(SBT("etm", [64, 2, NCH, 8], F32))
                ctm = sd.enter_context(SBT("ctm", [64, 2, NCH, 8], F32))
                omb = sd.enter_context(SBT("omb", [64, 2, NCH, 8], F32))
                t_etm, t_ctm, t_omb = TU("etm"), TU("ctm"), TU("omb")
                orders = [list(range(NCH)), [3, 2, 1, 0] + list(range(NCH - 1, 3, -1))]
                with ExitStack() as sd1:
                    bg = sd1.enter_context(SBT("bg", [8, 4], F32))
                    rmask = sd1.enter_context(SBT("rmask", [8, NT], F32))
                    t_bg, t_rm = TU("bg"), TU("rm")
                    fw.dma("sp", bg[:], I["b_mlstm_gate"].ap[l].rearrange("j h -> h j"), writes=[t_bg], allow_slow_non_contiguous=True)
                    fw.op("dve", lambda e: e.tensor_scalar(out=bg[:], in0=bg[:], scalar1=1.0 / 15.0, scalar2=None, op0=ALU.mult), reads=[t_bg], writes=[t_bg])
                    fw.op("pool", lambda e: e.memset(rmask[:], 1.0), writes=[t_rm])
                    fw.op("pool", lambda e: e.memset(rmask[:].rearrange("p (c l) -> p c l", l=64)[:, :, 0:1], 0.0), writes=[t_rm])
                    for d in range(2):
                        It = sd1.enter_context(SBT(f"It{d}", [8, NT], F32))
                        Ft = sd1.enter_context(SBT(f"Ft{d}", [8, NT], F32))
                        Cs = sd1.enter_context(SBT(f"Cs{d}", [8, NT], F32))
                        Bn = sd1.enter_context(SBT(f"Bn{d}", [8, NT], F32))
                        sm_ = sd1.enter_context(SBT(f"gsm{d}", [8, 8 * NCH], F32))
                        t_i, t_f, t_c, t_b, t_s = TU("It"), TU("Ft"), TU("Cs"), TU("Bn"), TU("gsm")
                        tot = sm_[:, 0:NCH]; G = sm_[:, NCH:2 * NCH]; Mc = sm_[:, 2 * NCH:3 * NCH]; mm_ = sm_[:, 3 * NCH:4 * NCH + 1]; om = sm_[:, 5 * NCH:6 * NCH]
                        fw.dma("sp", It[:], S["gT"].ap[(2 * d) * 8:(2 * d) * 8 + 8, :], reads=S["gT"].tu(), writes=[t_i])
                        fw.dma("sp", Ft[:], S["gT"].ap[(2 * d + 1) * 8:(2 * d + 1) * 8 + 8, :], reads=S["gT"].tu(), writes=[t_f])
                        fw.op("act", lambda e, It=It, d=d: e.activation(out=It[:], in_=It[:], func=AF.Tanh, scale=1.0 / 15.0, bias=bg[:, 2 * d:2 * d + 1]), reads=[t_i, t_bg], writes=[t_i])
                        fw.op("act", lambda e, Ft=Ft, d=d: e.activation(out=Ft[:], in_=Ft[:], func=AF.Tanh, scale=1.0 / 15.0, bias=bg[:, 2 * d + 1:2 * d + 2]), reads=[t_f, t_bg], writes=[t_f])
                        fw.op("act", lambda e, Ft=Ft: e.activation(out=Ft[:], in_=Ft[:], func=AF.Exp, scale=-15.0), reads=[t_f], writes=[t_f])
                        fw.op("act", lambda e, Ft=Ft: e.activation(out=Ft[:], in_=Ft[:], func=AF.Ln, bias=1.0), reads=[t_f], writes=[t_f])
                        fw.op("dve", lambda e, Cs=Cs, Ft=Ft: e.tensor_tensor_scan(out=Cs[:], data0=rmask[:], data1=Ft[:], initial=0.0, op0=ALU.mult, op1=ALU.add), reads=[t_rm, t_f], writes=[t_c])
                        cs3 = Cs[:].rearrange("p (c l) -> p c l", l=64)
                        fw.op("dve", lambda e, tot=tot, cs3=cs3: e.tensor_copy(out=tot.unsqueeze(2), in_=cs3[:, :, 63:64]), reads=[t_c], writes=[t_s])
                        if d == 0:
                            fw.op("dve", lambda e, Bn=Bn, Cs=Cs: e.tensor_copy(out=Bn[:], in_=Cs[:]), reads=[t_c], writes=[t_b])
                        else:
                            fw.op("dve", lambda e, Bn=Bn, Cs=Cs, Ft=Ft: e.tensor_tensor(out=Bn[:], in0=Ft[:], in1=Cs[:], op=ALU.subtract), reads=[t_c, t_f], writes=[t_b])
                            bn3 = Bn[:].rearrange("p (c l) -> p c l", l=64)
                            fw.op("dve", lambda e, bn3=bn3, tot=tot: e.tensor_tensor(out=bn3, in0=bn3, in1=tot.unsqueeze(2).to_broadcast([8, NCH, 64]), op=ALU.add), reads=[t_b, t_s], writes=[t_b])
                        fw.op("dve", lambda e, It=It, Bn=Bn: e.scalar_tensor_tensor(out=It[:], in0=It[:], scalar=15.0, in1=Bn[:], op0=ALU.mult, op1=ALU.add), reads=[t_i, t_b], writes=[t_i])
                        it3 = It[:].rearrange("p (c l) -> p c l", l=64)
                        fw.op("dve", lambda e, G=G, it3=it3: e.tensor_reduce(out=G, in_=it3, axis=AX.X, op=ALU.max), reads=[t_i], writes=[t_s])
                        fw.op("dve", lambda e, mm_=mm_: e.memset(mm_[:, 0:1], 0.0), writes=[t_s])
                        for i, c in enumerate(orders[d]):
                            fw.op("dve", lambda e, i=i, c=c, Mc=Mc, mm_=mm_, G=G: e.tensor_tensor(out=Mc[:, c:c + 1], in0=mm_[:, i:i + 1], in1=G[:, c:c + 1], op=ALU.max), reads=[t_s], writes=[t_s])
                            fw.op("dve", lambda e, i=i, c=c, Mc=Mc, mm_=mm_, om=om: e.tensor_tensor(out=om[:, c:c + 1], in0=mm_[:, i:i + 1], in1=Mc[:, c:c + 1], op=ALU.subtract), reads=[t_s], writes=[t_s])
                            fw.op("dve", lambda e, i=i, c=c, Mc=Mc, mm_=mm_, tot=tot: e.tensor_tensor(out=mm_[:, i + 1:i + 2], in0=Mc[:, c:c + 1], in1=tot[:, c:c + 1], op=ALU.subtract), reads=[t_s], writes=[t_s])
                        fw.op("act", lambda e, om=om: e.activation(out=om, in_=om, func=AF.Exp), reads=[t_s], writes=[t_s])
                        mcb = Mc.unsqueeze(2).to_broadcast([8, NCH, 64])
                        fw.op("dve", lambda e, it3=it3, mcb=mcb: e.tensor_tensor(out=it3, in0=it3, in1=mcb, op=ALU.subtract), reads=[t_i, t_s], writes=[t_i])
                        fw.op("act", lambda e, It=It: e.activation(out=It[:], in_=It[:], func=AF.Exp), reads=[t_i], writes=[t_i])
                        bn3 = Bn[:].rearrange("p (c l) -> p c l", l=64)
                        fw.op("dve", lambda e, bn3=bn3, mcb=mcb: e.tensor_tensor(out=bn3, in0=bn3, in1=mcb, op=ALU.subtract), reads=[t_b, t_s], writes=[t_b])
                        fw.op("act", lambda e, Bn=Bn: e.activation(out=Bn[:], in_=Bn[:], func=AF.Exp), reads=[t_b], writes=[t_b])
                        for (src, t_src, dst, t_dst) in ((It, t_i, etm, t_etm), (Bn, t_b, ctm, t_ctm)):
                            for half in range(2):
                                ps, t_ps = PS.next()
                                for cc in range(NCH // 2):
                                    c = half * (NCH // 2) + cc
                                    fw.op("pe", lambda e, ps=ps, src=src, c=c, cc=cc: e.matmul(ps[0:64, cc * 8:cc * 8 + 8], lhsT=src[:, c * 64:(c + 1) * 64], rhs=ident_f[0:8, 0:8], start=True, stop=True),
                                          reads=[t_src, t_cst], writes=[t_ps])
                                fw.op("dve", lambda e, ps=ps, dst=dst, half=half, d=d: e.tensor_copy(out=dst[:, d, half * (NCH // 2):(half + 1) * (NCH // 2), :], in_=ps[0:64, 0:(NCH // 2) * 8].rearrange("p (c h) -> p c h", h=8)),
                                      reads=[t_ps], writes=[t_dst])
                        X = sd1.enter_context(SBT(f"omx{d}", [8, NCH, 8], F32))
                        t_x = TU("omx")
                        fw.op("dve", lambda e, X=X, om=om: e.tensor_tensor(out=X[:], in0=om.unsqueeze(2).to_broadcast([8, NCH, 8]), in1=ident_f[0:8, 0:8].unsqueeze(1).to_broadcast([8, NCH, 8]), op=ALU.mult), reads=[t_s, t_cst], writes=[t_x])
                        for half in range(2):
                            ps, t_ps = PS.next()
                            hw = (NCH // 2) * 8
                            fw.op("pe", lambda e, ps=ps, X=X, half=half, hw=hw: e.matmul(ps[0:64, 0:hw], lhsT=ones_f[0:8, 0:64], rhs=X[:].rearrange("p c h -> p (c h)")[:, half * hw:(half + 1) * hw], start=True, stop=True), reads=[t_x, t_cst], writes=[t_ps])
                            fw.op("dve", lambda e, ps=ps, half=half, hw=hw, d=d: e.tensor_copy(out=omb[:, d, half * (NCH // 2):(half + 1) * (NCH // 2), :], in_=ps[0:64, 0:hw].rearrange("p (c h) -> p c h", h=8)), reads=[t_ps], writes=[t_omb])
                    fw.barrier()
                for nm_, tl_, tt_ in (("etm", etm, t_etm), ("ctm", ctm, t_ctm), ("omb", omb, t_omb)):
                    if nm_ in dbg:
                        fw.dma("sp", dbg[nm_].ap, tl_[:].rearrange("p d c h -> p (d c h)"), reads=[tt_], writes=dbg[nm_].tu())
                with ExitStack() as sd2:
                    QC = Ring(nc, sd2, "qc", [64, 8, 64], BF16, 4)
                    KC = Ring(nc, sd2, "kc", [64, 8, 64], BF16, 4)
                    KM = Ring(nc, sd2, "km", [64, 8, 64], BF16, 4)
                    VA = Ring(nc, sd2, "va", [64, 8, 130], BF16, 4)
                    SMF = Ring(nc, sd2, "smf", [64, 8, 64], F32, 4)
                    SM = Ring(nc, sd2, "sm", [64, 8, 64], BF16, 4)
                    KP = Ring(nc, sd2, "kp", [64, 8, 64], BF16, 4)
                    HO = Ring(nc, sd2, "ho", [64, 8, 128], F32, 4)
                    DEN = Ring(nc, sd2, "den", [64, 8], F32, 4)
                    for (va, t_va) in VA.bufs:
                        fw.op("pool", lambda e, va=va: e.memset(va[:, :, 128:130], 1.0), writes=[t_va])
                    st_d = []
                    for d in range(2):
                        C32 = sd2.enter_context(SBT(f"C32_{d}", [64, 8, 129], F32))
                        Cb = sd2.enter_context(SBT(f"Cb_{d}", [64, 8, 129], BF16))
                        t_C, t_Cb = TU("C32"), TU("Cb")
                        fw.op("pool", lambda e, C32=C32: e.memset(C32[:], 0.0), writes=[t_C])
                        fw.op("pool", lambda e, Cb=Cb: e.memset(Cb[:], 0.0), writes=[t_Cb])
                        mask = (m_le if d == 0 else m_ge)[0:64, 0:64]
                        st_d.append((C32, Cb, t_C, t_Cb, mask))
                    for i in range(NCH):
                        for d in range(2):
                            C32, Cb, t_C, t_Cb, mask = st_d[d]
                            order = orders[d]
                            c = order[i]
                            t0 = c * 64
                            qc, t_qc = QC.next(); kc, t_kc = KC.next(); km, t_km = KM.next(); va, t_va = VA.next()
                            fw.dma("sp", qc[:], S["qT"].ap[:, t0:t0 + 64].rearrange("(h d) t -> d h t", d=64), reads=[x for r0 in range(0, 512, 128) for x in S["qT"].tu(t0, 64, key=r0)], writes=[t_qc])
                            fw.dma("sp", kc[:], S["kT"].ap[:, t0:t0 + 64].rearrange("(h d) t -> d h t", d=64), reads=[x for r0 in range(0, 512, 128) for x in S["kT"].tu(t0, 64, key=r0)], writes=[t_kc])
                            fw.dma("sp", km[:].rearrange("p h d -> p (h d)"), S["ktm"].ap[t0:t0 + 64, :], reads=S["ktm"].tu(t0, 64, key=0), writes=[t_km])
                            fw.dma("sp", va[:, :, 0:128], S["vtm"].ap[t0:t0 + 64, :].rearrange("t (h v) -> t h v", v=128), reads=S["vtm"].tu(t0, 64, key=0) + S["vtm"].tu(t0, 64, key=512), writes=[t_va])
                            ps1, t_ps1 = PS.next()
                            for h in range(8):
                                fw.op("pe", lambda e, ps1=ps1, kc=kc, qc=qc, h=h: e.matmul(ps1[0:64, h * 64:(h + 1) * 64], lhsT=kc[:, h, :], rhs=qc[:, h, :], start=True, stop=True), reads=[t_kc, t_qc], writes=[t_ps1])
                            smf, t_smf = SMF.next(); sm, t_sm = SM.next(); kp, t_kp = KP.next()
                            eb = etm[:, d, c, :].unsqueeze(2).to_broadcast([64, 8, 64])
                            fw.op("dve", lambda e, smf=smf, ps1=ps1, eb=eb: e.tensor_tensor(out=smf[:], in0=ps1[0:64, :].rearrange("p (h t) -> p h t", t=64), in1=eb, op=ALU.mult), reads=[t_ps1, t_etm], writes=[t_smf])
                            fw.op("dve", lambda e, smf=smf, sm=sm, mask=mask: e.tensor_tensor(out=sm[:], in0=smf[:], in1=mask.unsqueeze(1).to_broadcast([64, 8, 64]), op=ALU.mult), reads=[t_smf, t_cst], writes=[t_sm])
                            fw.op("dve", lambda e, kp=kp, km=km, eb=eb: e.tensor_tensor(out=kp[:], in0=km[:], in1=eb, op=ALU.mult), reads=[t_km, t_etm], writes=[t_kp])
                            groups3 = [(0, 3), (3, 3), (6, 2)]
                            psn = [PS.next() for _ in range(3)]
                            for gi, (h0, nh) in enumerate(groups3):
                                for j in range(nh):
                                    h = h0 + j
                                    fw.op("pe", lambda e, p=psn[gi][0], sm=sm, va=va, h=h, j=j: e.matmul(p[0:64, j * 129:(j + 1) * 129], lhsT=sm[:, h, :], rhs=va[:, h, 0:129], start=True, stop=False), reads=[t_sm, t_va], writes=[psn[gi][1]])
                                    fw.op("pe", lambda e, p=psn[gi][0], qc=qc, Cb=Cb, h=h, j=j: e.matmul(p[0:64, j * 129:(j + 1) * 129], lhsT=qc[:, h, :], rhs=Cb[:, h, :], start=False, stop=True), reads=[t_qc, t_Cb], writes=[psn[gi][1]])
                            ho, t_ho = HO.next(); den, t_den = DEN.next()
                            for gi, (h0, nh) in enumerate(groups3):
                                pv = psn[gi][0][0:64, 0:nh * 129].rearrange("p (h n) -> p h n", n=129)
                                fw.op("act", lambda e, den=den, pv=pv, h0=h0, nh=nh: e.activation(out=den[:, h0:h0 + nh].unsqueeze(2), in_=pv[:, :, 128:129], func=AF.Abs), reads=[psn[gi][1]], writes=[t_den])
                            fw.op("dve", lambda e, den=den, d=d, c=c: e.tensor_tensor(out=den[:], in0=den[:], in1=ctm[:, d, c, :], op=ALU.max), reads=[t_den, t_ctm], writes=[t_den])
                            fw.op("dve", lambda e, den=den: e.reciprocal(out=den[:], in_=den[:]), reads=[t_den], writes=[t_den])
                            for gi, (h0, nh) in enumerate(groups3):
                                pv = psn[gi][0][0:64, 0:nh * 129].rearrange("p (h n) -> p h n", n=129)
                                fw.op("dve", lambda e, ho=ho, den=den, pv=pv, h0=h0, nh=nh: e.tensor_tensor(out=ho[:, h0:h0 + nh, :], in0=pv[:, :, 0:128], in1=den[:, h0:h0 + nh].unsqueeze(2).to_broadcast([64, nh, 128]), op=ALU.mult), reads=[psn[gi][1], t_den], writes=[t_ho])
                            fw.dma("pool", S["hm"].ap[d, t0:t0 + 64, :], ho[:].rearrange("p h v -> p (h v)"), reads=[t_ho], writes=S["hm"].tu(t0, 64, key=d))
                            if i + 1 < len(order):
                                psc = [PS.next() for _ in range(3)]
                                for gi, (h0, nh) in enumerate(groups3):
                                    for j in range(nh):
                                        h = h0 + j
                                        fw.op("pe", lambda e, p=psc[gi][0], kp=kp, va=va, h=h, j=j: e.matmul(p[0:64, j * 129:(j + 1) * 129], lhsT=kp[:, h, :], rhs=va[:, h, 0:129], start=True, stop=True), reads=[t_kp, t_va], writes=[psc[gi][1]])
                                for gi, (h0, nh) in enumerate(groups3):
                                    pv = psc[gi][0][0:64, 0:nh * 129].rearrange("p (h n) -> p h n", n=129)
                                    fw.op("dve", lambda e, C32=C32, pv=pv, h0=h0, nh=nh: e.tensor_tensor(out=C32[:, h0:h0 + nh, :], in0=pv, in1=C32[:, h0:h0 + nh, :], op=ALU.add), reads=[psc[gi][1], t_C], writes=[t_C])
                                cn = order[i + 1]
                                fw.op("dve", lambda e, C32=C32, cn=cn, d=d: e.tensor_tensor(out=C32[:], in0=C32[:], in1=omb[:, d, cn, :].unsqueeze(2).to_broadcast([64, 8, 129]), op=ALU.mult), reads=[t_C, t_omb], writes=[t_C])
                                fw.op("act", lambda e, C32=C32, Cb=Cb: e.activation(out=Cb[:], in_=C32[:], func=AF.Copy), reads=[t_C], writes=[t_Cb])
                    fw.barrier()
                with ExitStack() as sd3:
                    gmb = sd3.enter_context(SBT("gmb", [128, 1024], F32))
                    t_gmb = TU("gmb")
                    fw.dma("sp", gmb[:], I["g_mlstm_norm"].ap[l].partition_broadcast(128), writes=[t_gmb])
                    HF = Ring(nc, sd3, "hf", [128, 1024], F32, 2)
                    HB = Ring(nc, sd3, "hb", [128, 1024], F32, 2)
                    SO = Ring(nc, sd3, "so", [128, 1024], BF16, 2)
                    SQ = Ring(nc, sd3, "hsq", [128, 1024], F32, 1)
                    SS = Ring(nc, sd3, "hss", [128, 8], F32, 2)
                    MO = Ring(nc, sd3, "mo", [128, 1024], BF16, 2)
                    MT = Ring(nc, sd3, "mt", [128, 8, 128], BF16, 2)
                    for tt in range(NT // 128):
                        t0 = tt * 128
                        hf, t_hf = HF.next(); hb, t_hb = HB.next(); so, t_so = SO.next(); sq, t_sq = SQ.next(); ss, t_ss = SS.next(); mo, t_mo = MO.next(); mt, t_mt = MT.next()
                        fw.dma("sp", hf[:], S["hm"].ap[0, t0:t0 + 128, :], reads=S["hm"].tu(t0, 128, key=0), writes=[t_hf])
                        fw.dma("sp", hb[:], S["hm"].ap[1, t0:t0 + 128, :], reads=S["hm"].tu(t0, 128, key=1), writes=[t_hb])
                        fw.dma("sp", so[:], S["otm"].ap[t0:t0 + 128, :], reads=S["otm"].tu(t0, 128, key=0) + S["otm"].tu(t0, 128, key=512), writes=[t_so])
                        fw.op("dve", lambda e, hf=hf, hb=hb: e.tensor_tensor(out=hf[:], in0=hf[:], in1=hb[:], op=ALU.add), reads=[t_hf, t_hb], writes=[t_hf])
                        fw.op("act", lambda e, sq=sq, hf=hf: e.activation(out=sq[:], in_=hf[:], func=AF.Square), reads=[t_hf], writes=[t_sq])
                        fw.op("dve", lambda e, ss=ss, sq=sq: e.tensor_reduce(out=ss[:], in_=sq[:].rearrange("p (h v) -> p h v", v=128), axis=AX.X, op=ALU.add), reads=[t_sq], writes=[t_ss])
                        fw.op("act", lambda e, ss=ss: e.activation(out=ss[:], in_=ss[:], func=AF.Sqrt, scale=1.0 / 128, bias=1e-6), reads=[t_ss], writes=[t_ss])
                        fw.op("dve", lambda e, ss=ss: e.reciprocal(out=ss[:], in_=ss[:]), reads=[t_ss], writes=[t_ss])
                        hf3 = hf[:].rearrange("p (h v) -> p h v", v=128)
                        fw.op("dve", lambda e, hf3=hf3, ss=ss: e.tensor_tensor(out=hf3, in0=hf3, in1=ss[:].unsqueeze(2).to_broadcast([128, 8, 128]), op=ALU.mult), reads=[t_hf, t_ss], writes=[t_hf])
                        fw.op("pool", lambda e, hf=hf: e.tensor_tensor(out=hf[:], in0=hf[:], in1=gmb[:], op=ALU.mult), reads=[t_hf, t_gmb], writes=[t_hf])
                        fw.op("dve", lambda e, hf=hf, so=so, mo=mo: e.tensor_tensor(out=mo[:], in0=hf[:], in1=so[:], op=ALU.mult), reads=[t_hf, t_so], writes=[t_mo])
                        if "motm" in dbg:
                            fw.dma("pool", dbg["motm"].ap[t0:t0 + 128, :], mo[:], reads=[t_mo], writes=dbg["motm"].tu(t0, 128))
                        for hb2 in range(2):
                            ps, t_ps = PS.next()
                            for j in range(4):
                                cb = hb2 * 4 + j
                                fw.op("pe", lambda e, ps=ps, mo=mo, cb=cb, j=j: e.matmul(ps[:, j * 128:(j + 1) * 128], lhsT=mo[:, cb * 128:(cb + 1) * 128], rhs=ident_b, start=True, stop=True), reads=[t_mo, t_cst], writes=[t_ps])
                            fw.op("act", lambda e, ps=ps, mt=mt, hb2=hb2: e.activation(out=mt[:, hb2 * 4:(hb2 + 1) * 4, :], in_=ps[:].rearrange("p (c t) -> p c t", t=128), func=AF.Copy), reads=[t_ps], writes=[t_mt])
                        fw.dma("pool", S["moT"].ap[:, t0:t0 + 128].rearrange("(c p) t -> p c t", p=128), mt[:], reads=[t_mt], writes=S["moT"].tu(t0, 128))
                    fw.barrier()
            if stop == "D":
                break
            NC2 = NT // 128

            def TT(eng, out, in0, in1, op, reads, writes):
                fw.op(eng, "tensor_tensor", reads, writes, out=out, in0=in0, in1=in1, op=op)

            def TS(eng, out, in0, s1, s2, op0, op1, reads, writes):
                if op1 is None:
                    fw.op(eng, "tensor_scalar", reads, writes, out=out, in0=in0, scalar1=s1, scalar2=None, op0=op0)
                else:
                    fw.op(eng, "tensor_scalar", reads, writes, out=out, in0=in0, scalar1=s1, scalar2=s2, op0=op0, op1=op1)

            def STT(eng, out, in0, scalar, in1, op0, op1, reads, writes):
                fw.op(eng, "scalar_tensor_tensor", reads, writes, out=out, in0=in0, scalar=scalar, in1=in1, op0=op0, op1=op1)

            def ACT(out, in_, func, reads, writes, **kw):
                fw.op("act", "activation", reads, writes, out=out, in_=in_, func=func, **kw)

            def MM(out, lhsT, rhs, reads, writes, start=True, stop=True):
                fw.op("pe", "matmul", reads, writes, out=out, lhsT=lhsT, rhs=rhs, start=start, stop=stop)

            if l == 0:
                for nm_ in ("abT", "rbT", "bbT", "kbT"):
                    scr(nm_, [2, D, NT], BF16)
                scr("Khat", [2, NT, D], BF16); scr("Bhat", [2, NT, D], BF16); scr("v_tm", [NT, D], BF16)
                scr("gdT", [128, NT], BF16); scr("yr", [2, NT, D], F32)
                scr("Tinv", [2, NC2, 128, 16, 128], BF16); scr("roT", [D, NT], BF16)
            with ExitStack() as se:
                plb = se.enter_context(SBT("plb", [128, 2, 8, NC2], F32))
                bon = se.enter_context(SBT("bon", [128, NC2, 16], F32))
                t_plb, t_bon = TU("plb"), TU("bon")
                with ExitStack() as se1:
                    def colload(name, src1d, ncol):
                        t = se1.enter_context(SBT(name, [128, ncol], F32))
                        tu_ = TU(name)
                        fw.dma("sp", t[:], src1d.rearrange("(j p) -> p j", p=128), writes=[tu_], allow_slow_non_contiguous=True)
                        return t, tu_
                    mu, t_mu = colload("mu_sb", I["mu_shift"].ap[l], 27)
                    a1 = se1.enter_context(SBT("a1_sb", [128, 27], F32))
                    TS("dve", a1[:], mu[:], -1.0, 1.0, ALU.mult, ALU.add, [t_mu], [t_mu])
                    kk_c, t_kkc = colload("kk_c", I["k_k"].ap[l], 8)
                    ka_c, t_kac = colload("ka_c", I["k_a"].ap[l], 8)
                    nka_c = se1.enter_context(SBT("nka_c", [128, 8], F32))
                    TS("dve", nka_c[:], ka_c[:], -1.0, None, ALU.mult, None, [t_kac], [t_kac])
                    rk_c, t_rkc = colload("rk_c", I["r_k"].ap[l], 8)
                    w0_c = [colload(f"w0_c{d}", I["w0"].ap[l, d], 8) for d in range(2)]
                    a0_c = [colload(f"a0_c{d}", I["a0"].ap[l, d], 8) for d in range(2)]
                    w2b = se1.enter_context(SBT("w2b", [128, D], BF16))
                    a2b = se1.enter_context(SBT("a2b", [128, D], BF16))
                    t_w2, t_a2 = TU("w2"), TU("a2")
                    with ExitStack() as se0:
                        w2f = se0.enter_context(SBT("w2f", [128, D], F32))
                        a2f = se0.enter_context(SBT("a2f", [128, D], F32))
                        fw.dma("sp", w2f[:], I["w2"].ap[l].rearrange("d k c -> (d k) c"), writes=[t_w2])
                        fw.dma("sp", a2f[:], I["a2"].ap[l].rearrange("d k c -> (d k) c"), writes=[t_a2])
                        fw.op("dve", "tensor_copy", [t_w2], [t_w2], out=w2b[:], in_=w2f[:])
                        fw.op("dve", "tensor_copy", [t_a2], [t_a2], out=a2b[:], in_=a2f[:])
                        fw.barrier()
                    rmask2 = se1.enter_context(SBT("rmask2", [128, NT // 2], F32))
                    t_rm2 = TU("rm2")
                    fw.op("pool", "memset", [], [t_rm2], ap=rmask2[:], constant=1.0)
                    fw.op("pool", "memset", [], [t_rm2], ap=rmask2[:].rearrange("p (c l) -> p c l", l=128)[:, :, 0:1], constant=0.0)
                    NP = NT // 2
                    Pt = se1.enter_context(SBT("Pt", [128, NT], BF16)); t_P = TU("Pt")
                    SH = se1.enter_context(SBT("SH", [128, NP], F32)); t_SH = TU("SH")
                    wdT = se1.enter_context(SBT("wdT", [128, NT], BF16)); t_wd = TU("wdT")
                    adT = se1.enter_context(SBT("adT", [128, NT], BF16)); t_ad = TU("adT")
                    XV = se1.enter_context(SBT("XV", [128, NP], BF16)); t_XV = TU("XV")
                    XR = se1.enter_context(SBT("XR", [128, NP], F32)); t_XR = TU("XR")
                    XK = se1.enter_context(SBT("XK", [128, NP], F32)); t_XK = TU("XK")
                    KK = se1.enter_context(SBT("KK", [128, NP], F32)); t_KK = TU("KK")
                    LW = se1.enter_context(SBT("LW", [128, NP], F32)); t_LW = TU("LW")
                    CL = se1.enter_context(SBT("CL", [128, NP], F32)); t_CL = TU("CL")
                    AA = se1.enter_context(SBT("AA", [128, NP], F32)); t_AA = TU("AA")
                    KT = se1.enter_context(SBT("KT", [128, NP], F32)); t_KT = TU("KT")
                    KS = se1.enter_context(SBT("KS", [128, NP], F32)); t_KS = TU("KS")
                    EE = se1.enter_context(SBT("EE", [128, NP], F32)); t_EE = TU("EE")
                    E2 = se1.enter_context(SBT("E2", [128, NP], F32)); t_E2 = TU("E2")
                    TOT = se1.enter_context(SBT("TOT", [128, NC2 // 2], F32)); t_TOT = TU("TOT")
                    OUT = Ring(nc, se1, "rout", [128, NP], BF16, 3)
                    RN = Ring(nc, se1, "rn", [128, 512], F32, 2)
                    TRB = Ring(nc, se1, "trb", [128, 4, 128], BF16, 3)

                    def shift_lerp(j, p0, dst, t_dst, eng="pool"):
                        rngs = []
                        a = 0
                        while a < 128:
                            qd = (j * 128 + a) // 864
                            b = min(128, (qd + 1) * 864 - j * 128)
                            while a < b:
                                mx = {0: 128, 32: 32, 64: 64, 96: 32}[a]
                                e_ = min(b, a + mx)
                                rngs.append((a, e_, qd))
                                a = e_
                        for (a, b, qd) in rngs:
                            muc = mu[a:b, j:j + 1]
                            if p0 == 0:
                                off = -1 if qd < 2 else 1
                                lo, hi = (1, NCTX) if off < 0 else (0, NCTX - 1)
                                TS(eng, SH[a:b, lo:hi], Pt[a:b, lo + off:hi + off], muc, None, ALU.mult, None, [t_P, t_mu], [t_SH])
                                z = 0 if off < 0 else NCTX - 1
                                fw.op(eng, "memset", [], [t_SH], ap=SH[a:b, z:z + 1], constant=0.0)
                            off = (-1, 1, -64, 64)[qd]
                            lo = max(p0, NCTX); hi = p0 + NP
                            if off > 0:
                                hi = min(hi, NT - off)
                            ACT(SH[a:b, lo - p0:hi - p0], Pt[a:b, lo + off:hi + off], AF.Copy, [t_P, t_mu], [t_SH], scale=muc)
                            l0 = max(p0, NCTX) - p0
                            lat = SH[a:b, l0:NP].rearrange("p (r c) -> p r c", c=64)
                            if qd == 0:
                                fw.op(eng, "memset", [], [t_SH], ap=lat[:, :, 0:1], constant=0.0)
                            elif qd == 1:
                                fw.op(eng, "memset", [], [t_SH], ap=lat[:, :, 63:64], constant=0.0)
                            elif qd == 2 and p0 == 0:
                                fw.op(eng, "memset", [], [t_SH], ap=SH[a:b, l0:l0 + 64], constant=0.0)
                            elif qd == 3 and p0 + NP == NT:
                                fw.op(eng, "memset", [], [t_SH], ap=SH[a:b, NP - 64:NP], constant=0.0)
                        STT("dve", dst, Pt[:, p0:p0 + NP], a1[:, j:j + 1], SH[:], ALU.mult, ALU.add, [t_P, t_SH, t_mu], [t_dst])

                    def transpose_store(src, t_src, dram_ap_fn, tus_fn, p0):
                        for g in range(0, NP // 128, 4):
                            ng = min(4, NP // 128 - g)
                            ps, t_ps = PS.next()
                            for jj in range(ng):
                                MM(ps[:, jj * 128:(jj + 1) * 128], src[:, (g + jj) * 128:(g + jj + 1) * 128], ident_b, [t_src, t_cst], [t_ps])
                            tb, t_tb = TRB.next()
                            ACT(tb[:, 0:ng, :], ps[:, 0:ng * 128].rearrange("p (j c) -> p j c", c=128), AF.Copy, [t_ps], [t_tb])
                            for jj in range(ng):
                                t0 = p0 + (g + jj) * 128
                                fw.dma("pool", dram_ap_fn(t0), tb[:, jj, :], reads=[t_tb], writes=tus_fn(t0))

                    for j, (dstT, t_d, func) in ((24, (wdT, t_wd, AF.Tanh)), (25, (adT, t_ad, AF.Copy)), (26, (None, None, AF.Sigmoid))):
                        fw.dma("sp", Pt[:], S["pT"].ap[j * 128:(j + 1) * 128, :], reads=[x for r0 in range(3072, 3456, 128) for x in S["pT"].tu(key=r0)], writes=[t_P])
                        for p0 in (0, NP):
                            shift_lerp(j, p0, EE[:], t_EE)
                            if dstT is not None:
                                ACT(dstT[:, p0:p0 + NP], EE[:], func, [t_EE], [t_d])
                            else:
                                ob, t_ob = OUT.next()
                                ACT(ob[:], EE[:], func, [t_EE], [t_ob])
                                fw.dma("pool", S["gdT"].ap[:, p0:p0 + NP], ob[:], reads=[t_ob], writes=S["gdT"].tu(p0, NP))
                    for cb in range(8):
                        for p0 in (0, NP):
                            pi = p0 // NP
                            ntile = [(q0, min(512, NP - q0)) for q0 in range(0, NP, 512)]
                            fw.dma("sp", Pt[:], S["pT"].ap[(16 + cb) * 128:(17 + cb) * 128, :], reads=[x for r0 in range(0, 3456, 128) for x in S["pT"].tu(key=r0)], writes=[t_P])
                            shift_lerp(16 + cb, p0, XV[:], t_XV)
                            transpose_store(XV, t_XV, lambda t0, cb=cb: S["v_tm"].ap[t0:t0 + 128, cb * 128:(cb + 1) * 128], lambda t0, cb=cb: S["v_tm"].tu(t0, 128, key=cb), p0)
                            fw.dma("sp", Pt[:], S["pT"].ap[cb * 128:(cb + 1) * 128, :], reads=S["pT"].tu(key=0), writes=[t_P])
                            shift_lerp(cb, p0, XR[:], t_XR)
                            fw.dma("sp", Pt[:], S["pT"].ap[(8 + cb) * 128:(9 + cb) * 128, :], reads=S["pT"].tu(key=0), writes=[t_P])
                            shift_lerp(8 + cb, p0, XK[:], t_XK)
                            ACT(KK[:], XK[:], AF.Copy, [t_XK, t_kkc], [t_KK], scale=kk_c[:, cb:cb + 1])
                            ob, t_ob = OUT.next()
                            ACT(ob[:], KK[:], AF.Square, [t_KK], [t_ob])
                            for (q0, qn) in ntile:
                                ps, t_ps = PS.next()
                                MM(ps[:, :qn], blk1_b, ob[:, q0:q0 + qn], [t_ob, t_cst], [t_ps])
                                rn, t_rn = RN.next()
                                ACT(rn[:, :qn], ps[:, :qn], AF.Sqrt, [t_ps], [t_rn])
                                TS("dve", rn[:, :qn], rn[:, :qn], 1e-12, None, ALU.max, None, [t_rn], [t_rn])
                                fw.op("dve", "reciprocal", [t_rn], [t_rn], out=rn[:, :qn], in_=rn[:, :qn])
                                TT("dve", KK[:, q0:q0 + qn], KK[:, q0:q0 + qn], rn[:, :qn], ALU.mult, [t_KK, t_rn], [t_KK])
                            for d in range(2):
                                w0c, t_w0c = w0_c[d]; a0c, t_a0c = a0_c[d]
                                for (q0, qn) in ntile:
                                    ps, t_ps = PS.next()
                                    MM(ps[:, :qn], w2b[d * 64:(d + 1) * 64, cb * 128:(cb + 1) * 128], wdT[d * 64:(d + 1) * 64, p0 + q0:p0 + q0 + qn], [t_w2, t_wd], [t_ps])
                                    ACT(LW[:, q0:q0 + qn], ps[:, :qn], AF.Sigmoid, [t_ps, t_w0c], [t_LW], bias=w0c[:, cb:cb + 1])
                                    ps, t_ps = PS.next()
                                    MM(ps[:, :qn], a2b[d * 64:(d + 1) * 64, cb * 128:(cb + 1) * 128], adT[d * 64:(d + 1) * 64, p0 + q0:p0 + q0 + qn], [t_a2, t_ad], [t_ps])
                                    ACT(AA[:, q0:q0 + qn], ps[:, :qn], AF.Sigmoid, [t_ps, t_a0c], [t_AA], bias=a0c[:, cb:cb + 1])
                                fw.op("dve", "tensor_tensor_scan", [t_rm2, t_LW], [t_CL], out=CL[:], data0=rmask2[:], data1=LW[:], initial=0.0, op0=ALU.mult, op1=ALU.add)
                                cl3 = CL[:].rearrange("p (c l) -> p c l", l=128)
                                fw.op("dve", "tensor_copy", [t_CL], [t_TOT], out=TOT[:].unsqueeze(2), in_=cl3[:, :, 127:128])
                                totb = TOT[:].unsqueeze(2).to_broadcast([128, NC2 // 2, 128])
                                if d == 1:
                                    TT("dve", CL[:], LW[:], CL[:], ALU.subtract, [t_LW, t_CL], [t_CL])
                                    TT("dve", cl3, cl3, totb, ALU.add, [t_CL, t_TOT], [t_CL])
                                ACT(plb[:, d, cb, pi * (NC2 // 2):(pi + 1) * (NC2 // 2)], TOT[:], AF.Exp, [t_TOT], [t_plb], scale=-LAM)
                                ACT(EE[:], AA[:], AF.Identity, [t_AA, t_kac], [t_EE], scale=ka_c[:, cb:cb + 1], bias=nka_c[:, cb:cb + 1])
                                STT("dve", KT[:], EE[:], 1.0, XK[:], ALU.add, ALU.mult, [t_EE, t_XK], [t_KT])
                                if d == 0:
                                    ACT(KS[:], KT[:], AF.Copy, [t_KT], [t_KS])
                                else:
                                    TT("pool", KS[:], KS[:], KT[:], ALU.add, [t_KT, t_KS], [t_KS])
                                TT("dve", EE[:], CL[:], LW[:], ALU.subtract, [t_CL, t_LW], [t_EE])
                                ACT(EE[:], EE[:], AF.Exp, [t_EE], [t_EE], scale=-LAM)
                                ob, t_ob = OUT.next()
                                TT("dve", ob[:], KK[:], EE[:], ALU.mult, [t_KK, t_EE], [t_ob])
                                fw.dma("pool", S["abT"].ap[d, cb * 128:(cb + 1) * 128, p0:p0 + NP], ob[:], reads=[t_ob], writes=S["abT"].tu(p0, NP, key=(d, cb)))
                                ACT(EE[:], CL[:], AF.Exp, [t_CL], [t_EE], scale=-LAM)
                                ob, t_ob = OUT.next()
                                TT("dve", ob[:], XR[:], EE[:], ALU.mult, [t_XR, t_EE], [t_ob])
                                fw.dma("pool", S["rbT"].ap[d, cb * 128:(cb + 1) * 128, p0:p0 + NP], ob[:], reads=[t_ob], writes=S["rbT"].tu(p0, NP, key=(d, cb)))
                                TT("pool", LW[:], KK[:], AA[:], ALU.mult, [t_KK, t_AA], [t_LW])
                                ACT(EE[:], CL[:], AF.Exp, [t_CL], [t_EE], scale=LAM)
                                ob, t_ob = OUT.next()
                                TT("dve", ob[:], LW[:], EE[:], ALU.mult, [t_LW, t_EE], [t_ob])
                                fw.dma("pool", S["bbT"].ap[d, cb * 128:(cb + 1) * 128, p0:p0 + NP], ob[:], reads=[t_ob], writes=S["bbT"].tu(p0, NP, key=(d, cb)))
                                transpose_store(ob, t_ob, lambda t0, cb=cb, d=d: S["Bhat"].ap[d, t0:t0 + 128, cb * 128:(cb + 1) * 128], lambda t0, cb=cb, d=d: S["Bhat"].tu(t0, 128, key=(d, cb)), p0)
                                ob, t_ob = OUT.next()
                                TT("dve", ob[:], KT[:], EE[:], ALU.mult, [t_KT, t_EE], [t_ob])
                                fw.dma("pool", S["kbT"].ap[d, cb * 128:(cb + 1) * 128, p0:p0 + NP], ob[:], reads=[t_ob], writes=S["kbT"].tu(p0, NP, key=(d, cb)))
                                transpose_store(ob, t_ob, lambda t0, cb=cb, d=d: S["Khat"].ap[d, t0:t0 + 128, cb * 128:(cb + 1) * 128], lambda t0, cb=cb, d=d: S["Khat"].tu(t0, 128, key=(d, cb)), p0)
                            STT("dve", EE[:], XR[:], rk_c[:, cb:cb + 1], KS[:], ALU.mult, ALU.mult, [t_XR, t_KS, t_rkc], [t_EE])
                            ps, t_ps = PS.next()
                            for tt in range(NP // 128):
                                MM(ps[:, 2 * tt:2 * tt + 2], EE[:, tt * 128:(tt + 1) * 128], sel2_f, [t_EE, t_cst], [t_ps])
                            fw.op("dve", "tensor_copy", [t_ps], [t_bon], out=bon[:, pi * (NP // 128):(pi + 1) * (NP // 128), 2 * cb:2 * cb + 2], in_=ps[:, 0:2 * (NP // 128)].rearrange("p (t h) -> p t h", h=2))
                    fw.barrier()
                if stop == "E1":
                    for nm_, tl_, tt_ in (("plb", plb, t_plb), ("bon", bon, t_bon)):
                        if nm_ in dbg:
                            fw.dma("sp", dbg[nm_].ap, tl_[:].rearrange("p a b c -> p (a b c)") if nm_ == "plb" else tl_[:].rearrange("p a b -> p (a b)"), reads=[tt_], writes=dbg[nm_].tu())
                    break
                with ExitStack() as se2:
                    AB = Ring(nc, se2, "e2ab", [128, 8, 128], BF16, 2)
                    BB = Ring(nc, se2, "e2bb", [128, 8, 128], BF16, 2)
                    YP = Ring(nc, se2, "e2yp", [128, 2, 256], BF16, 12)
                    YT = Ring(nc, se2, "e2yt", [128, 2, 128], BF16, 12)
                    TO = Ring(nc, se2, "e2to", [128, 16, 128], BF16, 2)
                    bank = lambda b: (psall[:, b * 512:(b + 1) * 512], PS.tus[b])
                    evi2 = 0
                    for d in range(2):
                        mk_s = m_lt if d == 0 else m_gt
                        mk_t = m_gt if d == 0 else m_lt
                        for c in range(NC2):
                            t0 = c * 128
                            ab, t_ab = AB.next(); bb, t_bb = BB.next(); to, t_to = TO.next()
                            fw.dma("sp", ab[:], S["abT"].ap[d, :, t0:t0 + 128].rearrange("(cb p) t -> p cb t", p=128), reads=[x for cb_ in range(8) for x in S["abT"].tu(t0, 128, key=(d, cb_))], writes=[t_ab])
                            fw.dma("sp", bb[:], S["bbT"].ap[d, :, t0:t0 + 128].rearrange("(cb p) t -> p cb t", p=128), reads=[x for cb_ in range(8) for x in S["bbT"].tu(t0, 128, key=(d, cb_))], writes=[t_bb])
                            for g in range(2):
                                chains = []
                                for k in range(4):
                                    cb = 4 * g + k
                                    yp, t_yp = YP.next(); yt, t_yt = YT.next()
                                    ps, t_ps = PS.pair(2 * k)
                                    for hh in range(2):
                                        pb = hh * 64
                                        MM(ps[:, hh, 0:128], bb[pb:pb + 64, cb, :], ab[pb:pb + 64, cb, :], [t_ab, t_bb], [t_ps[hh]])
                                        MM(ps[:, hh, 128:256], ab[pb:pb + 64, cb, :], bb[pb:pb + 64, cb, :], [t_ab, t_bb], [t_ps[hh]])
                                    STT("dve", yp[:, :, 0:128], ps[:, :, 0:128], -1.0, mk_s.unsqueeze(1).to_broadcast([128, 2, 128]), ALU.mult, ALU.mult, t_ps + [t_cst], [t_yp])
                                    STT("dve", yt[:], ps[:, :, 128:256], -1.0, mk_t.unsqueeze(1).to_broadcast([128, 2, 128]), ALU.mult, ALU.mult, t_ps + [t_cst], [t_yt])
                                    chains.append([cb, yp, t_yp, yt, t_yt])
                                for lev in range(7):
                                    last = lev == 6
                                    nxt = []
                                    for k in range(4):
                                        cb, yp, t_yp, yt, t_yt = chains[k]
                                        pa, t_pa = bank(2 * k)
                                        pbb, t_pbb = bank(2 * k + 1)
                                        for hh in range(2):
                                            if lev == 0:
                                                MM(pa[:, hh * 256:hh * 256 + 128], yt[:, hh, :], yp[:, hh, 0:128], [t_yt, t_yp], [t_pa])
                                                MM(pa[:, hh * 256 + 128:(hh + 1) * 256], yt[:, hh, :], ident_b, [t_yt, t_cst], [t_pa])
                                                MM(pbb[:, hh * 128:(hh + 1) * 128], yp[:, hh, 0:128], yt[:, hh, :], [t_yt, t_yp], [t_pbb])
                                            elif not last:
                                                MM(pa[:, hh * 256:(hh + 1) * 256], yt[:, hh, :], yp[:, hh, :], [t_yt, t_yp], [t_pa])
                                                MM(pbb[:, hh * 128:(hh + 1) * 128], yp[:, hh, 0:128], yt[:, hh, :], [t_yt, t_yp], [t_pbb])
                                            else:
                                                MM(pa[:, hh * 256 + 128:(hh + 1) * 256], yt[:, hh, :], yp[:, hh, 128:256], [t_yt, t_yp], [t_pa])
                                    for k in range(4):
                                        cb, yp, t_yp, yt, t_yt = chains[k]
                                        pa, t_pa = bank(2 * k)
                                        pbb, t_pbb = bank(2 * k + 1)
                                        pav = pa.rearrange("p (h x) -> p h x", x=256)
                                        if not last:
                                            ypn, t_ypn = YP.next(); ytn, t_ytn = YT.next()
                                            ACT(ypn[:, :, 0:128], pav[:, :, 0:128], AF.Copy, [t_pa], [t_ypn])
                                            if lev == 0:
                                                TT("dve", ypn[:, :, 128:256], pav[:, :, 128:256], ident_b.unsqueeze(1).to_broadcast([128, 2, 128]), ALU.add, [t_pa, t_cst], [t_ypn])
                                            else:
                                                TT("dve", ypn[:, :, 128:256], pav[:, :, 128:256], yp[:, :, 128:256], ALU.add, [t_pa, t_yp], [t_ypn])
                                            evi2 += 1
                                            if evi2 % 2 == 0:
                                                ACT(ytn[:], pbb[:, 0:256].rearrange("p (h x) -> p h x", x=128), AF.Copy, [t_pbb], [t_ytn])
                                            else:
                                                fw.op("dve", "tensor_copy", [t_pbb], [t_ytn], out=ytn[:], in_=pbb[:, 0:256].rearrange("p (h x) -> p h x", x=128))
                                            chains[k] = [cb, ypn, t_ypn, ytn, t_ytn]
                                        else:
                                            TT("dve", to[:, 2 * cb:2 * cb + 2, :], pav[:, :, 128:256], yp[:, :, 128:256], ALU.add, [t_pa, t_yp], [t_to])
                            fw.dma("pool", S["Tinv"].ap[d, c], to[:], reads=[t_to], writes=S["Tinv"].tu(t0, 128, key=d))
                    fw.barrier()
                if stop == "E2":
                    break
                with ExitStack() as se3:
                    AR = Ring(nc, se3, "e3ar", [128, 8, 2, 128], BF16, 3)
                    BB3 = Ring(nc, se3, "e3bb", [128, 8, 128], BF16, 3)
                    KB3 = Ring(nc, se3, "e3kb", [128, 8, 128], BF16, 3)
                    V3 = Ring(nc, se3, "e3v", [128, D], BF16, 3)
                    KH3 = Ring(nc, se3, "e3kh", [128, D], BF16, 3)
                    BH3 = Ring(nc, se3, "e3bh", [128, D], BF16, 3)
                    TI3 = Ring(nc, se3, "e3ti", [128, 16, 128], BF16, 3)
                    AM = Ring(nc, se3, "e3am", [128, 384], BF16, 8)
                    WB3 = Ring(nc, se3, "e3w", [128, 64], BF16, 8)
                    UN3 = Ring(nc, se3, "e3u", [128, 64], BF16, 8)
                    YO3 = Ring(nc, se3, "e3yo", [128, D], F32, 3)
                    orders2 = [list(range(NC2)), [1, 0] + list(range(NC2 - 1, 1, -1))]
                    bank = lambda b: (psall[:, b * 512:(b + 1) * 512], PS.tus[b])
                    st3 = []
                    for d in range(2):
                        S32 = se3.enter_context(SBT(f"S32_{d}", [128, 8, 64], F32))
                        Sb = se3.enter_context(SBT(f"Sb_{d}", [128, 8, 64], BF16))
                        t_S = [TU(f"S32_{cb}") for cb in range(8)]
                        t_Sb = [TU(f"Sb_{cb}") for cb in range(8)]
                        fw.op("pool", "memset", [], t_S, ap=S32[:], constant=0.0)
                        fw.op("pool", "memset", [], t_Sb, ap=Sb[:], constant=0.0)
                        st3.append((S32, Sb, t_S, t_Sb))
                    psa_i = 0
                    STMP = [(se3.enter_context(SBT(f"stmp{d_}", [128, 8, 64], F32)), TU(f"stmp{d_}")) for d_ in range(2)]
                    for i in range(NC2):
                        cx = []
                        for d in range(2):
                            c = orders2[d][i]
                            t0 = c * 128
                            ar, t_ar = AR.next(); bb, t_bb = BB3.next(); kb, t_kb = KB3.next(); vv, t_vv = V3.next(); kh, t_kh = KH3.next(); bh, t_bh = BH3.next(); ti, t_ti = TI3.next()
                            rd = lambda nm: [x for cb_ in range(8) for x in S[nm].tu(t0, 128, key=(d, cb_))]
                            fw.dma("sp", ar[:, :, 0, :], S["abT"].ap[d, :, t0:t0 + 128].rearrange("(cb p) t -> p cb t", p=128), reads=rd("abT"), writes=[t_ar])
                            fw.dma("sp", ar[:, :, 1, :], S["rbT"].ap[d, :, t0:t0 + 128].rearrange("(cb p) t -> p cb t", p=128), reads=rd("rbT"), writes=[t_ar])
                            fw.dma("sp", bb[:], S["bbT"].ap[d, :, t0:t0 + 128].rearrange("(cb p) t -> p cb t", p=128), reads=rd("bbT"), writes=[t_bb])
                            fw.dma("sp", kb[:], S["kbT"].ap[d, :, t0:t0 + 128].rearrange("(cb p) t -> p cb t", p=128), reads=rd("kbT"), writes=[t_kb])
                            fw.dma("sp", vv[:], S["v_tm"].ap[t0:t0 + 128, :], reads=[x for cb_ in range(8) for x in S["v_tm"].tu(t0, 128, key=cb_)], writes=[t_vv])
                            fw.dma("sp", kh[:], S["Khat"].ap[d, t0:t0 + 128, :], reads=rd("Khat"), writes=[t_kh])
                            fw.dma("sp", bh[:], S["Bhat"].ap[d, t0:t0 + 128, :], reads=rd("Bhat"), writes=[t_bh])
                            fw.dma("sp", ti[:], S["Tinv"].ap[d, c], reads=S["Tinv"].tu(t0, 128, key=d), writes=[t_ti])
                            yo, t_yo = YO3.next()
                            stmp, t_stmp = STMP[d]
                            S32_, _, t_S_, _ = st3[d]
                            fw.op("pool", "tensor_tensor", t_S_ + [t_plb], [t_stmp], out=stmp[:], in0=S32_[:], in1=plb[:, d, :, c:c + 1].to_broadcast([128, 8, 64]), op=ALU.mult)
                            cx.append(dict(d=d, c=c, t0=t0, stmp=stmp, t_stmp=t_stmp, ar=ar, t_ar=t_ar, bb=bb, t_bb=t_bb, kb=kb, t_kb=t_kb, vv=vv, t_vv=t_vv, kh=kh, t_kh=t_kh, bh=bh, t_bh=t_bh, ti=ti, t_ti=t_ti, yo=yo, t_yo=t_yo,
                                           mk_st=(m_lt if d == 0 else m_gt), mk_in=(m_le if d == 0 else m_ge), ams={}))

                        def front(x, hd_):
                            nonlocal_i = [0]
                            cb_, hh_ = hd_ // 2, hd_ % 2
                            pb_ = hh_ * 64
                            psa, t_psa = bank(6 + (front.k % 2))
                            front.k += 1
                            MM(psa[:, 0:256], x["kb"][pb_:pb_ + 64, cb_, :], x["ar"][pb_:pb_ + 64, cb_, :, :].rearrange("p a t -> p (a t)"), [x["t_kb"], x["t_ar"]], [t_psa])
                            MM(psa[:, 256:384], x["bb"][pb_:pb_ + 64, cb_, :], x["ar"][pb_:pb_ + 64, cb_, 1, :], [x["t_bb"], x["t_ar"]], [t_psa])
                            am_, t_am_ = AM.next()
                            TT("dve", am_[:, 0:128], psa[:, 0:128], x["mk_st"], ALU.mult, [t_psa, t_cst], [t_am_])
                            TT("dve", am_[:, 128:384].rearrange("p (a t) -> p a t", t=128), psa[:, 128:384].rearrange("p (a t) -> p a t", t=128), x["mk_in"].unsqueeze(1).to_broadcast([128, 2, 128]), ALU.mult, [t_psa, t_cst], [t_am_])
                            x["ams"][hd_] = (am_, t_am_)
                        front.k = 0
                        for x in cx:
                            front(x, 0)
                        for cb in range(8):
                            for hh in range(2):
                                hd = 2 * cb + hh
                                pb = hh * 64
                                hs = slice(hd * 64, (hd + 1) * 64)
                                if hd + 1 < 16:
                                    for x in cx:
                                        front(x, hd + 1)
                                loc = []
                                for x in cx:
                                    d = x["d"]
                                    S32, Sb, t_S, t_Sb = st3[d]
                                    am, t_am = x["ams"][hd]
                                    psw, t_psw = bank(2 * d + (hd % 2))
                                    MM(psw[:, 0:64], am[:, 0:128], x["vv"][:, hs], [t_am, x["t_vv"]], [t_psw], start=True, stop=False)
                                    MM(psw[:, 0:64], x["ar"][pb:pb + 64, cb, 0, :], Sb[pb:pb + 64, cb, :], [x["t_ar"], t_Sb[cb]], [t_psw], start=False, stop=True)
                                    loc.append((x, d, S32, Sb, t_S, t_Sb, am, t_am, psw, t_psw))
                                wbs = []
                                for (x, d, S32, Sb, t_S, t_Sb, am, t_am, psw, t_psw) in loc:
                                    wb_, t_wb_ = WB3.next()
                                    ACT(wb_[:], psw[:, 0:64], AF.Copy, [t_psw], [t_wb_])
                                    wbs.append((wb_, t_wb_))
                                for k_, (x, d, S32, Sb, t_S, t_Sb, am, t_am, psw, t_psw) in enumerate(loc):
                                    MM(psw[:, 64:128], x["ti"][:, hd, :], wbs[k_][0][:], [x["t_ti"], wbs[k_][1]], [t_psw])
                                uns = []
                                for (x, d, S32, Sb, t_S, t_Sb, am, t_am, psw, t_psw) in loc:
                                    un, t_un = UN3.next()
                                    ACT(un[:], psw[:, 64:128], AF.Copy, [t_psw], [t_un], scale=-1.0)
                                    uns.append((un, t_un))
                                for k_, (x, d, S32, Sb, t_S, t_Sb, am, t_am, psw, t_psw) in enumerate(loc):
                                    un, t_un = uns[k_]
                                    psS, t_psS = bank(4 + d)
                                    MM(psw[:, 128:192], am[:, 128:256], x["vv"][:, hs], [t_am, x["t_vv"]], [t_psw], start=True, stop=False)
                                    MM(psw[:, 128:192], am[:, 256:384], un[:], [t_am, t_un], [t_psw], start=False, stop=False)
                                    MM(psw[:, 128:192], x["ar"][pb:pb + 64, cb, 1, :], Sb[pb:pb + 64, cb, :], [x["t_ar"], t_Sb[cb]], [t_psw], start=False, stop=True)
                                    MM(psS[pb:pb + 64, 0:64], x["kh"][:, hs], x["vv"][:, hs], [x["t_kh"], x["t_vv"]], [t_psS], start=True, stop=False)
                                    MM(psS[pb:pb + 64, 0:64], x["bh"][:, hs], un[:], [x["t_bh"], t_un], [t_psS], start=False, stop=True)
                                for (x, d, S32, Sb, t_S, t_Sb, am, t_am, psw, t_psw) in loc:
                                    ACT(x["yo"][:, hs], psw[:, 128:192], AF.Copy, [t_psw], [x["t_yo"]])
                                if hh == 1:
                                    for (x, d, S32, Sb, t_S, t_Sb, am, t_am, psw, t_psw) in loc:
                                        psS, t_psS = bank(4 + d)
                                        c = x["c"]
                                        STT("dve", S32[:, cb, :], psS[:, 0:64], plb[:, d, cb, c:c + 1], x["stmp"][:, cb, :], ALU.mult, ALU.add, [t_S[cb], t_psS, t_plb, x["t_stmp"]], [t_S[cb]])
                                        ACT(Sb[:, cb, :], S32[:, cb, :], AF.Copy, [t_S[cb]], [t_Sb[cb]])
                        for x in cx:
                            fw.dma("pool", S["yr"].ap[x["d"], x["t0"]:x["t0"] + 128, :], x["yo"][:], reads=[x["t_yo"]], writes=S["yr"].tu(x["t0"], 128, key=x["d"]))
                    fw.barrier()
                if stop == "E3":
                    break
                with ExitStack() as se4:
                    def bc_load(name, src1d):
                        t = se4.enter_context(SBT(name, [128, D], F32))
                        tu_ = TU(name)
                        fw.dma("sp", t[:], src1d.partition_broadcast(128), writes=[tu_])
                        return t, tu_
                    lnw, t_lnw = bc_load("lnw_bc", I["ln_w"].ap[l])
                    lnb, t_lnb = bc_load("lnb_bc", I["ln_b"].ap[l])
                    g2b = se4.enter_context(SBT("g2b", [128, D], BF16)); t_g2 = TU("g2b")
                    with ExitStack() as se40:
                        g2f = se40.enter_context(SBT("g2f", [128, D], F32))
                        fw.dma("sp", g2f[:], I["g2"].ap[l], writes=[t_g2])
                        fw.op("dve", "tensor_copy", [t_g2], [t_g2], out=g2b[:], in_=g2f[:])
                        fw.barrier()
                    YF = Ring(nc, se4, "yf", [128, D], F32, 2)
                    YB = Ring(nc, se4, "yb", [128, D], F32, 2)
                    VT = Ring(nc, se4, "vt4", [128, D], BF16, 2)
                    GD = Ring(nc, se4, "gd4", [128, 128], BF16, 2)
                    SQ4 = Ring(nc, se4, "sq4", [128, D], F32, 1)
                    ST4 = Ring(nc, se4, "st4", [128, 2, 16], F32, 2)
                    RO = Ring(nc, se4, "ro4", [128, D], BF16, 2)
                    RT = Ring(nc, se4, "rt4", [128, 8, 128], BF16, 2)
                    for tt in range(NC2):
                        t0 = tt * 128
                        yf, t_yf = YF.next(); yb, t_yb = YB.next(); vt, t_vt = VT.next(); gd, t_gd = GD.next(); sq, t_sq = SQ4.next(); st4, t_st = ST4.next(); ro, t_ro = RO.next(); rt, t_rt = RT.next()
                        fw.dma("sp", yf[:], S["yr"].ap[0, t0:t0 + 128, :], reads=S["yr"].tu(t0, 128, key=0), writes=[t_yf])
                        fw.dma("sp", yb[:], S["yr"].ap[1, t0:t0 + 128, :], reads=S["yr"].tu(t0, 128, key=1), writes=[t_yb])
                        fw.dma("sp", vt[:], S["v_tm"].ap[t0:t0 + 128, :], reads=[x for cb_ in range(8) for x in S["v_tm"].tu(t0, 128, key=cb_)], writes=[t_vt])
                        fw.dma("sp", gd[:], S["gdT"].ap[:, t0:t0 + 128], reads=S["gdT"].tu(t0, 128), writes=[t_gd])
                        TT("dve", yf[:], yf[:], yb[:], ALU.add, [t_yf, t_yb], [t_yf])
                        y3 = yf[:].rearrange("p (h v) -> p h v", v=64)
                        fw.op("dve", "tensor_reduce", [t_yf], [t_st], out=st4[:, 0, :], in_=y3, axis=AX.X, op=ALU.add)
                        TS("dve", st4[:, 0, :], st4[:, 0, :], 1.0 / 64, None, ALU.mult, None, [t_st], [t_st])
                        TT("dve", y3, y3, st4[:, 0, :].unsqueeze(2).to_broadcast([128, 16, 64]), ALU.subtract, [t_yf, t_st], [t_yf])
                        ACT(sq[:], yf[:], AF.Square, [t_yf], [t_sq])
                        fw.op("dve", "tensor_reduce", [t_sq], [t_st], out=st4[:, 1, :], in_=sq[:].rearrange("p (h v) -> p h v", v=64), axis=AX.X, op=ALU.add)
                        ACT(st4[:, 1, :], st4[:, 1, :], AF.Sqrt, [t_st], [t_st], scale=1.0 / 64, bias=64e-5)
                        fw.op("dve", "reciprocal", [t_st], [t_st], out=st4[:, 1, :], in_=st4[:, 1, :])
                        TT("dve", y3, y3, st4[:, 1, :].unsqueeze(2).to_broadcast([128, 16, 64]), ALU.mult, [t_yf, t_st], [t_yf])
                        TT("pool", yf[:], yf[:], lnw[:], ALU.mult, [t_yf, t_lnw], [t_yf])
                        TT("pool", yf[:], yf[:], lnb[:], ALU.add, [t_yf, t_lnb], [t_yf])
                        TT("dve", sq[:].rearrange("p (h v) -> p h v", v=64), vt[:].rearrange("p (h v) -> p h v", v=64), bon[:, tt, :].unsqueeze(2).to_broadcast([128, 16, 64]), ALU.mult, [t_vt, t_bon], [t_sq])
                        TT("dve", yf[:], yf[:], sq[:], ALU.add, [t_yf, t_sq], [t_yf])
                        pg, t_pg = PS.next2()
                        for hf_ in range(2):
                            MM(pg[:, hf_, :], gd[:], g2b[:, hf_ * 512:(hf_ + 1) * 512], [t_gd, t_g2], [t_pg[hf_]])
                        TT("dve", ro[:].rearrange("p (a x) -> p a x", x=512), yf[:].rearrange("p (a x) -> p a x", x=512), pg, ALU.mult, [t_yf] + t_pg, [t_ro])
                        if "rotm" in dbg:
                            fw.dma("pool", dbg["rotm"].ap[t0:t0 + 128, :], ro[:], reads=[t_ro], writes=dbg["rotm"].tu(t0, 128))
                        for hb2 in range(2):
                            ps, t_ps = PS.next()
                            for j in range(4):
                                cb = hb2 * 4 + j
                                MM(ps[:, j * 128:(j + 1) * 128], ro[:, cb * 128:(cb + 1) * 128], ident_b, [t_ro, t_cst], [t_ps])
                            ACT(rt[:, hb2 * 4:(hb2 + 1) * 4, :], ps[:].rearrange("p (c t) -> p c t", t=128), AF.Copy, [t_ps], [t_rt])
                        fw.dma("pool", S["roT"].ap[:, t0:t0 + 128].rearrange("(c p) t -> p c t", p=128), rt[:], reads=[t_rt], writes=S["roT"].tu(t0, 128))
                    fw.barrier()
            if stop == "E4":
                break
            with ExitStack() as sf:
                wts = {}
                for nm_ in ("w_proj_mlstm", "w_proj_rwkv", "w_out"):
                    wts[nm_] = (sf.enter_context(SBT("f_" + nm_, [128, 8, D], BF16)), TU(nm_))
                with ExitStack() as sf0:
                    WST = Ring(nc, sf0, "fwst", [128, 8, 512], F32, 2)
                    for nm_ in ("w_proj_mlstm", "w_proj_rwkv", "w_out"):
                        for hf_ in range(2):
                            wst, t_wst = WST.next()
                            fw.dma("sp", wst[:], I[nm_].ap[l, :, hf_ * 512:(hf_ + 1) * 512].rearrange("(c p) m -> p c m", p=128), writes=[t_wst])
                            if hf_ == 0:
                                fw.op("dve", "tensor_copy", [t_wst], [wts[nm_][1]], out=wts[nm_][0][:, :, hf_ * 512:(hf_ + 1) * 512], in_=wst[:])
                            else:
                                ACT(wts[nm_][0][:, :, hf_ * 512:(hf_ + 1) * 512], wst[:], AF.Copy, [t_wst], [wts[nm_][1]])
                    fw.barrier()
                MO_ = Ring(nc, sf, "f_mo", [128, 8, 512], BF16, 2)
                RO_ = Ring(nc, sf, "f_ro", [128, 8, 512], BF16, 2)
                GM_ = Ring(nc, sf, "f_gm", [128, 8, 512], BF16, 1)
                GR_ = Ring(nc, sf, "f_gr", [128, 8, 512], BF16, 1)
                ZT_ = Ring(nc, sf, "f_zt", [128, 512], F32, 2)
                ZB_ = Ring(nc, sf, "f_zb", [128, 8, 512], BF16, 1)
                XT_ = Ring(nc, sf, "f_xt", [128, 8, 512], F32, 1)
                for ti, (t0, n) in enumerate(TILES):
                    if l == DEPTH - 1 and t0 < NCTX:
                        continue
                    j = 1 if t0 < NCTX else 0
                    mo_, t_mo_ = MO_.next(); ro_, t_ro_ = RO_.next(); gm_, t_gm_ = GM_.next(); gr_, t_gr_ = GR_.next(); zb_, t_zb_ = ZB_.next(); xt_, t_xt_ = XT_.next()
                    fw.dma("sp", mo_[:, :, :n], S["moT"].ap[:, t0:t0 + n].rearrange("(c p) t -> p c t", p=128), reads=S["moT"].tu(t0, n), writes=[t_mo_])
                    fw.dma("sp", ro_[:, :, :n], S["roT"].ap[:, t0:t0 + n].rearrange("(c p) t -> p c t", p=128), reads=S["roT"].tu(t0, n), writes=[t_ro_])
                    fw.dma("sp", gm_[:, :, :n], S["mgT"].ap[0:D, t0:t0 + n].rearrange("(c p) t -> p c t", p=128), reads=[x for r0 in range(0, 2048, 128) for x in S["mgT"].tu(t0, n, key=r0)], writes=[t_gm_])
                    fw.dma("sp", gr_[:, :, :n], S["mgT"].ap[D:2 * D, t0:t0 + n].rearrange("(c p) t -> p c t", p=128), reads=[x for r0 in range(0, 2048, 128) for x in S["mgT"].tu(t0, n, key=r0)], writes=[t_gr_])
                    fw.dma("sp", xt_[:, :, :n], xsrc.ap[:, t0:t0 + n].rearrange("(c p) t -> p c t", p=128), reads=xsrc.tu(t0, n), writes=[t_xt_])
                    for m in range(8):
                        pm, t_pm = PS.next(); pr, t_pr = PS.next()
                        for c in range(8):
                            MM(pm[:, :n], wts["w_proj_mlstm"][0][:, c, m * 128:(m + 1) * 128], mo_[:, c, :n], [wts["w_proj_mlstm"][1], t_mo_], [t_pm], start=(c == 0), stop=(c == 7))
                        for c in range(8):
                            MM(pr[:, :n], wts["w_proj_rwkv"][0][:, c, m * 128:(m + 1) * 128], ro_[:, c, :n], [wts["w_proj_rwkv"][1], t_ro_], [t_pr], start=(c == 0), stop=(c == 7))
                        zt_, t_zt_ = ZT_.next()
                        TT("dve", zt_[:, :n], pm[:, :n], gm_[:, m, :n], ALU.mult, [t_pm, t_gm_], [t_zt_])
                        TT("dve", zb_[:, m, :n], pr[:, :n], gr_[:, m, :n], ALU.mult, [t_pr, t_gr_], [t_zb_])
                        TT("pool", zb_[:, m, :n], zb_[:, m, :n], zt_[:, :n], ALU.add, [t_zb_, t_zt_], [t_zb_])
                    for m in range(8):
                        py_, t_py_ = PS.next()
                        for c in range(8):
                            MM(py_[:, :n], wts["w_out"][0][:, c, m * 128:(m + 1) * 128], zb_[:, c, :n], [wts["w_out"][1], t_zb_], [t_py_], start=(c == 0), stop=(c == 7))
                        STT("dve", xt_[:, m, :n], py_[:, :n], prm[:, l, 2, m, j:j + 1], xt_[:, m, :n], ALU.mult, ALU.add, [t_py_, t_xt_, t_prm], [t_xt_])
                    fw.dma("pool", S["xres"].ap[:, t0:t0 + n].rearrange("(c p) t -> p c t", p=128), xt_[:, :, :n], reads=[t_xt_], writes=S["xres"].tu(t0, n))
                fw.barrier()
            if stop == "F":
                break
            moe = (l % 2 == 1)
            with ExitStack() as sg:
                work = {"x": Ring(nc, sg, "gx", [128, 8, 512], F32, 1 if moe else 2), "sq": Ring(nc, sg, "gsq", [128, 8, 512], BF16, 1), "rs": Ring(nc, sg, "grs", [128, 512], F32, 2)}
                FP = 256
                H2 = Ring(nc, sg, "gh2", [128, 8, 512], BF16, 1)
                YA = Ring(nc, sg, "gya", [128, 8, 512], F32, 1)
                WGS = Ring(nc, sg, "gwgs", [128, 8, FP], F32, 3)
                WG = Ring(nc, sg, "gwg", [128, 8, FP], BF16, 2)
                WU = Ring(nc, sg, "gwu", [128, 8, FP], BF16, 2)
                WD = Ring(nc, sg, "gwd", [128, FP // 128, D], BF16, 2)
                ACTB = Ring(nc, sg, "gact", [128, FP // 128, 512], BF16, 2)
                SIL = Ring(nc, sg, "gsil", [128, 512], F32, 3)
                XB = Ring(nc, sg, "gxb", [128, 8, 512], F32, 1)
                if moe:
                    H32 = Ring(nc, sg, "gh32", [128, 8, 512], F32, 1)
                    wr = sg.enter_context(SBT("wr_sb", [128, 8, NEXP], F32)); t_wr = TU("wr")
                    fw.dma("sp", wr[:], I["w_router"].ap[0].rearrange("(c p) e -> p c e", p=128), writes=[t_wr])
                    SEL = sg.enter_context(SBT("SEL", [8, NEXP, 128], F32)); t_SEL = TU("SEL")
                    fw.op("dve", "tensor_copy", [t_cst], [t_SEL], out=SEL[:], in_=ident_f[0:8, 0:8].unsqueeze(2).to_broadcast([8, NEXP, 128]))
                    GTM = Ring(nc, sg, "gtm", [128, 4, 24], F32, 2)
                    GTT = Ring(nc, sg, "gtt", [8, 512], F32, 2)
                    GBC = Ring(nc, sg, "gbc", [128, 512], F32, 2)
                    gfin = sg.enter_context(SBT("gfin", [128, 8], F32)); t_gfin = TU("gfin")
                    fw.dma("sp", gfin[:], I["g_final"].ap.rearrange("(c p) -> p c", p=128), writes=[t_gfin], allow_slow_non_contiguous=True)
                if moe:
                    tiles_g = [(256 + 512 * i, 512, i) for i in range(4)]
                else:
                    tiles_g = [(t0, n, None) for (t0, n) in TILES]
                dff = D_FFE if moe else D_FF
                t_h2b_persist = TU("h2b")
                for (t0, n, hi) in tiles_g:
                    h2, t_h2a = H2.next(); ya, t_ya = YA.next()
                    t_h2l = [t_h2a, t_h2b_persist]
                    xkeep = {}
                    if moe:
                        xb, t_xb = XB.next()
                        t1 = t0 + 2048

                        def loader(xt, t_xt, t0=t0, t1=t1, n=n, xb=xb, t_xb=t_xb):
                            fw.dma("sp", xt[:, :, :n], S["xres"].ap[:, t0:t0 + n].rearrange("(c p) t -> p c t", p=128), reads=S["xres"].tu(t0, n), writes=[t_xt])
                            fw.dma("sp", xb[:, :, :n], S["xres"].ap[:, t1:t1 + n].rearrange("(c p) t -> p c t", p=128), reads=S["xres"].tu(t1, n), writes=[t_xb])
                            TS("pool", xt[:, :, :n], xt[:, :, :n], sel_sb[:, 0:1], None, ALU.mult, None, [t_xt, t_cst], [t_xt])
                            STT("dve", xt[:, :, :n], xb[:, :, :n], sel_sb[:, 1:2], xt[:, :, :n], ALU.mult, ALU.add, [t_xb, t_xt, t_cst], [t_xt])
                            fw.op("pool", "tensor_copy", [t_xt], [t_xb], out=xb[:, :, :n], in_=xt[:, :, :n])
                        h32, t_h32 = H32.next()
                        norm_mod(sg, loader, None, t0, n, l, 1, h2[:, :, :n], t_h2l, work, h32=(h32, t_h32))
                    else:
                        norm_mod(sg, S["xres"].ap, S["xres"].tu(t0, n), t0, n, l, 1, h2[:, :, :n], t_h2l, work)
                    fw.op("pool", "memset", [], [t_ya], ap=ya[:, :, :n], constant=0.0)
                    gbcs = [None] * NEXP
                    if moe:
                        gtm, t_gtm = GTM.next(); gtt, t_gtt = GTT.next()
                        pl_, t_pl = PS.next()
                        for sub in range(4):
                            for c in range(8):
                                MM(pl_[:, sub * 8:sub * 8 + 8], h32[:, c, sub * 128:(sub + 1) * 128], wr[:, c, :], [t_h32, t_wr], [t_pl], start=(c == 0), stop=(c == 7))
                        lg = gtm[:, :, 0:8]; eq1 = gtm[:, :, 8:16]; eq2 = gtm[:, :, 16:24]
                        fw.op("dve", "tensor_copy", [t_pl], [t_gtm], out=lg, in_=pl_[:, 0:32].rearrange("p (s e) -> p s e", e=8))
                        mx, t_mx = SIL.next()
                        m1 = mx[:, 0:4]; m2 = mx[:, 4:8]; p1 = mx[:, 8:12]; p2 = mx[:, 12:16]
                        fw.op("dve", "tensor_reduce", [t_gtm], [t_mx], out=m1, in_=lg, axis=AX.X, op=ALU.max)
                        TT("dve", eq1, lg, m1.unsqueeze(2).to_broadcast([128, 4, 8]), ALU.is_equal, [t_gtm, t_mx], [t_gtm])
                        STT("dve", eq2, eq1, -1e30, lg, ALU.mult, ALU.add, [t_gtm], [t_gtm])
                        fw.op("dve", "tensor_reduce", [t_gtm], [t_mx], out=m2, in_=eq2, axis=AX.X, op=ALU.max)
                        TT("dve", eq2, eq2, m2.unsqueeze(2).to_broadcast([128, 4, 8]), ALU.is_equal, [t_gtm, t_mx], [t_gtm])
                        TT("dve", p1, m2, m1, ALU.subtract, [t_mx], [t_mx])
                        ACT(p1, p1, AF.Exp, [t_mx], [t_mx])
                        TS("dve", p1, p1, 1.0, None, ALU.add, None, [t_mx], [t_mx])
                        fw.op("dve", "reciprocal", [t_mx], [t_mx], out=p1, in_=p1)
                        TS("dve", p2, p1, -1.0, 1.0, ALU.mult, ALU.add, [t_mx], [t_mx])
                        TT("dve", eq1, eq1, p1.unsqueeze(2).to_broadcast([128, 4, 8]), ALU.mult, [t_gtm, t_mx], [t_gtm])
                        TT("dve", eq2, eq2, p2.unsqueeze(2).to_broadcast([128, 4, 8]), ALU.mult, [t_gtm, t_mx], [t_gtm])
                        TT("dve", eq1, eq1, eq2, ALU.add, [t_gtm], [t_gtm])
                        pt_, t_pt = PS.next()
                        for sub in range(4):
                            MM(pt_[0:8, sub * 128:(sub + 1) * 128], gtm[:, sub, 8:16], ident_f, [t_gtm, t_cst], [t_pt])
                        fw.op("dve", "tensor_copy", [t_pt], [t_gtt], out=gtt[:], in_=pt_[0:8, :])
                        if "gates" in dbg:
                            fw.dma("pool", dbg["gates"].ap[:, hi * 512:(hi + 1) * 512], gtt[:], reads=[t_gtt], writes=dbg["gates"].tu(hi * 512, 512))
                    for ex in range(NEXP if moe else 1):
                        if moe:
                            gbc, t_gbc = GBC.next()
                            pb_, t_pb = PS.next()
                            MM(pb_[:, :n], SEL[:, ex, :], gtt[:, :n], [t_SEL, t_gtt], [t_pb])
                            fw.op("dve", "tensor_copy", [t_pb], [t_gbc], out=gbc[:, :n], in_=pb_[:, :n])
                            wg_ap = I["w_exp_gate"].ap[0, ex]; wu_ap = I["w_exp_up"].ap[0, ex]; wd_ap = I["w_exp_down"].ap[0, ex]
                        else:
                            wg_ap = I["w_ff_gate"].ap[0]; wu_ap = I["w_ff_up"].ap[0]; wd_ap = I["w_ff_down"].ap[0]
                        for f0 in range(0, dff, FP):
                            fwid = min(FP, dff - f0)
                            nj = fwid // 128
                            wg, t_wg = WG.next(); wu, t_wu = WU.next(); wd, t_wd = WD.next()
                            st1, t_st1 = WGS.next()
                            fw.dma("sp", st1[:, :, :fwid], wg_ap[:, f0:f0 + fwid].rearrange("(c p) m -> p c m", p=128), writes=[t_st1])
                            fw.op("dve", "tensor_copy", [t_st1], [t_wg], out=wg[:, :, :fwid], in_=st1[:, :, :fwid])
                            st2, t_st2 = WGS.next()
                            fw.dma("sp", st2[:, :, :fwid], wu_ap[:, f0:f0 + fwid].rearrange("(c p) m -> p c m", p=128), writes=[t_st2])
                            fw.op("act", "activation", [t_st2], [t_wu], out=wu[:, :, :fwid], in_=st2[:, :, :fwid], func=AF.Copy)
                            st3, t_st3 = WGS.next()
                            st3v = st3[:].rearrange("p c m -> p (c m)").rearrange("p (j m) -> p j m", m=D)
                            fw.dma("sp", st3v[:, :nj, :], wd_ap[f0:f0 + fwid, :].rearrange("(j p) m -> p j m", p=128), writes=[t_st3])
                            ACT(wd[:, :nj, :], st3v[:, :nj, :], AF.Copy, [t_st3], [t_wd])
                            ab_, t_ab_ = ACTB.next()
                            for jb in range(nj):
                                pg_, t_pg_ = PS.next(); pu_, t_pu_ = PS.next()
                                for c in range(8):
                                    MM(pg_[:, :n], wg[:, c, jb * 128:(jb + 1) * 128], h2[:, c, :n], [t_wg] + t_h2l, [t_pg_], start=(c == 0), stop=(c == 7))
                                for c in range(8):
                                    MM(pu_[:, :n], wu[:, c, jb * 128:(jb + 1) * 128], h2[:, c, :n], [t_wu] + t_h2l, [t_pu_], start=(c == 0), stop=(c == 7))
                                sil, t_sil = SIL.next()
                                ACT(sil[:, :n], pg_[:, :n], AF.Silu, [t_pg_], [t_sil])
                                if moe:
                                    TT("dve", sil[:, :n], sil[:, :n], gbc[:, :n], ALU.mult, [t_sil, t_gbc], [t_sil])
                                TT("dve", ab_[:, jb, :n], pu_[:, :n], sil[:, :n], ALU.mult, [t_pu_, t_sil], [t_ab_])
                            for m in range(8):
                                pd_, t_pd_ = PS.next()
                                for jb in range(nj):
                                    MM(pd_[:, :n], wd[:, jb, m * 128:(m + 1) * 128], ab_[:, jb, :n], [t_wd, t_ab_], [t_pd_], start=(jb == 0), stop=(jb == nj - 1))
                                TT("dve", ya[:, m, :n], pd_[:, :n], ya[:, m, :n], ALU.add, [t_pd_, t_ya], [t_ya])
                    j = 1 if t0 < NCTX else 0
                    if moe:
                        xs, t_xs = xb, t_xb
                    else:
                        xs, t_xs = work["x"].next()
                        fw.dma("sp", xs[:, :, :n], S["xres"].ap[:, t0:t0 + n].rearrange("(c p) t -> p c t", p=128), reads=S["xres"].tu(t0, n), writes=[t_xs])
                    for m in range(8):
                        STT("dve", xs[:, m, :n], ya[:, m, :n], prm[:, l, 5, m, j:j + 1], xs[:, m, :n], ALU.mult, ALU.add, [t_ya, t_xs, t_prm], [t_xs])
                    if not moe:
                        fw.dma("pool", S["xres"].ap[:, t0:t0 + n].rearrange("(c p) t -> p c t", p=128), xs[:, :, :n], reads=[t_xs], writes=S["xres"].tu(t0, n))
                    else:
                        sq, t_sq = work["sq"].next(); rs, t_rs = work["rs"].next()
                        ACT(sq[:, :, :n], xs[:, :, :n], AF.Square, [t_xs], [t_sq])
                        ps, t_ps = PS.next()
                        for c in range(8):
                            MM(ps[:, :n], ones_b, sq[:, c, :n], [t_sq, t_cst], [t_ps], start=(c == 0), stop=(c == 7))
                        ACT(rs[:, :n], ps[:, :n], AF.Sqrt, [t_ps], [t_rs], scale=1.0 / D, bias=1e-6)
                        fw.op("dve", "reciprocal", [t_rs], [t_rs], out=rs[:, :n], in_=rs[:, :n])
                        TT("dve", xs[:, :, :n], xs[:, :, :n], rs[:, :n].unsqueeze(1).to_broadcast([128, 8, n]), ALU.mult, [t_rs, t_xs], [t_xs])
                        TT("pool", xs[:, :, :n], xs[:, :, :n], gfin[:].unsqueeze(2).to_broadcast([128, 8, n]), ALU.mult, [t_xs, t_gfin], [t_xs])
                        fw.dma("pool", out.ap[:, hi * 512:(hi + 1) * 512].rearrange("(c p) t -> p c t", p=128), xs[:, :, :n], reads=[t_xs], writes=out.tu(hi * 512, 512))
                fw.barrier()

        fw.barrier()
        fw.finish()
        nc._fw_counts = {n: e.cnt for n, e in fw.engs.items()}
        nc._fw_counts["dma"] = {q: sum(u for _, u in lst) for q, (lst, _) in fw.dma_sems.items()}
    return nc


def make_consts():
    c = np.zeros((128, 1024), np.float32)
    p = np.arange(128)[:, None]
    f = np.arange(128)[None, :]
    c[:, 0:128] = (p == f)
    c[:, 128:256] = (p <= f)
    c[:, 256:384] = (p < f)
    c[:, 384:512] = (p >= f)
    c[:, 512:640] = (p > f)
    c[:, 640:768] = (p // 64 == f // 64)
    c[:, 768:896] = 1.0
    c[:, 896:898] = (p // 64 == np.arange(2)[None, :])
    return c


def make_in_maps(inputs):
    f32 = lambda a: np.ascontiguousarray(np.asarray(a, dtype=np.float32))
    shared = {k: f32(inputs[k]) for k in ["w_ada", "b_ada", "g_norm_mix", "g_norm_ffn", "w_in", "b_mlstm_gate", "g_mlstm_norm", "mu_shift", "w0", "w2",
                                          "a0", "a2", "g2", "k_k", "k_a", "ln_w", "ln_b", "w_proj_mlstm", "w_proj_rwkv", "w_out", "w_ff_gate", "w_ff_up",
                                          "w_ff_down", "w_router", "w_exp_gate", "w_exp_up", "w_exp_down", "g_final"]}
    shared["r_k"] = f32(inputs["r_k"]).reshape(DEPTH, D)
    shared["cst"] = make_consts()
    x = f32(inputs["x"]); ctx = f32(inputs["ctx"]); c = f32(inputs["c"]); c_ctx = f32(inputs["c_ctx"])
    maps = []
    for core in range(8):
        b, half = core // 2, core % 2
        m = dict(shared)
        m["xT"] = np.ascontiguousarray(np.concatenate([ctx[b], x[b]], axis=0).T)
        m["cs"] = np.ascontiguousarray(np.stack([c[b], c_ctx], axis=1))
        sel = np.zeros((128, 2), np.float32)
        sel[:, half] = 1.0
        m["sel"] = sel
        maps.append(m)
    return maps


_NC_CACHE = {}


def kernel(**inputs):
    if "nc" not in _NC_CACHE:
        _NC_CACHE["nc"] = build()
    nc = _NC_CACHE["nc"]
    maps = make_in_maps(inputs)
    res = run_bass_kernel_spmd(nc, maps, core_ids=list(range(8)))
    outp = np.zeros((4, NLAT, D), np.float32)
    for core in range(8):
        b, half = core // 2, core % 2
        outp[b, half * 2048:(half + 1) * 2048, :] = res.results[core]["out"].T
    return outp
```

```python
import numpy as np
from contextlib import ExitStack
import concourse.bass as bass
import concourse.mybir as mybir
from concourse.bass_utils import run_bass_kernel_spmd

F32 = mybir.dt.float32
BF16 = mybir.dt.bfloat16
AF = mybir.ActivationFunctionType
ALU = mybir.AluOpType
AX = mybir.AxisListType

SAME_ENGINE_SYNC = True
E3_PHASE2 = True

D = 1024
NCTX = 256
NLAT = 4096
NT = NCTX + NLAT
DEPTH = 2
D_IN = 8608
D_FF = 2816
D_FFE = 3584
NEXP = 8
TILES = [(0, 256)] + [(256 + 512 * i, 512) for i in range(8)]
LAM = 0.6065306597126334


class TU:
    __slots__ = ("name", "w", "r")

    def __init__(self, name=""):
        self.name = name
        self.w = {}
        self.r = {}


class Eng:
    def __init__(self, name, sem):
        self.name = name
        self.sem = sem
        self.cnt = 0
        self.seen = {}
        self.prog = []


class FW:
    def __init__(self, nc, stack):
        self.nc = nc
        self.engs = {}
        for n in ("pe", "act", "dve", "pool", "sp"):
            sem = stack.enter_context(nc.semaphore("sem_" + n))
            self.engs[n] = Eng(n, sem)
        self.dma_sems = {}
        for q, k in (("sp", 24), ("pool", 12), ("act", 4)):
            lst = [[stack.enter_context(nc.semaphore(f"dq_{q}_{i}")), 0] for i in range(k)]
            self.dma_sems[q] = [lst, 0]
        self.n_instr = 0

    def _gather(self, eng, reads, writes):
        deps = {}
        for t in reads:
            for k, (s, v) in t.w.items():
                if deps.get(k, (None, 0))[1] < v:
                    deps[k] = (s, v)
        for t in writes:
            for d in (t.w, t.r):
                for k, (s, v) in d.items():
                    if deps.get(k, (None, 0))[1] < v:
                        deps[k] = (s, v)
        for k, (s, v) in deps.items():
            if k == id(eng.sem) and (eng.name in ("pe", "sp") or not SAME_ENGINE_SYNC):
                continue
            if eng.seen.get(k, 0) < v:
                eng.seen[k] = v
                eng.prog.append(lambda e, s=s, v=v: e.wait_ge(s, v))

    def _record(self, ev, reads, writes):
        k = id(ev[0])
        for t in reads:
            if t.r.get(k, (None, 0))[1] < ev[1]:
                t.r[k] = ev
        for t in writes:
            t.w = {k: ev}
            t.r = {}

    def op(self, engname, fn, reads=(), writes=(), **kw):
        eng = self.engs[engname]
        self._gather(eng, reads, writes)
        eng.cnt += 1
        ev = (eng.sem, eng.cnt)
        sem = eng.sem
        if isinstance(fn, str):
            eng.prog.append(lambda e, fn=fn, sem=sem, kw=kw: getattr(e, fn)(**kw).then_inc(sem, 1))
        else:
            eng.prog.append(lambda e, fn=fn, sem=sem: fn(e).then_inc(sem, 1))
        self._record(ev, reads, writes)
        self.n_instr += 1

    def dma(self, q, out_ap, in_ap, reads=(), writes=(), **kw):
        eng = self.engs[q]
        self._gather(eng, reads, writes)
        lst, idx = self.dma_sems[q]
        ent = lst[idx % len(lst)]
        self.dma_sems[q][1] = idx + 1
        sem, uses = ent
        if uses > 0 and eng.seen.get(id(sem), 0) < 16 * uses:
            eng.seen[id(sem)] = 16 * uses
            eng.prog.append(lambda e, s=sem, v=16 * uses: e.wait_ge(s, v))
        ent[1] = uses + 1
        ev = (sem, 16 * (uses + 1))
        eng.prog.append(lambda e, o=out_ap, i=in_ap, sem=sem, kw=kw: e.dma_start(out=o, in_=i, **kw).then_inc(sem, 16))
        self._record(ev, reads, writes)
        self.n_instr += 1

    def barrier(self):
        evs = []
        for n, e in self.engs.items():
            if e.cnt > 0:
                evs.append((e.sem, e.cnt))
        for q, (lst, _) in self.dma_sems.items():
            for sem, uses in lst:
                if uses > 0:
                    evs.append((sem, 16 * uses))
        for n, e in self.engs.items():
            for s, v in evs:
                if s is e.sem:
                    continue
                if e.seen.get(id(s), 0) < v:
                    e.seen[id(s)] = v
                    e.prog.append(lambda en, s=s, v=v: en.wait_ge(s, v))

    def finish(self):
        nc = self.nc
        with nc.Block() as block:
            @block.tensor
            def _(e):
                for f in self.engs["pe"].prog:
                    f(e)

            @block.scalar
            def _(e):
                for f in self.engs["act"].prog:
                    f(e)

            @block.vector
            def _(e):
                for f in self.engs["dve"].prog:
                    f(e)

            @block.gpsimd
            def _(e):
                for f in self.engs["pool"].prog:
                    f(e)

            @block.sync
            def _(e):
                for f in self.engs["sp"].prog:
                    f(e)


_UID = [0]


class Ring:
    def __init__(self, nc, stack, name, shape, dtype, n, psum=False):
        self.bufs = []
        for i in range(n):
            if psum:
                t = stack.enter_context(nc.psum_tensor(f"{name}{i}", shape, dtype))
            else:
                _UID[0] += 1
                t = stack.enter_context(nc.sbuf_tensor(f"{name}{i}_u{_UID[0]}", shape, dtype))
            self.bufs.append((t, TU(f"{name}{i}")))
        self.i = 0

    def next(self):
        b = self.bufs[self.i % len(self.bufs)]
        self.i += 1
        return b


class DT:
    def __init__(self, nc, name, shape, dtype, kind="Internal"):
        self.t = nc.dram_tensor(name, shape, dtype, kind=kind)
        self.ap = self.t.ap()
        self.tus = {}
        self.name = name

    def tu(self, t0=0, n=NT, key=None):
        out = []
        for c in range(t0 // 128, (t0 + n + 127) // 128):
            k = (key, c)
            if k not in self.tus:
                self.tus[k] = TU(f"{self.name}{k}")
            out.append(self.tus[k])
        return out


def build(debug=None, nlayers=DEPTH, stop=None):
    debug = debug or []
    nc = bass.Bass("TRN2", target_bir_lowering=False)

    def SBT(name, shape, dt):
        _UID[0] += 1
        return nc.sbuf_tensor(f"{name}_u{_UID[0]}", shape, dt)
    I = {}

    def inp(name, shape, dt=F32):
        I[name] = DT(nc, name, shape, dt, kind="ExternalInput")
        return I[name]

    inp("xT", [D, NT]); inp("cs", [D, 2]); inp("sel", [128, 2]); inp("cst", [128, 1024])
    inp("w_ada", [DEPTH, D, 6 * D]); inp("b_ada", [DEPTH, 6 * D])
    inp("g_norm_mix", [DEPTH, D]); inp("g_norm_ffn", [DEPTH, D])
    inp("w_in", [DEPTH, D, D_IN]); inp("b_mlstm_gate", [DEPTH, 4, 8]); inp("g_mlstm_norm", [DEPTH, D])
    inp("mu_shift", [DEPTH, 3456]); inp("w0", [DEPTH, 2, D]); inp("w2", [DEPTH, 2, 64, D])
    inp("a0", [DEPTH, 2, D]); inp("a2", [DEPTH, 2, 64, D]); inp("g2", [DEPTH, 128, D])
    inp("k_k", [DEPTH, D]); inp("k_a", [DEPTH, D]); inp("r_k", [DEPTH, D])
    inp("ln_w", [DEPTH, D]); inp("ln_b", [DEPTH, D])
    inp("w_proj_mlstm", [DEPTH, D, D]); inp("w_proj_rwkv", [DEPTH, D, D]); inp("w_out", [DEPTH, D, D])
    inp("w_ff_gate", [1, D, D_FF]); inp("w_ff_up", [1, D, D_FF]); inp("w_ff_down", [1, D_FF, D])
    inp("w_router", [1, D, NEXP]); inp("w_exp_gate", [1, NEXP, D, D_FFE]); inp("w_exp_up", [1, NEXP, D, D_FFE])
    inp("w_exp_down", [1, NEXP, D_FFE, D]); inp("g_final", [D])
    out = DT(nc, "out", [D, NLAT // 2], F32, kind="ExternalOutput")
    dbg = {}
    for name, shape, dt in debug:
        dbg[name] = DT(nc, "dbg_" + name, shape, dt, kind="ExternalOutput")

    S = {}

    def scr(name, shape, dt):
        if name in dbg:
            S[name] = dbg[name]
        else:
            S[name] = DT(nc, "s_" + name, shape, dt)
        return S[name]

    scr("xres", [D, NT], F32)
    scr("qT", [512, NT], BF16); scr("kT", [512, NT], BF16); scr("ktm", [NT, 512], BF16)
    scr("vtm", [NT, 1024], BF16); scr("otm", [NT, 1024], BF16); scr("gT", [32, NT], F32)
    scr("pT", [3456, NT], BF16); scr("mgT", [2048, NT], BF16)
    scr("hm", [2, NT, 1024], F32); scr("moT", [D, NT], BF16)
    scr("gsc", [2, 3, 8, NT], F32)

    with ExitStack() as st:
        fw = FW(nc, st)
        psall = st.enter_context(nc.psum_tensor("psall", [128, 4096], F32))

        class PSRing:
            def __init__(self):
                self.tus = [TU(f"psb{i}") for i in range(8)]
                self.i = 0

            n = 8

            def next(self):
                b = self.i % self.n
                self.i += 1
                return psall[:, b * 512:(b + 1) * 512], self.tus[b]

            def next2(self):
                if self.i % 2:
                    self.i += 1
                b = self.i % self.n
                self.i += 2
                return psall[:, b * 512:(b + 2) * 512].rearrange("p (h x) -> p h x", x=512), [self.tus[b], self.tus[b + 1]]

            def pair(self, b):
                return psall[:, b * 512:(b + 2) * 512].rearrange("p (h x) -> p h x", x=512), [self.tus[b], self.tus[b + 1]]
        PS = PSRing()
        cst = st.enter_context(SBT("cst_sb", [128, 1024], F32))
        cstb = st.enter_context(SBT("cstb_sb", [128, 1024], BF16))
        t_cst = TU("cst")
        fw.dma("sp", cst[:], I["cst"].ap, writes=[t_cst])
        fw.op("dve", lambda e: e.tensor_copy(out=cstb[:], in_=cst[:]), reads=[t_cst], writes=[t_cst])
        ident_f, ident_b = cst[:, 0:128], cstb[:, 0:128]
        m_le, m_lt, m_ge, m_gt = (cst[:, 128 * i:128 * (i + 1)] for i in range(1, 5))
        blk1_f, blk1_b = cst[:, 640:768], cstb[:, 640:768]
        ones_b = cstb[:, 768:896]
        ones_f = cst[:, 768:896]
        sel2_f = cst[:, 896:898]
        mods = st.enter_context(SBT("mods", [128, DEPTH, 48, 2], F32))
        t_mods = TU("mods")
        prm = st.enter_context(SBT("prm", [128, DEPTH, 6, 8, 2], F32))
        t_prm = TU("prm")
        sel_sb = st.enter_context(SBT("sel_sb", [128, 2], F32))
        fw.dma("sp", sel_sb[:], I["sel"].ap, writes=[t_cst])

        with ExitStack() as sa:
            s_sb = sa.enter_context(SBT("s_sb", [128, 8, 2], F32))
            bada = sa.enter_context(SBT("bada", [128, DEPTH, 48], F32))
            gn = sa.enter_context(SBT("gn", [128, DEPTH, 2, 8], F32))
            t_s, t_b, t_gn = TU("s"), TU("bada"), TU("gn")
            fw.dma("sp", s_sb[:], I["cs"].ap.rearrange("(c p) j -> p c j", p=128), writes=[t_s])
            fw.op("act", lambda e: e.activation(out=s_sb[:], in_=s_sb[:], func=AF.Silu), reads=[t_s], writes=[t_s])
            for l_ in range(DEPTH):
                fw.dma("sp", bada[:, l_, :], I["b_ada"].ap[l_].rearrange("(j p) -> p j", p=128), writes=[t_b], allow_slow_non_contiguous=True)
                fw.dma("sp", gn[:, l_, 0, :], I["g_norm_mix"].ap[l_].rearrange("(c p) -> p c", p=128), writes=[t_gn], allow_slow_non_contiguous=True)
                fw.dma("sp", gn[:, l_, 1, :], I["g_norm_ffn"].ap[l_].rearrange("(c p) -> p c", p=128), writes=[t_gn], allow_slow_non_contiguous=True)
            WA = Ring(nc, sa, "wa", [128, 8, 512], F32, 3)
            for l in range(nlayers):
                ps, t_ps = PS.next()
                for jg in range(12):
                    wt, t_w = WA.next()
                    fw.dma("sp", wt[:], I["w_ada"].ap[l, :, jg * 512:(jg + 1) * 512].rearrange("(c p) m -> p c m", p=128), writes=[t_w])
                    for jj in range(4):
                        j = jg * 4 + jj
                        for c in range(8):
                            fw.op("pe", lambda e, ps=ps, wt=wt, c=c, j=j, jj=jj: e.matmul(ps[:, 2 * j:2 * j + 2], lhsT=wt[:, c, jj * 128:(jj + 1) * 128], rhs=s_sb[:, c, :], start=(c == 0), stop=(c == 7)),
                                  reads=[t_w, t_s], writes=[t_ps])
                fw.op("dve", lambda e, ps=ps, l=l: e.tensor_tensor(out=mods[:, l], in0=ps[:, 0:96].rearrange("p (j t) -> p j t", t=2), in1=bada[:, l].unsqueeze(2).to_broadcast([128, 48, 2]), op=ALU.add),
                      reads=[t_ps, t_b], writes=[t_mods])
                for si in range(2):
                    o = 3 * si
                    fw.op("dve", lambda e, l=l, si=si, o=o: e.scalar_tensor_tensor(out=prm[:, l, o], in0=mods[:, l, (o + 1) * 8:(o + 2) * 8, :], scalar=1.0, in1=gn[:, l, si, :].unsqueeze(2).to_broadcast([128, 8, 2]), op0=ALU.add, op1=ALU.mult),
                          reads=[t_mods, t_gn], writes=[t_prm])
                    fw.op("dve", lambda e, l=l, o=o: e.tensor_copy(out=prm[:, l, o + 1], in_=mods[:, l, o * 8:(o + 1) * 8, :]), reads=[t_mods], writes=[t_prm])
                    fw.op("dve", lambda e, l=l, o=o: e.tensor_copy(out=prm[:, l, o + 2], in_=mods[:, l, (o + 2) * 8:(o + 3) * 8, :]), reads=[t_mods], writes=[t_prm])
            fw.barrier()
        if "mods" in dbg:
            fw.dma("sp", dbg["mods"].ap, mods[:].rearrange("p l j t -> p (l j t)"), reads=[t_mods], writes=dbg["mods"].tu())

        def norm_mod(sx, xsrc_ap, src_tus, t0, n, l, si, h_out, h_tus, work, h32=None):
            xt, t_xt = work["x"].next()
            sq, t_sq = work["sq"].next()
            rs, t_rs = work["rs"].next()
            if callable(xsrc_ap):
                xsrc_ap(xt, t_xt)
            else:
                fw.dma("sp", xt[:, :, :n], xsrc_ap[:, t0:t0 + n].rearrange("(c p) t -> p c t", p=128), reads=src_tus, writes=[t_xt])
            fw.op("act", lambda e: e.activation(out=sq[:, :, :n], in_=xt[:, :, :n], func=AF.Square), reads=[t_xt], writes=[t_sq])
            ps, t_ps = PS.next()
            for c in range(8):
                fw.op("pe", lambda e, c=c: e.matmul(ps[:, :n], lhsT=ones_b, rhs=sq[:, c, :n], start=(c == 0), stop=(c == 7)), reads=[t_sq, t_cst], writes=[t_ps])
            fw.op("act", lambda e: e.activation(out=rs[:, :n], in_=ps[:, :n], func=AF.Sqrt, scale=1.0 / D, bias=1e-6), reads=[t_ps], writes=[t_rs])
            fw.op("dve", lambda e: e.reciprocal(out=rs[:, :n], in_=rs[:, :n]), reads=[t_rs], writes=[t_rs])
            fw.op("dve", lambda e: e.tensor_tensor(out=xt[:, :, :n], in0=xt[:, :, :n], in1=rs[:, :n].unsqueeze(1).to_broadcast([128, 8, n]), op=ALU.mult), reads=[t_rs, t_xt], writes=[t_xt])
            j = 1 if t0 < NCTX else 0
            o = 3 * si
            for c in range(8):
                if c % 2 == 0:
                    fw.op("dve", lambda e, c=c: e.tensor_scalar(out=h_out[:, c, :], in0=xt[:, c, :n], scalar1=prm[:, l, o, c, j:j + 1], scalar2=prm[:, l, o + 1, c, j:j + 1], op0=ALU.mult, op1=ALU.add),
                          reads=[t_xt, t_prm], writes=h_tus)
                else:
                    fw.op("act", lambda e, c=c: e.activation(out=h_out[:, c, :], in_=xt[:, c, :n], func=AF.Identity, scale=prm[:, l, o, c, j:j + 1], bias=prm[:, l, o + 1, c, j:j + 1]),
                          reads=[t_xt, t_prm], writes=h_tus)
                if h32 is not None:
                    if c % 2 == 1:
                        fw.op("dve", lambda e, c=c: e.tensor_scalar(out=h32[0][:, c, :n], in0=xt[:, c, :n], scalar1=prm[:, l, o, c, j:j + 1], scalar2=prm[:, l, o + 1, c, j:j + 1], op0=ALU.mult, op1=ALU.add),
                              reads=[t_xt, t_prm], writes=[h32[1]])
                    else:
                        fw.op("act", lambda e, c=c: e.activation(out=h32[0][:, c, :n], in_=xt[:, c, :n], func=AF.Identity, scale=prm[:, l, o, c, j:j + 1], bias=prm[:, l, o + 1, c, j:j + 1]),
                              reads=[t_xt, t_prm], writes=[h32[1]])

        for l in range(nlayers):
            xsrc = I["xT"] if l == 0 else S["xres"]
            with ExitStack() as sb:
                hT = sb.enter_context(SBT("hT", [128, 8, NT], BF16))
                t_h = [TU(f"h{i}") for i in range(len(TILES))]
                work = {"x": Ring(nc, sb, "nx", [128, 8, 512], F32, 2), "sq": Ring(nc, sb, "nsq", [128, 8, 512], BF16, 1), "rs": Ring(nc, sb, "nrs", [128, 512], F32, 2)}
                for ti, (t0, n) in enumerate(TILES):
                    norm_mod(sb, xsrc.ap, xsrc.tu(t0, n), t0, n, l, 0, hT[:, :, t0:t0 + n], [t_h[ti]], work)
                if l == 0 and "hT" in dbg:
                    for ti, (t0, n) in enumerate(TILES):
                        fw.dma("sp", dbg["hT"].ap[:, t0:t0 + n].rearrange("(c p) t -> p c t", p=128), hT[:, :, t0:t0 + n], reads=[t_h[ti]], writes=dbg["hT"].tu(t0, n))
                WS = Ring(nc, sb, "ws", [128, 8, 512], F32, 2)
                WB = Ring(nc, sb, "wb", [128, 8, 512], BF16, 2)
                EV = Ring(nc, sb, "ev", [128, 512], BF16, 4)
                EVF = Ring(nc, sb, "evf", [128, 512], F32, 2)
                groups = [(0, 512, "fq", None), (512, 512, "fk", None), (512, 512, "tk", None),
                          (1024, 512, "tv", 0), (1536, 512, "tv", 512), (2048, 512, "to", 0), (2560, 512, "to", 512),
                          (3072, 32, "fg", None)]
                for i in range(7):
                    w = 512 if i < 6 else 384
                    groups.append((3104 + 512 * i, w, "fp", 512 * i))
                for i in range(4):
                    groups.append((6560 + 512 * i, 512, "fm", 512 * i))
                evi = 0
                for (c0, w, kind, dst) in groups:
                    ws, t_ws = WS.next()
                    wb, t_wb = WB.next()
                    fw.dma("sp", ws[:, :, :w], I["w_in"].ap[l, :, c0:c0 + w].rearrange("(c p) m -> p c m", p=128), writes=[t_ws])
                    fw.op("dve", lambda e, ws=ws, wb=wb, w=w: e.tensor_copy(out=wb[:, :, :w], in_=ws[:, :, :w]), reads=[t_ws], writes=[t_wb])
                    for ti, (t0, n) in enumerate(TILES):
                        if kind[0] == "f":
                            for mb in range((w + 127) // 128):
                                mw = min(128, w - mb * 128)
                                ps, t_ps = PS.next()
                                for c in range(8):
                                    fw.op("pe", lambda e, ps=ps, wb=wb, c=c, mb=mb, mw=mw, t0=t0, n=n: e.matmul(ps[:mw, :n], lhsT=wb[:, c, mb * 128:mb * 128 + mw], rhs=hT[:, c, t0:t0 + n], start=(c == 0), stop=(c == 7)),
                                          reads=[t_wb, t_h[ti]], writes=[t_ps])
                                evi += 1
                                if kind == "fg":
                                    ev, t_ev = EVF.next()
                                    fw.op("dve", lambda e, ev=ev, ps=ps, mw=mw, n=n: e.tensor_copy(out=ev[:mw, :n], in_=ps[:mw, :n]), reads=[t_ps], writes=[t_ev])
                                    fw.dma("pool", S["gT"].ap[:, t0:t0 + n], ev[:mw, :n], reads=[t_ev], writes=S["gT"].tu(t0, n))
                                    continue
                                ev, t_ev = EV.next()
                                if kind == "fm":
                                    fw.op("act", lambda e, ev=ev, ps=ps, mw=mw, n=n: e.activation(out=ev[:mw, :n], in_=ps[:mw, :n], func=AF.Sigmoid), reads=[t_ps], writes=[t_ev])
                                elif kind == "fq":
                                    fw.op("act", lambda e, ev=ev, ps=ps, mw=mw, n=n: e.activation(out=ev[:mw, :n], in_=ps[:mw, :n], func=AF.Copy, scale=0.125), reads=[t_ps], writes=[t_ev])
                                elif evi % 2 == 0:
                                    fw.op("act", lambda e, ev=ev, ps=ps, mw=mw, n=n: e.activation(out=ev[:mw, :n], in_=ps[:mw, :n], func=AF.Copy), reads=[t_ps], writes=[t_ev])
                                else:
                                    fw.op("dve", lambda e, ev=ev, ps=ps, mw=mw, n=n: e.tensor_copy(out=ev[:mw, :n], in_=ps[:mw, :n]), reads=[t_ps], writes=[t_ev])
                                dname = {"fq": "qT", "fk": "kT", "fp": "pT", "fm": "mgT"}[kind]
                                r0 = (dst or 0) + mb * 128
                                fw.dma("pool", S[dname].ap[r0:r0 + mw, t0:t0 + n], ev[:mw, :n], reads=[t_ev], writes=S[dname].tu(t0, n, key=r0))
                        else:
                            for sub in range(n // 128):
                                ts = t0 + sub * 128
                                ps, t_ps = PS.next()
                                for c in range(8):
                                    fw.op("pe", lambda e, ps=ps, wb=wb, c=c, ts=ts, w=w: e.matmul(ps[:, :w], lhsT=hT[:, c, ts:ts + 128], rhs=wb[:, c, :w], start=(c == 0), stop=(c == 7)),
                                          reads=[t_wb, t_h[ti]], writes=[t_ps])
                                ev, t_ev = EV.next()
                                evi += 1
                                if kind == "to":
                                    fw.op("act", lambda e, ev=ev, ps=ps: e.activation(out=ev[:], in_=ps[:], func=AF.Sigmoid), reads=[t_ps], writes=[t_ev])
                                elif evi % 2 == 0:
                                    fw.op("act", lambda e, ev=ev, ps=ps: e.activation(out=ev[:], in_=ps[:], func=AF.Copy), reads=[t_ps], writes=[t_ev])
                                else:
                                    fw.op("dve", lambda e, ev=ev, ps=ps: e.tensor_copy(out=ev[:], in_=ps[:]), reads=[t_ps], writes=[t_ev])
                                dname = {"tk": "ktm", "tv": "vtm", "to": "otm"}[kind]
                                d0 = dst or 0
                                fw.dma("pool", S[dname].ap[ts:ts + 128, d0:d0 + w], ev[:, :w], reads=[t_ev], writes=S[dname].tu(ts, 128, key=d0))
                fw.barrier()
            if stop == "C":
                break
            NCH = NT // 64
            with ExitStack() as sd:
                etm = sd.enter_context(SBT("etm", [64, 2, NCH, 8], F32))
                ctm = sd.enter_context(SBT("ctm", [64, 2, NCH, 8], F32))
                omb = sd.enter_context(SBT("omb", [64, 2, NCH, 8], F32))
                t_etm, t_ctm, t_omb = TU("etm"), TU("ctm"), TU("omb")
                orders = [list(range(NCH)), [3, 2, 1, 0] + list(range(NCH - 1, 3, -1))]
                with ExitStack() as sd1:
                    bg = sd1.enter_context(SBT("bg", [8, 4], F32))
                    rmask = sd1.enter_context(SBT("rmask", [8, NT], F32))
                    t_bg, t_rm = TU("bg"), TU("rm")
                    fw.dma("sp", bg[:], I["b_mlstm_gate"].ap[l].rearrange("j h -> h j"), writes=[t_bg], allow_slow_non_contiguous=True)
                    fw.op("dve", lambda e: e.tensor_scalar(out=bg[:], in0=bg[:], scalar1=1.0 / 15.0, scalar2=None, op0=ALU.mult), reads=[t_bg], writes=[t_bg])
                    fw.op("pool", lambda e: e.memset(rmask[:], 1.0), writes=[t_rm])
                    fw.op("pool", lambda e: e.memset(rmask[:].rearrange("p (c l) -> p c l", l=64)[:, :, 0:1], 0.0), writes=[t_rm])
                    for d in range(2):
                        It = sd1.enter_context(SBT(f"It{d}", [8, NT], F32))
                        Ft = sd1.enter_context(SBT(f"Ft{d}", [8, NT], F32))
                        Cs = sd1.enter_context(SBT(f"Cs{d}", [8, NT], F32))
                        Bn = sd1.enter_context(SBT(f"Bn{d}", [8, NT], F32))
                        sm_ = sd1.enter_context(SBT(f"gsm{d}", [8, 8 * NCH], F32))
                        t_i, t_f, t_c, t_b, t_s = TU("It"), TU("Ft"), TU("Cs"), TU("Bn"), TU("gsm")
                        tot = sm_[:, 0:NCH]; G = sm_[:, NCH:2 * NCH]; Mc = sm_[:, 2 * NCH:3 * NCH]; mm_ = sm_[:, 3 * NCH:4 * NCH + 1]; om = sm_[:, 5 * NCH:6 * NCH]
                        fw.dma("sp", It[:], S["gT"].ap[(2 * d) * 8:(2 * d) * 8 + 8, :], reads=S["gT"].tu(), writes=[t_i])
                        fw.dma("sp", Ft[:], S["gT"].ap[(2 * d + 1) * 8:(2 * d + 1) * 8 + 8, :], reads=S["gT"].tu(), writes=[t_f])
                        fw.op("act", lambda e, It=It, d=d: e.activation(out=It[:], in_=It[:], func=AF.Tanh, scale=1.0 / 15.0, bias=bg[:, 2 * d:2 * d + 1]), reads=[t_i, t_bg], writes=[t_i])
                        fw.op("act", lambda e, Ft=Ft, d=d: e.activation(out=Ft[:], in_=Ft[:], func=AF.Tanh, scale=1.0 / 15.0, bias=bg[:, 2 * d + 1:2 * d + 2]), reads=[t_f, t_bg], writes=[t_f])
                        fw.op("act", lambda e, Ft=Ft: e.activation(out=Ft[:], in_=Ft[:], func=AF.Exp, scale=-15.0), reads=[t_f], writes=[t_f])
                        fw.op("act", lambda e, Ft=Ft: e.activation(out=Ft[:], in_=Ft[:], func=AF.Ln, bias=1.0), reads=[t_f], writes=[t_f])
                        fw.op("dve", lambda e, Cs=Cs, Ft=Ft: e.tensor_tensor_scan(out=Cs[:], data0=rmask[:], data1=Ft[:], initial=0.0, op0=ALU.mult, op1=ALU.add), reads=[t_rm, t_f], writes=[t_c])
                        cs3 = Cs[:].rearrange("p (c l) -> p c l", l=64)
                        fw.op("dve", lambda e, tot=tot, cs3=cs3: e.tensor_copy(out=tot.unsqueeze(2), in_=cs3[:, :, 63:64]), reads=[t_c], writes=[t_s])
                        if d == 0:
                            fw.op("dve", lambda e, Bn=Bn, Cs=Cs: e.tensor_copy(out=Bn[:], in_=Cs[:]), reads=[t_c], writes=[t_b])
                        else:
                            fw.op("dve", lambda e, Bn=Bn, Cs=Cs, Ft=Ft: e.tensor_tensor(out=Bn[:], in0=Ft[:], in1=Cs[:], op=ALU.subtract), reads=[t_c, t_f], writes=[t_b])
                            bn3 = Bn[:].rearrange("p (c l) -> p c l", l=64)
                            fw.op("dve", lambda e, bn3=bn3, tot=tot: e.tensor_tensor(out=bn3, in0=bn3, in1=tot.unsqueeze(2).to_broadcast([8, NCH, 64]), op=ALU.add), reads=[t_b, t_s], writes=[t_b])
                        fw.op("dve", lambda e, It=It, Bn=Bn: e.scalar_tensor_tensor(out=It[:], in0=It[:], scalar=15.0, in1=Bn[:], op0=ALU.mult, op1=ALU.add), reads=[t_i, t_b], writes=[t_i])
                        it3 = It[:].rearrange("p (c l) -> p c l", l=64)
                        fw.op("dve", lambda e, G=G, it3=it3: e.tensor_reduce(out=G, in_=it3, axis=AX.X, op=ALU.max), reads=[t_i], writes=[t_s])
                        fw.op("dve", lambda e, mm_=mm_: e.memset(mm_[:, 0:1], 0.0), writes=[t_s])
                        for i, c in enumerate(orders[d]):
                            fw.op("dve", lambda e, i=i, c=c, Mc=Mc, mm_=mm_, G=G: e.tensor_tensor(out=Mc[:, c:c + 1], in0=mm_[:, i:i + 1], in1=G[:, c:c + 1], op=ALU.max), reads=[t_s], writes=[t_s])
                            fw.op("dve", lambda e, i=i, c=c, Mc=Mc, mm_=mm_, om=om: e.tensor_tensor(out=om[:, c:c + 1], in0=mm_[:, i:i + 1], in1=Mc[:, c:c + 1], op=ALU.subtract), reads=[t_s], writes=[t_s])
                            fw.op("dve", lambda e, i=i, c=c, Mc=Mc, mm_=mm_, tot=tot: e.tensor_tensor(out=mm_[:, i + 1:i + 2], in0=Mc[:, c:c + 1], in1=tot[:, c:c + 1], op=ALU.subtract), reads=[t_s], writes=[t_s])
                        fw.op("act", lambda e, om=om: e.activation(out=om, in_=om, func=AF.Exp), reads=[t_s], writes=[t_s])
                        mcb = Mc.unsqueeze(2).to_broadcast([8, NCH, 64])
                        fw.op("dve", lambda e, it3=it3, mcb=mcb: e.tensor_tensor(out=it3, in0=it3, in1=mcb, op=ALU.subtract), reads=[t_i, t_s], writes=[t_i])
                        fw.op("act", lambda e, It=It: e.activation(out=It[:], in_=It[:], func=AF.Exp), reads=[t_i], writes=[t_i])
                        bn3 = Bn[:].rearrange("p (c l) -> p c l", l=64)
                        fw.op("dve", lambda e, bn3=bn3, mcb=mcb: e.tensor_tensor(out=bn3, in0=bn3, in1=mcb, op=ALU.subtract), reads=[t_b, t_s], writes=[t_b])
                        fw.op("act", lambda e, Bn=Bn: e.activation(out=Bn[:], in_=Bn[:], func=AF.Exp), reads=[t_b], writes=[t_b])
                        for (src, t_src, dst, t_dst) in ((It, t_i, etm, t_etm), (Bn, t_b, ctm, t_ctm)):
                            for half in range(2):
                                ps, t_ps = PS.next()
                                for cc in range(NCH // 2):
                                    c = half * (NCH // 2) + cc
                                    fw.op("pe", lambda e, ps=ps, src=src, c=c, cc=cc: e.matmul(ps[0:64, cc * 8:cc * 8 + 8], lhsT=src[:, c * 64:(c + 1) * 64], rhs=ident_f[0:8, 0:8], start=True, stop=True),
                                          reads=[t_src, t_cst], writes=[t_ps])
                                fw.op("dve", lambda e, ps=ps, dst=dst, half=half, d=d: e.tensor_copy(out=dst[:, d, half * (NCH // 2):(half + 1) * (NCH // 2), :], in_=ps[0:64, 0:(NCH // 2) * 8].rearrange("p (c h) -> p c h", h=8)),
                                      reads=[t_ps], writes=[t_dst])
                        X = sd1.enter_context(SBT(f"omx{d}", [8, NCH, 8], F32))
                        t_x = TU("omx")
                        fw.op("dve", lambda e, X=X, om=om: e.tensor_tensor(out=X[:], in0=om.unsqueeze(2).to_broadcast([8, NCH, 8]), in1=ident_f[0:8, 0:8].unsqueeze(1).to_broadcast([8, NCH, 8]), op=ALU.mult), reads=[t_s, t_cst], writes=[t_x])
                        for half in range(2):
                            ps, t_ps = PS.next()
                            hw = (NCH // 2) * 8
                            fw.op("pe", lambda e, ps=ps, X=X, half=half, hw=hw: e.matmul(ps[0:64, 0:hw], lhsT=ones_f[0:8, 0:64], rhs=X[:].rearrange("p c h -> p (c h)")[:, half * hw:(half + 1) * hw], start=True, stop=True), reads=[t_x, t_cst], writes=[t_ps])
                            fw.op("dve", lambda e, ps=ps, half=half, hw=hw, d=d: e.tensor_copy(out=omb[:, d, half * (NCH // 2):(half + 1) * (NCH // 2), :], in_=ps[0:64, 0:hw].rearrange("p (c h) -> p c h", h=8)), reads=[t_ps], writes=[t_omb])
                    fw.barrier()
                for nm_, tl_, tt_ in (("etm", etm, t_etm), ("ctm", ctm, t_ctm), ("omb", omb, t_omb)):
                    if nm_ in dbg:
                        fw.dma("sp", dbg[nm_].ap, tl_[:].rearrange("p d c h -> p (d c h)"), reads=[tt_], writes=dbg[nm_].tu())
                with ExitStack() as sd2:
                    QC = Ring(nc, sd2, "qc", [64, 8, 64], BF16, 4)
                    KC = Ring(nc, sd2, "kc", [64, 8, 64], BF16, 4)
                    KM = Ring(nc, sd2, "km", [64, 8, 64], BF16, 4)
                    VA = Ring(nc, sd2, "va", [64, 8, 130], BF16, 4)
                    SMF = Ring(nc, sd2, "smf", [64, 8, 64], F32, 4)
                    SM = Ring(nc, sd2, "sm", [64, 8, 64], BF16, 4)
                    KP = Ring(nc, sd2, "kp", [64, 8, 64], BF16, 4)
                    HO = Ring(nc, sd2, "ho", [64, 8, 128], F32, 4)
                    DEN = Ring(nc, sd2, "den", [64, 8], F32, 4)
                    for (va, t_va) in VA.bufs:
                        fw.op("pool", lambda e, va=va: e.memset(va[:, :, 128:130], 1.0), writes=[t_va])
                    st_d = []
                    for d in range(2):
                        C32 = sd2.enter_context(SBT(f"C32_{d}", [64, 8, 129], F32))
                        Cb = sd2.enter_context(SBT(f"Cb_{d}", [64, 8, 129], BF16))
                        t_C, t_Cb = TU("C32"), TU("Cb")
                        fw.op("pool", lambda e, C32=C32: e.memset(C32[:], 0.0), writes=[t_C])
                        fw.op("pool", lambda e, Cb=Cb: e.memset(Cb[:], 0.0), writes=[t_Cb])
                        mask = (m_le if d == 0 else m_ge)[0:64, 0:64]
                        st_d.append((C32, Cb, t_C, t_Cb, mask))
                    for i in range(NCH):
                        for d in range(2):
                            C32, Cb, t_C, t_Cb, mask = st_d[d]
                            order = orders[d]
                            c = order[i]
                            t0 = c * 64
                            qc, t_qc = QC.next(); kc, t_kc = KC.next(); km, t_km = KM.next(); va, t_va = VA.next()
                            fw.dma("sp", qc[:], S["qT"].ap[:, t0:t0 + 64].rearrange("(h d) t -> d h t", d=64), reads=[x for r0 in range(0, 512, 128) for x in S["qT"].tu(t0, 64, key=r0)], writes=[t_qc])
                            fw.dma("sp", kc[:], S["kT"].ap[:, t0:t0 + 64].rearrange("(h d) t -> d h t", d=64), reads=[x for r0 in range(0, 512, 128) for x in S["kT"].tu(t0, 64, key=r0)], writes=[t_kc])
                            fw.dma("sp", km[:].rearrange("p h d -> p (h d)"), S["ktm"].ap[t0:t0 + 64, :], reads=S["ktm"].tu(t0, 64, key=0), writes=[t_km])
                            fw.dma("sp", va[:, :, 0:128], S["vtm"].ap[t0:t0 + 64, :].rearrange("t (h v) -> t h v", v=128), reads=S["vtm"].tu(t0, 64, key=0) + S["vtm"].tu(t0, 64, key=512), writes=[t_va])
                            ps1, t_ps1 = PS.next()
                            for h in range(8):
                                fw.op("pe", lambda e, ps1=ps1, kc=kc, qc=qc, h=h: e.matmul(ps1[0:64, h * 64:(h + 1) * 64], lhsT=kc[:, h, :], rhs=qc[:, h, :], start=True, stop=True), reads=[t_kc, t_qc], writes=[t_ps1])
                            smf, t_smf = SMF.next(); sm, t_sm = SM.next(); kp, t_kp = KP.next()
                            eb = etm[:, d, c, :].unsqueeze(2).to_broadcast([64, 8, 64])
                            fw.op("dve", lambda e, smf=smf, ps1=ps1, eb=eb: e.tensor_tensor(out=smf[:], in0=ps1[0:64, :].rearrange("p (h t) -> p h t", t=64), in1=eb, op=ALU.mult), reads=[t_ps1, t_etm], writes=[t_smf])
                            fw.op("dve", lambda e, smf=smf, sm=sm, mask=mask: e.tensor_tensor(out=sm[:], in0=smf[:], in1=mask.unsqueeze(1).to_broadcast([64, 8, 64]), op=ALU.mult), reads=[t_smf, t_cst], writes=[t_sm])
                            fw.op("dve", lambda e, kp=kp, km=km, eb=eb: e.tensor_tensor(out=kp[:], in0=km[:], in1=eb, op=ALU.mult), reads=[t_km, t_etm], writes=[t_kp])
                            groups3 = [(0, 3), (3, 3), (6, 2)]
                            psn = [PS.next() for _ in range(3)]
                            for gi, (h0, nh) in enumerate(groups3):
                                for j in range(nh):
                                    h = h0 + j
                                    fw.op("pe", lambda e, p=psn[gi][0], sm=sm, va=va, h=h, j=j: e.matmul(p[0:64, j * 129:(j + 1) * 129], lhsT=sm[:, h, :], rhs=va[:, h, 0:129], start=True, stop=False), reads=[t_sm, t_va], writes=[psn[gi][1]])
                                    fw.op("pe", lambda e, p=psn[gi][0], qc=qc, Cb=Cb, h=h, j=j: e.matmul(p[0:64, j * 129:(j + 1) * 129], lhsT=qc[:, h, :], rhs=Cb[:, h, :], start=False, stop=True), reads=[t_qc, t_Cb], writes=[psn[gi][1]])
                            ho, t_ho = HO.next(); den, t_den = DEN.next()
                            for gi, (h0, nh) in enumerate(groups3):
                                pv = psn[gi][0][0:64, 0:nh * 129].rearrange("p (h n) -> p h n", n=129)
                                fw.op("act", lambda e, den=den, pv=pv, h0=h0, nh=nh: e.activation(out=den[:, h0:h0 + nh].unsqueeze(2), in_=pv[:, :, 128:129], func=AF.Abs), reads=[psn[gi][1]], writes=[t_den])
                            fw.op("dve", lambda e, den=den, d=d, c=c: e.tensor_tensor(out=den[:], in0=den[:], in1=ctm[:, d, c, :], op=ALU.max), reads=[t_den, t_ctm], writes=[t_den])
                            fw.op("dve", lambda e, den=den: e.reciprocal(out=den[:], in_=den[:]), reads=[t_den], writes=[t_den])
                            for gi, (h0, nh) in enumerate(groups3):
                                pv = psn[gi][0][0:64, 0:nh * 129].rearrange("p (h n) -> p h n", n=129)
                                fw.op("dve", lambda e, ho=ho, den=den, pv=pv, h0=h0, nh=nh: e.tensor_tensor(out=ho[:, h0:h0 + nh, :], in0=pv[:, :, 0:128], in1=den[:, h0:h0 + nh].unsqueeze(2).to_broadcast([64, nh, 128]), op=ALU.mult), reads=[psn[gi][1], t_den], writes=[t_ho])
                            fw.dma("pool", S["hm"].ap[d, t0:t0 + 64, :], ho[:].rearrange("p h v -> p (h v)"), reads=[t_ho], writes=S["hm"].tu(t0, 64, key=d))
                            if i + 1 < len(order):
                                psc = [PS.next() for _ in range(3)]
                                for gi, (h0, nh) in enumerate(groups3):
                                    for j in range(nh):
                                        h = h0 + j
                                        fw.op("pe", lambda e, p=psc[gi][0], kp=kp, va=va, h=h, j=j: e.matmul(p[0:64, j * 129:(j + 1) * 129], lhsT=kp[:, h, :], rhs=va[:, h, 0:129], start=True, stop=True), reads=[t_kp, t_va], writes=[psc[gi][1]])
                                for gi, (h0, nh) in enumerate(groups3):
                                    pv = psc[gi][0][0:64, 0:nh * 129].rearrange("p (h n) -> p h n", n=129)
                                    fw.op("dve", lambda e, C32=C32, pv=pv, h0=h0, nh=nh: e.tensor_tensor(out=C32[:, h0:h0 + nh, :], in0=pv, in1=C32[:, h0:h0 + nh, :], op=ALU.add), reads=[psc[gi][1], t_C], writes=[t_C])
                                cn = order[i + 1]
                                fw.op("dve", lambda e, C32=C32, cn=cn, d=d: e.tensor_tensor(out=C32[:], in0=C32[:], in1=omb[:, d, cn, :].unsqueeze(2).to_broadcast([64, 8, 129]), op=ALU.mult), reads=[t_C, t_omb], writes=[t_C])
                                fw.op("act", lambda e, C32=C32, Cb=Cb: e.activation(out=Cb[:], in_=C32[:], func=AF.Copy), reads=[t_C], writes=[t_Cb])
                    fw.barrier()
                with ExitStack() as sd3:
                    gmb = sd3.enter_context(SBT("gmb", [128, 1024], F32))
                    t_gmb = TU("gmb")
                    fw.dma("sp", gmb[:], I["g_mlstm_norm"].ap[l].partition_broadcast(128), writes=[t_gmb])
                    HF = Ring(nc, sd3, "hf", [128, 1024], F32, 2)
                    HB = Ring(nc, sd3, "hb", [128, 1024], F32, 2)
                    SO = Ring(nc, sd3, "so", [128, 1024], BF16, 2)
                    SQ = Ring(nc, sd3, "hsq", [128, 1024], F32, 1)
                    SS = Ring(nc, sd3, "hss", [128, 8], F32, 2)
                    MO = Ring(nc, sd3, "mo", [128, 1024], BF16, 2)
                    MT = Ring(nc, sd3, "mt", [128, 8, 128], BF16, 2)
                    for tt in range(NT // 128):
                        t0 = tt * 128
                        hf, t_hf = HF.next(); hb, t_hb = HB.next(); so, t_so = SO.next(); sq, t_sq = SQ.next(); ss, t_ss = SS.next(); mo, t_mo = MO.next(); mt, t_mt = MT.next()
                        fw.dma("sp", hf[:], S["hm"].ap[0, t0:t0 + 128, :], reads=S["hm"].tu(t0, 128, key=0), writes=[t_hf])
                        fw.dma("sp", hb[:], S["hm"].ap[1, t0:t0 + 128, :], reads=S["hm"].tu(t0, 128, key=1), writes=[t_hb])
                        fw.dma("sp", so[:], S["otm"].ap[t0:t0 + 128, :], reads=S["otm"].tu(t0, 128, key=0) + S["otm"].tu(t0, 128, key=512), writes=[t_so])
                        fw.op("dve", lambda e, hf=hf, hb=hb: e.tensor_tensor(out=hf[:], in0=hf[:], in1=hb[:], op=ALU.add), reads=[t_hf, t_hb], writes=[t_hf])
                        fw.op("act", lambda e, sq=sq, hf=hf: e.activation(out=sq[:], in_=hf[:], func=AF.Square), reads=[t_hf], writes=[t_sq])
                        fw.op("dve", lambda e, ss=ss, sq=sq: e.tensor_reduce(out=ss[:], in_=sq[:].rearrange("p (h v) -> p h v", v=128), axis=AX.X, op=ALU.add), reads=[t_sq], writes=[t_ss])
                        fw.op("act", lambda e, ss=ss: e.activation(out=ss[:], in_=ss[:], func=AF.Sqrt, scale=1.0 / 128, bias=1e-6), reads=[t_ss], writes=[t_ss])
                        fw.op("dve", lambda e, ss=ss: e.reciprocal(out=ss[:], in_=ss[:]), reads=[t_ss], writes=[t_ss])
                        hf3 = hf[:].rearrange("p (h v) -> p h v", v=128)
                        fw.op("dve", lambda e, hf3=hf3, ss=ss: e.tensor_tensor(out=hf3, in0=hf3, in1=ss[:].unsqueeze(2).to_broadcast([128, 8, 128]), op=ALU.mult), reads=[t_hf, t_ss], writes=[t_hf])
                        fw.op("pool", lambda e, hf=hf: e.tensor_tensor(out=hf[:], in0=hf[:], in1=gmb[:], op=ALU.mult), reads=[t_hf, t_gmb], writes=[t_hf])
                        fw.op("dve", lambda e, hf=hf, so=so, mo=mo: e.tensor_tensor(out=mo[:], in0=hf[:], in1=so[:], op=ALU.mult), reads=[t_hf, t_so], writes=[t_mo])
                        if "motm" in dbg:
                            fw.dma("pool", dbg["motm"].ap[t0:t0 + 128, :], mo[:], reads=[t_mo], writes=dbg["motm"].tu(t0, 128))
                        for hb2 in range(2):
                            ps, t_ps = PS.next()
                            for j in range(4):
                                cb = hb2 * 4 + j
                                fw.op("pe", lambda e, ps=ps, mo=mo, cb=cb, j=j: e.matmul(ps[:, j * 128:(j + 1) * 128], lhsT=mo[:, cb * 128:(cb + 1) * 128], rhs=ident_b, start=True, stop=True), reads=[t_mo, t_cst], writes=[t_ps])
                            fw.op("act", lambda e, ps=ps, mt=mt, hb2=hb2: e.activation(out=mt[:, hb2 * 4:(hb2 + 1) * 4, :], in_=ps[:].rearrange("p (c t) -> p c t", t=128), func=AF.Copy), reads=[t_ps], writes=[t_mt])
                        fw.dma("pool", S["moT"].ap[:, t0:t0 + 128].rearrange("(c p) t -> p c t", p=128), mt[:], reads=[t_mt], writes=S["moT"].tu(t0, 128))
                    fw.barrier()
            if stop == "D":
                break
            NC2 = NT // 128

            def TT(eng, out, in0, in1, op, reads, writes):
                fw.op(eng, "tensor_tensor", reads, writes, out=out, in0=in0, in1=in1, op=op)

            def TS(eng, out, in0, s1, s2, op0, op1, reads, writes):
                if op1 is None:
                    fw.op(eng, "tensor_scalar", reads, writes, out=out, in0=in0, scalar1=s1, scalar2=None, op0=op0)
                else:
                    fw.op(eng, "tensor_scalar", reads, writes, out=out, in0=in0, scalar1=s1, scalar2=s2, op0=op0, op1=op1)

            def STT(eng, out, in0, scalar, in1, op0, op1, reads, writes):
                fw.op(eng, "scalar_tensor_tensor", reads, writes, out=out, in0=in0, scalar=scalar, in1=in1, op0=op0, op1=op1)

            def ACT(out, in_, func, reads, writes, **kw):
                fw.op("act", "activation", reads, writes, out=out, in_=in_, func=func, **kw)

            def MM(out, lhsT, rhs, reads, writes, start=True, stop=True):
                fw.op("pe", "matmul", reads, writes, out=out, lhsT=lhsT, rhs=rhs, start=start, stop=stop)

            if l == 0:
                for nm_ in ("abT", "rbT", "bbT", "kbT"):
                    scr(nm_, [2, D, NT], BF16)
                scr("Khat", [2, NT, D], BF16); scr("Bhat", [2, NT, D], BF16); scr("v_tm", [NT, D], BF16)
                scr("gdT", [128, NT], BF16); scr("yr", [2, NT, D], F32)
                scr("Tinv", [2, NC2, 128, 16, 128], BF16); scr("roT", [D, NT], BF16)
            with ExitStack() as se:
                plb = se.enter_context(SBT("plb", [128, 2, 8, NC2], F32))
                bon = se.enter_context(SBT("bon", [128, NC2, 16], F32))
                t_plb, t_bon = TU("plb"), TU("bon")
                with ExitStack() as se1:
                    def colload(name, src1d, ncol):
                        t = se1.enter_context(SBT(name, [128, ncol], F32))
                        tu_ = TU(name)
                        fw.dma("sp", t[:], src1d.rearrange("(j p) -> p j", p=128), writes=[tu_], allow_slow_non_contiguous=True)
                        return t, tu_
                    mu, t_mu = colload("mu_sb", I["mu_shift"].ap[l], 27)
                    a1 = se1.enter_context(SBT("a1_sb", [128, 27], F32))
                    TS("dve", a1[:], mu[:], -1.0, 1.0, ALU.mult, ALU.add, [t_mu], [t_mu])
                    kk_c, t_kkc = colload("kk_c", I["k_k"].ap[l], 8)
                    ka_c, t_kac = colload("ka_c", I["k_a"].ap[l], 8)
                    nka_c = se1.enter_context(SBT("nka_c", [128, 8], F32))
                    TS("dve", nka_c[:], ka_c[:], -1.0, None, ALU.mult, None, [t_kac], [t_kac])
                    rk_c, t_rkc = colload("rk_c", I["r_k"].ap[l], 8)
                    w0_c = [colload(f"w0_c{d}", I["w0"].ap[l, d], 8) for d in range(2)]
                    a0_c = [colload(f"a0_c{d}", I["a0"].ap[l, d], 8) for d in range(2)]
                    w2b = se1.enter_context(SBT("w2b", [128, D], BF16))
                    a2b = se1.enter_context(SBT("a2b", [128, D], BF16))
                    t_w2, t_a2 = TU("w2"), TU("a2")
                    with ExitStack() as se0:
                        w2f = se0.enter_context(SBT("w2f", [128, D], F32))
                        a2f = se0.enter_context(SBT("a2f", [128, D], F32))
                        fw.dma("sp", w2f[:], I["w2"].ap[l].rearrange("d k c -> (d k) c"), writes=[t_w2])
                        fw.dma("sp", a2f[:], I["a2"].ap[l].rearrange("d k c -> (d k) c"), writes=[t_a2])
                        fw.op("dve", "tensor_copy", [t_w2], [t_w2], out=w2b[:], in_=w2f[:])
                        fw.op("dve", "tensor_copy", [t_a2], [t_a2], out=a2b[:], in_=a2f[:])
                        fw.barrier()
                    rmask2 = se1.enter_context(SBT("rmask2", [128, NT // 2], F32))
                    t_rm2 = TU("rm2")
                    fw.op("pool", "memset", [], [t_rm2], ap=rmask2[:], constant=1.0)
                    fw.op("pool", "memset", [], [t_rm2], ap=rmask2[:].rearrange("p (c l) -> p c l", l=128)[:, :, 0:1], constant=0.0)
                    NP = NT // 2
                    Pt = se1.enter_context(SBT("Pt", [128, NT], BF16)); t_P = TU("Pt")
                    SH = se1.enter_context(SBT("SH", [128, NP], F32)); t_SH = TU("SH")
                    wdT = se1.enter_context(SBT("wdT", [128, NT], BF16)); t_wd = TU("wdT")
                    adT = se1.enter_context(SBT("adT", [128, NT], BF16)); t_ad = TU("adT")
                    XV = se1.enter_context(SBT("XV", [128, NP], BF16)); t_XV = TU("XV")
                    XR = se1.enter_context(SBT("XR", [128, NP], F32)); t_XR = TU("XR")
                    XK = se1.enter_context(SBT("XK", [128, NP], F32)); t_XK = TU("XK")
                    KK = se1.enter_context(SBT("KK", [128, NP], F32)); t_KK = TU("KK")
                    LW = se1.enter_context(SBT("LW", [128, NP], F32)); t_LW = TU("LW")
                    CL = se1.enter_context(SBT("CL", [128, NP], F32)); t_CL = TU("CL")
                    AA = se1.enter_context(SBT("AA", [128, NP], F32)); t_AA = TU("AA")
                    KT = se1.enter_context(SBT("KT", [128, NP], F32)); t_KT = TU("KT")
                    KS = se1.enter_context(SBT("KS", [128, NP], F32)); t_KS = TU("KS")
                    EE = se1.enter_context(SBT("EE", [128, NP], F32)); t_EE = TU("EE")
                    E2 = se1.enter_context(SBT("E2", [128, NP], F32)); t_E2 = TU("E2")
                    TOT = se1.enter_context(SBT("TOT", [128, NC2 // 2], F32)); t_TOT = TU("TOT")
                    OUT = Ring(nc, se1, "rout", [128, NP], BF16, 3)
                    RN = Ring(nc, se1, "rn", [128, 512], F32, 2)
                    TRB = Ring(nc, se1, "trb", [128, 4, 128], BF16, 3)

                    def shift_lerp(j, p0, dst, t_dst, eng="pool"):
                        rngs = []
                        a = 0
                        while a < 128:
                            qd = (j * 128 + a) // 864
                            b = min(128, (qd + 1) * 864 - j * 128)
                            while a < b:
                                mx = {0: 128, 32: 32, 64: 64, 96: 32}[a]
                                e_ = min(b, a + mx)
                                rngs.append((a, e_, qd))
                                a = e_
                        for (a, b, qd) in rngs:
                            muc = mu[a:b, j:j + 1]
                            if p0 == 0:
                                off = -1 if qd < 2 else 1
                                lo, hi = (1, NCTX) if off < 0 else (0, NCTX - 1)
                                TS(eng, SH[a:b, lo:hi], Pt[a:b, lo + off:hi + off], muc, None, ALU.mult, None, [t_P, t_mu], [t_SH])
                                z = 0 if off < 0 else NCTX - 1
                                fw.op(eng, "memset", [], [t_SH], ap=SH[a:b, z:z + 1], constant=0.0)
                            off = (-1, 1, -64, 64)[qd]
                            lo = max(p0, NCTX); hi = p0 + NP
                            if off > 0:
                                hi = min(hi, NT - off)
                            ACT(SH[a:b, lo - p0:hi - p0], Pt[a:b, lo + off:hi + off], AF.Copy, [t_P, t_mu], [t_SH], scale=muc)
                            l0 = max(p0, NCTX) - p0
                            lat = SH[a:b, l0:NP].rearrange("p (r c) -> p r c", c=64)
                            if qd == 0:
                                fw.op(eng, "memset", [], [t_SH], ap=lat[:, :, 0:1], constant=0.0)
                            elif qd == 1:
                                fw.op(eng, "memset", [], [t_SH], ap=lat[:, :, 63:64], constant=0.0)
                            elif qd == 2 and p0 == 0:
                                fw.op(eng, "memset", [], [t_SH], ap=SH[a:b, l0:l0 + 64], constant=0.0)
                            elif qd == 3 and p0 + NP == NT:
                                fw.op(eng, "memset", [], [t_SH], ap=SH[a:b, NP - 64:NP], constant=0.0)
                        STT("dve", dst, Pt[:, p0:p0 + NP], a1[:, j:j + 1], SH[:], ALU.mult, ALU.add, [t_P, t_SH, t_mu], [t_dst])

                    def transpose_store(src, t_src, dram_ap_fn, tus_fn, p0):
                        for g in range(0, NP // 128, 4):
                            ng = min(4, NP // 128 - g)
                            ps, t_ps = PS.next()
                            for jj in range(ng):
                                MM(ps[:, jj * 128:(jj + 1) * 128], src[:, (g + jj) * 128:(g + jj + 1) * 128], ident_b, [t_src, t_cst], [t_ps])
                            tb, t_tb = TRB.next()
                            ACT(tb[:, 0:ng, :], ps[:, 0:ng * 128].rearrange("p (j c) -> p j c", c=128), AF.Copy, [t_ps], [t_tb])
                            tg = p0 + g * 128
                            fw.dma("pool", dram_ap_fn(tg, ng * 128).rearrange("(j t) c -> t j c", t=128), tb[:, 0:ng, :], reads=[t_tb],
                                   writes=[x for jj in range(ng) for x in tus_fn(tg + jj * 128)])

                    for j, (dstT, t_d, func) in ((24, (wdT, t_wd, AF.Tanh)), (25, (adT, t_ad, AF.Copy)), (26, (None, None, AF.Sigmoid))):
                        fw.dma("sp", Pt[:], S["pT"].ap[j * 128:(j + 1) * 128, :], reads=[x for r0 in range(3072, 3456, 128) for x in S["pT"].tu(key=r0)], writes=[t_P])
                        for p0 in (0, NP):
                            shift_lerp(j, p0, EE[:], t_EE)
                            if dstT is not None:
                                ACT(dstT[:, p0:p0 + NP], EE[:], func, [t_EE], [t_d])
                            else:
                                ob, t_ob = OUT.next()
                                ACT(ob[:], EE[:], func, [t_EE], [t_ob])
                                fw.dma("pool", S["gdT"].ap[:, p0:p0 + NP], ob[:], reads=[t_ob], writes=S["gdT"].tu(p0, NP))
                    for cb in range(8):
                        for p0 in (0, NP):
                            pi = p0 // NP
                            ntile = [(q0, min(512, NP - q0)) for q0 in range(0, NP, 512)]
                            fw.dma("sp", Pt[:], S["pT"].ap[(16 + cb) * 128:(17 + cb) * 128, :], reads=[x for r0 in range(0, 3456, 128) for x in S["pT"].tu(key=r0)], writes=[t_P])
                            shift_lerp(16 + cb, p0, XV[:], t_XV)
                            transpose_store(XV, t_XV, lambda t0, nr, cb=cb: S["v_tm"].ap[t0:t0 + nr, cb * 128:(cb + 1) * 128], lambda t0, cb=cb: S["v_tm"].tu(t0, 128, key=cb), p0)
                            fw.dma("sp", Pt[:], S["pT"].ap[cb * 128:(cb + 1) * 128, :], reads=S["pT"].tu(key=0), writes=[t_P])
                            shift_lerp(cb, p0, XR[:], t_XR)
                            fw.dma("sp", Pt[:], S["pT"].ap[(8 + cb) * 128:(9 + cb) * 128, :], reads=S["pT"].tu(key=0), writes=[t_P])
                            shift_lerp(8 + cb, p0, XK[:], t_XK)
                            ACT(KK[:], XK[:], AF.Copy, [t_XK, t_kkc], [t_KK], scale=kk_c[:, cb:cb + 1])
                            ob, t_ob = OUT.next()
                            ACT(ob[:], KK[:], AF.Square, [t_KK], [t_ob])
                            for (q0, qn) in ntile:
                                ps, t_ps = PS.next()
                                MM(ps[:, :qn], blk1_b, ob[:, q0:q0 + qn], [t_ob, t_cst], [t_ps])
                                rn, t_rn = RN.next()
                                ACT(rn[:, :qn], ps[:, :qn], AF.Sqrt, [t_ps], [t_rn])
                                TS("dve", rn[:, :qn], rn[:, :qn], 1e-12, None, ALU.max, None, [t_rn], [t_rn])
                                fw.op("dve", "reciprocal", [t_rn], [t_rn], out=rn[:, :qn], in_=rn[:, :qn])
                                TT("dve", KK[:, q0:q0 + qn], KK[:, q0:q0 + qn], rn[:, :qn], ALU.mult, [t_KK, t_rn], [t_KK])
                            for d in range(2):
                                w0c, t_w0c = w0_c[d]; a0c, t_a0c = a0_c[d]
                                for (q0, qn) in ntile:
                                    ps, t_ps = PS.next()
                                    MM(ps[:, :qn], w2b[d * 64:(d + 1) * 64, cb * 128:(cb + 1) * 128], wdT[d * 64:(d + 1) * 64, p0 + q0:p0 + q0 + qn], [t_w2, t_wd], [t_ps])
                                    ACT(LW[:, q0:q0 + qn], ps[:, :qn], AF.Sigmoid, [t_ps, t_w0c], [t_LW], bias=w0c[:, cb:cb + 1])
                                    ps, t_ps = PS.next()
                                    MM(ps[:, :qn], a2b[d * 64:(d + 1) * 64, cb * 128:(cb + 1) * 128], adT[d * 64:(d + 1) * 64, p0 + q0:p0 + q0 + qn], [t_a2, t_ad], [t_ps])
                                    ACT(AA[:, q0:q0 + qn], ps[:, :qn], AF.Sigmoid, [t_ps, t_a0c], [t_AA], bias=a0c[:, cb:cb + 1])
                                fw.op("dve", "tensor_tensor_scan", [t_rm2, t_LW], [t_CL], out=CL[:], data0=rmask2[:], data1=LW[:], initial=0.0, op0=ALU.mult, op1=ALU.add)
                                cl3 = CL[:].rearrange("p (c l) -> p c l", l=128)
                                fw.op("dve", "tensor_copy", [t_CL], [t_TOT], out=TOT[:].unsqueeze(2), in_=cl3[:, :, 127:128])
                                totb = TOT[:].unsqueeze(2).to_broadcast([128, NC2 // 2, 128])
                                if d == 1:
                                    TT("dve", CL[:], LW[:], CL[:], ALU.subtract, [t_LW, t_CL], [t_CL])
                                    TT("dve", cl3, cl3, totb, ALU.add, [t_CL, t_TOT], [t_CL])
                                ACT(plb[:, d, cb, pi * (NC2 // 2):(pi + 1) * (NC2 // 2)], TOT[:], AF.Exp, [t_TOT], [t_plb], scale=-LAM)
                                ACT(EE[:], AA[:], AF.Identity, [t_AA, t_kac], [t_EE], scale=ka_c[:, cb:cb + 1], bias=nka_c[:, cb:cb + 1])
                                STT("dve", KT[:], EE[:], 1.0, XK[:], ALU.add, ALU.mult, [t_EE, t_XK], [t_KT])
                                if d == 0:
                                    ACT(KS[:], KT[:], AF.Copy, [t_KT], [t_KS])
                                else:
                                    TT("pool", KS[:], KS[:], KT[:], ALU.add, [t_KT, t_KS], [t_KS])
                                TT("dve", EE[:], CL[:], LW[:], ALU.subtract, [t_CL, t_LW], [t_EE])
                                ACT(EE[:], EE[:], AF.Exp, [t_EE], [t_EE], scale=-LAM)
                                ob, t_ob = OUT.next()
                                TT("dve", ob[:], KK[:], EE[:], ALU.mult, [t_KK, t_EE], [t_ob])
                                fw.dma("pool", S["abT"].ap[d, cb * 128:(cb + 1) * 128, p0:p0 + NP], ob[:], reads=[t_ob], writes=S["abT"].tu(p0, NP, key=(d, cb)))
                                ACT(EE[:], CL[:], AF.Exp, [t_CL], [t_EE], scale=-LAM)
                                ob, t_ob = OUT.next()
                                TT("dve", ob[:], XR[:], EE[:], ALU.mult, [t_XR, t_EE], [t_ob])
                                fw.dma("pool", S["rbT"].ap[d, cb * 128:(cb + 1) * 128, p0:p0 + NP], ob[:], reads=[t_ob], writes=S["rbT"].tu(p0, NP, key=(d, cb)))
                                TT("pool", LW[:], KK[:], AA[:], ALU.mult, [t_KK, t_AA], [t_LW])
                                ACT(EE[:], CL[:], AF.Exp, [t_CL], [t_EE], scale=LAM)
                                ob, t_ob = OUT.next()
                                TT("dve", ob[:], LW[:], EE[:], ALU.mult, [t_LW, t_EE], [t_ob])
                                fw.dma("pool", S["bbT"].ap[d, cb * 128:(cb + 1) * 128, p0:p0 + NP], ob[:], reads=[t_ob], writes=S["bbT"].tu(p0, NP, key=(d, cb)))
                                transpose_store(ob, t_ob, lambda t0, nr, cb=cb, d=d: S["Bhat"].ap[d, t0:t0 + nr, cb * 128:(cb + 1) * 128], lambda t0, cb=cb, d=d: S["Bhat"].tu(t0, 128, key=(d, cb)), p0)
                                ob, t_ob = OUT.next()
                                TT("dve", ob[:], KT[:], EE[:], ALU.mult, [t_KT, t_EE], [t_ob])
                                fw.dma("pool", S["kbT"].ap[d, cb * 128:(cb + 1) * 128, p0:p0 + NP], ob[:], reads=[t_ob], writes=S["kbT"].tu(p0, NP, key=(d, cb)))
                                transpose_store(ob, t_ob, lambda t0, nr, cb=cb, d=d: S["Khat"].ap[d, t0:t0 + nr, cb * 128:(cb + 1) * 128], lambda t0, cb=cb, d=d: S["Khat"].tu(t0, 128, key=(d, cb)), p0)
                            STT("dve", EE[:], XR[:], rk_c[:, cb:cb + 1], KS[:], ALU.mult, ALU.mult, [t_XR, t_KS, t_rkc], [t_EE])
                            ps, t_ps = PS.next()
                            for tt in range(NP // 128):
                                MM(ps[:, 2 * tt:2 * tt + 2], EE[:, tt * 128:(tt + 1) * 128], sel2_f, [t_EE, t_cst], [t_ps])
                            fw.op("dve", "tensor_copy", [t_ps], [t_bon], out=bon[:, pi * (NP // 128):(pi + 1) * (NP // 128), 2 * cb:2 * cb + 2], in_=ps[:, 0:2 * (NP // 128)].rearrange("p (t h) -> p t h", h=2))
                    fw.barrier()
                if stop == "E1":
                    for nm_, tl_, tt_ in (("plb", plb, t_plb), ("bon", bon, t_bon)):
                        if nm_ in dbg:
                            fw.dma("sp", dbg[nm_].ap, tl_[:].rearrange("p a b c -> p (a b c)") if nm_ == "plb" else tl_[:].rearrange("p a b -> p (a b)"), reads=[tt_], writes=dbg[nm_].tu())
                    break
                with ExitStack() as se2:
                    AB = Ring(nc, se2, "e2ab", [128, 8, 128], BF16, 2)
                    BB = Ring(nc, se2, "e2bb", [128, 8, 128], BF16, 2)
                    YP = Ring(nc, se2, "e2yp", [128, 2, 256], BF16, 12)
                    YT = Ring(nc, se2, "e2yt", [128, 2, 128], BF16, 12)
                    TO = Ring(nc, se2, "e2to", [128, 16, 128], BF16, 2)
                    bank = lambda b: (psall[:, b * 512:(b + 1) * 512], PS.tus[b])
                    evi2 = 0
                    for d in range(2):
                        mk_s = m_lt if d == 0 else m_gt
                        mk_t = m_gt if d == 0 else m_lt
                        for c in range(NC2):
                            t0 = c * 128
                            ab, t_ab = AB.next(); bb, t_bb = BB.next(); to, t_to = TO.next()
                            fw.dma("sp", ab[:], S["abT"].ap[d, :, t0:t0 + 128].rearrange("(cb p) t -> p cb t", p=128), reads=[x for cb_ in range(8) for x in S["abT"].tu(t0, 128, key=(d, cb_))], writes=[t_ab])
                            fw.dma("sp", bb[:], S["bbT"].ap[d, :, t0:t0 + 128].rearrange("(cb p) t -> p cb t", p=128), reads=[x for cb_ in range(8) for x in S["bbT"].tu(t0, 128, key=(d, cb_))], writes=[t_bb])
                            for g in range(2):
                                chains = []
                                for k in range(4):
                                    cb = 4 * g + k
                                    yp, t_yp = YP.next(); yt, t_yt = YT.next()
                                    ps, t_ps = PS.pair(2 * k)
                                    for hh in range(2):
                                        pb = hh * 64
                                        MM(ps[:, hh, 0:128], bb[pb:pb + 64, cb, :], ab[pb:pb + 64, cb, :], [t_ab, t_bb], [t_ps[hh]])
                                        MM(ps[:, hh, 128:256], ab[pb:pb + 64, cb, :], bb[pb:pb + 64, cb, :], [t_ab, t_bb], [t_ps[hh]])
                                    STT("dve", yp[:, :, 0:128], ps[:, :, 0:128], -1.0, mk_s.unsqueeze(1).to_broadcast([128, 2, 128]), ALU.mult, ALU.mult, t_ps + [t_cst], [t_yp])
                                    STT("dve", yt[:], ps[:, :, 128:256], -1.0, mk_t.unsqueeze(1).to_broadcast([128, 2, 128]), ALU.mult, ALU.mult, t_ps + [t_cst], [t_yt])
                                    chains.append([cb, yp, t_yp, yt, t_yt])
                                for lev in range(7):
                                    last = lev == 6
                                    nxt = []
                                    for k in range(4):
                                        cb, yp, t_yp, yt, t_yt = chains[k]
                                        pa, t_pa = bank(2 * k)
                                        pbb, t_pbb = bank(2 * k + 1)
                                        for hh in range(2):
                                            if lev == 0:
                                                MM(pa[:, hh * 256:hh * 256 + 128], yt[:, hh, :], yp[:, hh, 0:128], [t_yt, t_yp], [t_pa])
                                                MM(pa[:, hh * 256 + 128:(hh + 1) * 256], yt[:, hh, :], ident_b, [t_yt, t_cst], [t_pa])
                                                MM(pbb[:, hh * 128:(hh + 1) * 128], yp[:, hh, 0:128], yt[:, hh, :], [t_yt, t_yp], [t_pbb])
                                            elif not last:
                                                MM(pa[:, hh * 256:(hh + 1) * 256], yt[:, hh, :], yp[:, hh, :], [t_yt, t_yp], [t_pa])
                                                MM(pbb[:, hh * 128:(hh + 1) * 128], yp[:, hh, 0:128], yt[:, hh, :], [t_yt, t_yp], [t_pbb])
                                            else:
                                                MM(pa[:, hh * 256 + 128:(hh + 1) * 256], yt[:, hh, :], yp[:, hh, 128:256], [t_yt, t_yp], [t_pa])
                                    for k in range(4):
                                        cb, yp, t_yp, yt, t_yt = chains[k]
                                        pa, t_pa = bank(2 * k)
                                        pbb, t_pbb = bank(2 * k + 1)
                                        pav = pa.rearrange("p (h x) -> p h x", x=256)
                                        if not last:
                                            ypn, t_ypn = YP.next(); ytn, t_ytn = YT.next()
                                            ACT(ypn[:, :, 0:128], pav[:, :, 0:128], AF.Copy, [t_pa], [t_ypn])
                                            if lev == 0:
                                                TT("dve", ypn[:, :, 128:256], pav[:, :, 128:256], ident_b.unsqueeze(1).to_broadcast([128, 2, 128]), ALU.add, [t_pa, t_cst], [t_ypn])
                                            else:
                                                TT("dve", ypn[:, :, 128:256], pav[:, :, 128:256], yp[:, :, 128:256], ALU.add, [t_pa, t_yp], [t_ypn])
                                            evi2 += 1
                                            if evi2 % 2 == 0:
                                                ACT(ytn[:], pbb[:, 0:256].rearrange("p (h x) -> p h x", x=128), AF.Copy, [t_pbb], [t_ytn])
                                            else:
                                                fw.op("dve", "tensor_copy", [t_pbb], [t_ytn], out=ytn[:], in_=pbb[:, 0:256].rearrange("p (h x) -> p h x", x=128))
                                            chains[k] = [cb, ypn, t_ypn, ytn, t_ytn]
                                        else:
                                            TT("dve", to[:, 2 * cb:2 * cb + 2, :], pav[:, :, 128:256], yp[:, :, 128:256], ALU.add, [t_pa, t_yp], [t_to])
                            fw.dma("pool", S["Tinv"].ap[d, c], to[:], reads=[t_to], writes=S["Tinv"].tu(t0, 128, key=d))
                    fw.barrier()
                if stop == "E2":
                    break
                with ExitStack() as se3:
                    AR = Ring(nc, se3, "e3ar", [128, 8, 2, 128], BF16, 3)
                    BB3 = Ring(nc, se3, "e3bb", [128, 8, 128], BF16, 3)
                    KB3 = Ring(nc, se3, "e3kb", [128, 8, 128], BF16, 3)
                    V3 = Ring(nc, se3, "e3v", [128, D], BF16, 3)
                    KH3 = Ring(nc, se3, "e3kh", [128, D], BF16, 3)
                    BH3 = Ring(nc, se3, "e3bh", [128, D], BF16, 3)
                    TI3 = Ring(nc, se3, "e3ti", [128, 16, 128], BF16, 3)
                    AM = Ring(nc, se3, "e3am", [128, 384], BF16, 8)
                    WB3 = Ring(nc, se3, "e3w", [128, 64], BF16, 8)
                    UN3 = Ring(nc, se3, "e3u", [128, 64], BF16, 8)
                    YO3 = Ring(nc, se3, "e3yo", [128, D], F32, 3)
                    orders2 = [list(range(NC2)), [1, 0] + list(range(NC2 - 1, 1, -1))]
                    bank = lambda b: (psall[:, b * 512:(b + 1) * 512], PS.tus[b])
                    st3 = []
                    for d in range(2):
                        S32 = se3.enter_context(SBT(f"S32_{d}", [128, 8, 64], F32))
                        Sb = se3.enter_context(SBT(f"Sb_{d}", [128, 8, 64], BF16))
                        t_S = [TU(f"S32_{cb}") for cb in range(8)]
                        t_Sb = [TU(f"Sb_{cb}") for cb in range(8)]
                        fw.op("pool", "memset", [], t_S, ap=S32[:], constant=0.0)
                        fw.op("pool", "memset", [], t_Sb, ap=Sb[:], constant=0.0)
                        st3.append((S32, Sb, t_S, t_Sb))
                    psa_i = 0
                    STMP = [(se3.enter_context(SBT(f"stmp{d_}", [128, 8, 64], F32)), TU(f"stmp{d_}")) for d_ in range(2)]
                    for i in range(NC2):
                        cx = []
                        for d in range(2):
                            c = orders2[d][i]
                            t0 = c * 128
                            ar, t_ar = AR.next(); bb, t_bb = BB3.next(); kb, t_kb = KB3.next(); vv, t_vv = V3.next(); kh, t_kh = KH3.next(); bh, t_bh = BH3.next(); ti, t_ti = TI3.next()
                            rd = lambda nm: [x for cb_ in range(8) for x in S[nm].tu(t0, 128, key=(d, cb_))]
                            fw.dma("sp", ar[:, :, 0, :], S["abT"].ap[d, :, t0:t0 + 128].rearrange("(cb p) t -> p cb t", p=128), reads=rd("abT"), writes=[t_ar])
                            fw.dma("sp", ar[:, :, 1, :], S["rbT"].ap[d, :, t0:t0 + 128].rearrange("(cb p) t -> p cb t", p=128), reads=rd("rbT"), writes=[t_ar])
                            fw.dma("sp", bb[:], S["bbT"].ap[d, :, t0:t0 + 128].rearrange("(cb p) t -> p cb t", p=128), reads=rd("bbT"), writes=[t_bb])
                            fw.dma("sp", kb[:], S["kbT"].ap[d, :, t0:t0 + 128].rearrange("(cb p) t -> p cb t", p=128), reads=rd("kbT"), writes=[t_kb])
                            fw.dma("sp", vv[:], S["v_tm"].ap[t0:t0 + 128, :], reads=[x for cb_ in range(8) for x in S["v_tm"].tu(t0, 128, key=cb_)], writes=[t_vv])
                            fw.dma("sp", kh[:], S["Khat"].ap[d, t0:t0 + 128, :], reads=rd("Khat"), writes=[t_kh])
                            fw.dma("sp", bh[:], S["Bhat"].ap[d, t0:t0 + 128, :], reads=rd("Bhat"), writes=[t_bh])
                            fw.dma("sp", ti[:], S["Tinv"].ap[d, c], reads=S["Tinv"].tu(t0, 128, key=d), writes=[t_ti])
                            yo, t_yo = YO3.next()
                            stmp, t_stmp = STMP[d]
                            S32_, _, t_S_, _ = st3[d]
                            fw.op("pool", "tensor_tensor", t_S_ + [t_plb], [t_stmp], out=stmp[:], in0=S32_[:], in1=plb[:, d, :, c:c + 1].to_broadcast([128, 8, 64]), op=ALU.mult)
                            cx.append(dict(d=d, c=c, t0=t0, stmp=stmp, t_stmp=t_stmp, ar=ar, t_ar=t_ar, bb=bb, t_bb=t_bb, kb=kb, t_kb=t_kb, vv=vv, t_vv=t_vv, kh=kh, t_kh=t_kh, bh=bh, t_bh=t_bh, ti=ti, t_ti=t_ti, yo=yo, t_yo=t_yo,
                                           mk_st=(m_lt if d == 0 else m_gt), mk_in=(m_le if d == 0 else m_ge), ams={}))

                        def front(x, hd_):
                            nonlocal_i = [0]
                            cb_, hh_ = hd_ // 2, hd_ % 2
                            pb_ = hh_ * 64
                            psa, t_psa = bank(6 + (front.k % 2))
                            front.k += 1
                            MM(psa[:, 0:256], x["kb"][pb_:pb_ + 64, cb_, :], x["ar"][pb_:pb_ + 64, cb_, :, :].rearrange("p a t -> p (a t)"), [x["t_kb"], x["t_ar"]], [t_psa])
                            MM(psa[:, 256:384], x["bb"][pb_:pb_ + 64, cb_, :], x["ar"][pb_:pb_ + 64, cb_, 1, :], [x["t_bb"], x["t_ar"]], [t_psa])
                            am_, t_am_ = AM.next()
                            TT("dve", am_[:, 0:128], psa[:, 0:128], x["mk_st"], ALU.mult, [t_psa, t_cst], [t_am_])
                            TT("dve", am_[:, 128:384].rearrange("p (a t) -> p a t", t=128), psa[:, 128:384].rearrange("p (a t) -> p a t", t=128), x["mk_in"].unsqueeze(1).to_broadcast([128, 2, 128]), ALU.mult, [t_psa, t_cst], [t_am_])
                            x["ams"][hd_] = (am_, t_am_)
                        front.k = 0
                        for x in cx:
                            front(x, 0)
                        for cb in range(8):
                            for hh in range(2):
                                hd = 2 * cb + hh
                                pb = hh * 64
                                hs = slice(hd * 64, (hd + 1) * 64)
                                if hd + 1 < 16:
                                    for x in cx:
                                        front(x, hd + 1)
                                loc = []
                                for x in cx:
                                    d = x["d"]
                                    S32, Sb, t_S, t_Sb = st3[d]
                                    am, t_am = x["ams"][hd]
                                    psw, t_psw = bank(2 * d + (hd % 2))
                                    MM(psw[:, 0:64], am[:, 0:128], x["vv"][:, hs], [t_am, x["t_vv"]], [t_psw], start=True, stop=False)
                                    MM(psw[:, 0:64], x["ar"][pb:pb + 64, cb, 0, :], Sb[pb:pb + 64, cb, :], [x["t_ar"], t_Sb[cb]], [t_psw], start=False, stop=True)
                                    loc.append((x, d, S32, Sb, t_S, t_Sb, am, t_am, psw, t_psw))
                                wbs = []
                                for (x, d, S32, Sb, t_S, t_Sb, am, t_am, psw, t_psw) in loc:
                                    wb_, t_wb_ = WB3.next()
                                    ACT(wb_[:], psw[:, 0:64], AF.Copy, [t_psw], [t_wb_])
                                    wbs.append((wb_, t_wb_))
                                for k_, (x, d, S32, Sb, t_S, t_Sb, am, t_am, psw, t_psw) in enumerate(loc):
                                    MM(psw[:, 64:128], x["ti"][:, hd, :], wbs[k_][0][:], [x["t_ti"], wbs[k_][1]], [t_psw])
                                uns = []
                                for (x, d, S32, Sb, t_S, t_Sb, am, t_am, psw, t_psw) in loc:
                                    un, t_un = UN3.next()
                                    ACT(un[:], psw[:, 64:128], AF.Copy, [t_psw], [t_un], scale=-1.0)
                                    uns.append((un, t_un))
                                for k_, (x, d, S32, Sb, t_S, t_Sb, am, t_am, psw, t_psw) in enumerate(loc):
                                    un, t_un = uns[k_]
                                    psS, t_psS = bank(4 + d)
                                    MM(psw[:, 128:192], am[:, 128:256], x["vv"][:, hs], [t_am, x["t_vv"]], [t_psw], start=True, stop=False)
                                    MM(psw[:, 128:192], am[:, 256:384], un[:], [t_am, t_un], [t_psw], start=False, stop=False)
                                    MM(psw[:, 128:192], x["ar"][pb:pb + 64, cb, 1, :], Sb[pb:pb + 64, cb, :], [x["t_ar"], t_Sb[cb]], [t_psw], start=False, stop=True)
                                    MM(psS[pb:pb + 64, 0:64], x["kh"][:, hs], x["vv"][:, hs], [x["t_kh"], x["t_vv"]], [t_psS], start=True, stop=False)
                                    MM(psS[pb:pb + 64, 0:64], x["bh"][:, hs], un[:], [x["t_bh"], t_un], [t_psS], start=False, stop=True)
                                for (x, d, S32, Sb, t_S, t_Sb, am, t_am, psw, t_psw) in loc:
                                    ACT(x["yo"][:, hs], psw[:, 128:192], AF.Copy, [t_psw], [x["t_yo"]])
                                if hh == 1:
                                    for (x, d, S32, Sb, t_S, t_Sb, am, t_am, psw, t_psw) in loc:
                                        psS, t_psS = bank(4 + d)
                                        c = x["c"]
                                        STT("dve", S32[:, cb, :], psS[:, 0:64], plb[:, d, cb, c:c + 1], x["stmp"][:, cb, :], ALU.mult, ALU.add, [t_S[cb], t_psS, t_plb, x["t_stmp"]], [t_S[cb]])
                                        ACT(Sb[:, cb, :], S32[:, cb, :], AF.Copy, [t_S[cb]], [t_Sb[cb]])
                        for x in cx:
                            fw.dma("pool", S["yr"].ap[x["d"], x["t0"]:x["t0"] + 128, :], x["yo"][:], reads=[x["t_yo"]], writes=S["yr"].tu(x["t0"], 128, key=x["d"]))
                    fw.barrier()
                if stop == "E3":
                    break
                with ExitStack() as se4:
                    def bc_load(name, src1d):
                        t = se4.enter_context(SBT(name, [128, D], F32))
                        tu_ = TU(name)
                        fw.dma("sp", t[:], src1d.partition_broadcast(128), writes=[tu_])
                        return t, tu_
                    lnw, t_lnw = bc_load("lnw_bc", I["ln_w"].ap[l])
                    lnb, t_lnb = bc_load("lnb_bc", I["ln_b"].ap[l])
                    g2b = se4.enter_context(SBT("g2b", [128, D], BF16)); t_g2 = TU("g2b")
                    with ExitStack() as se40:
                        g2f = se40.enter_context(SBT("g2f", [128, D], F32))
                        fw.dma("sp", g2f[:], I["g2"].ap[l], writes=[t_g2])
                        fw.op("dve", "tensor_copy", [t_g2], [t_g2], out=g2b[:], in_=g2f[:])
                        fw.barrier()
                    YF = Ring(nc, se4, "yf", [128, D], F32, 2)
                    YB = Ring(nc, se4, "yb", [128, D], F32, 2)
                    VT = Ring(nc, se4, "vt4", [128, D], BF16, 2)
                    GD = Ring(nc, se4, "gd4", [128, 128], BF16, 2)
                    SQ4 = Ring(nc, se4, "sq4", [128, D], F32, 1)
                    ST4 = Ring(nc, se4, "st4", [128, 2, 16], F32, 2)
                    RO = Ring(nc, se4, "ro4", [128, D], BF16, 2)
                    RT = Ring(nc, se4, "rt4", [128, 8, 128], BF16, 2)
                    for tt in range(NC2):
                        t0 = tt * 128
                        yf, t_yf = YF.next(); yb, t_yb = YB.next(); vt, t_vt = VT.next(); gd, t_gd = GD.next(); sq, t_sq = SQ4.next(); st4, t_st = ST4.next(); ro, t_ro = RO.next(); rt, t_rt = RT.next()
                        fw.dma("sp", yf[:], S["yr"].ap[0, t0:t0 + 128, :], reads=S["yr"].tu(t0, 128, key=0), writes=[t_yf])
                        fw.dma("sp", yb[:], S["yr"].ap[1, t0:t0 + 128, :], reads=S["yr"].tu(t0, 128, key=1), writes=[t_yb])
                        fw.dma("sp", vt[:], S["v_tm"].ap[t0:t0 + 128, :], reads=[x for cb_ in range(8) for x in S["v_tm"].tu(t0, 128, key=cb_)], writes=[t_vt])
                        fw.dma("sp", gd[:], S["gdT"].ap[:, t0:t0 + 128], reads=S["gdT"].tu(t0, 128), writes=[t_gd])
                        TT("dve", yf[:], yf[:], yb[:], ALU.add, [t_yf, t_yb], [t_yf])
                        y3 = yf[:].rearrange("p (h v) -> p h v", v=64)
                        fw.op("dve", "tensor_reduce", [t_yf], [t_st], out=st4[:, 0, :], in_=y3, axis=AX.X, op=ALU.add)
                        TS("dve", st4[:, 0, :], st4[:, 0, :], 1.0 / 64, None, ALU.mult, None, [t_st], [t_st])
                        TT("dve", y3, y3, st4[:, 0, :].unsqueeze(2).to_broadcast([128, 16, 64]), ALU.subtract, [t_yf, t_st], [t_yf])
                        ACT(sq[:], yf[:], AF.Square, [t_yf], [t_sq])
                        fw.op("dve", "tensor_reduce", [t_sq], [t_st], out=st4[:, 1, :], in_=sq[:].rearrange("p (h v) -> p h v", v=64), axis=AX.X, op=ALU.add)
                        ACT(st4[:, 1, :], st4[:, 1, :], AF.Sqrt, [t_st], [t_st], scale=1.0 / 64, bias=64e-5)
                        fw.op("dve", "reciprocal", [t_st], [t_st], out=st4[:, 1, :], in_=st4[:, 1, :])
                        TT("dve", y3, y3, st4[:, 1, :].unsqueeze(2).to_broadcast([128, 16, 64]), ALU.mult, [t_yf, t_st], [t_yf])
                        TT("pool", yf[:], yf[:], lnw[:], ALU.mult, [t_yf, t_lnw], [t_yf])
                        TT("pool", yf[:], yf[:], lnb[:], ALU.add, [t_yf, t_lnb], [t_yf])
                        TT("dve", sq[:].rearrange("p (h v) -> p h v", v=64), vt[:].rearrange("p (h v) -> p h v", v=64), bon[:, tt, :].unsqueeze(2).to_broadcast([128, 16, 64]), ALU.mult, [t_vt, t_bon], [t_sq])
                        TT("dve", yf[:], yf[:], sq[:], ALU.add, [t_yf, t_sq], [t_yf])
                        pg, t_pg = PS.next2()
                        for hf_ in range(2):
                            MM(pg[:, hf_, :], gd[:], g2b[:, hf_ * 512:(hf_ + 1) * 512], [t_gd, t_g2], [t_pg[hf_]])
                        TT("dve", ro[:].rearrange("p (a x) -> p a x", x=512), yf[:].rearrange("p (a x) -> p a x", x=512), pg, ALU.mult, [t_yf] + t_pg, [t_ro])
                        if "rotm" in dbg:
                            fw.dma("pool", dbg["rotm"].ap[t0:t0 + 128, :], ro[:], reads=[t_ro], writes=dbg["rotm"].tu(t0, 128))
                        for hb2 in range(2):
                            ps, t_ps = PS.next()
                            for j in range(4):
                                cb = hb2 * 4 + j
                                MM(ps[:, j * 128:(j + 1) * 128], ro[:, cb * 128:(cb + 1) * 128], ident_b, [t_ro, t_cst], [t_ps])
                            ACT(rt[:, hb2 * 4:(hb2 + 1) * 4, :], ps[:].rearrange("p (c t) -> p c t", t=128), AF.Copy, [t_ps], [t_rt])
                        fw.dma("pool", S["roT"].ap[:, t0:t0 + 128].rearrange("(c p) t -> p c t", p=128), rt[:], reads=[t_rt], writes=S["roT"].tu(t0, 128))
                    fw.barrier()
            if stop == "E4":
                break
            with ExitStack() as sf:
                wts = {}
                for nm_ in ("w_proj_mlstm", "w_proj_rwkv", "w_out"):
                    wts[nm_] = (sf.enter_context(SBT("f_" + nm_, [128, 8, D], BF16)), TU(nm_))
                with ExitStack() as sf0:
                    WST = Ring(nc, sf0, "fwst", [128, 8, 512], F32, 2)
                    for nm_ in ("w_proj_mlstm", "w_proj_rwkv", "w_out"):
                        for hf_ in range(2):
                            wst, t_wst = WST.next()
                            fw.dma("sp", wst[:], I[nm_].ap[l, :, hf_ * 512:(hf_ + 1) * 512].rearrange("(c p) m -> p c m", p=128), writes=[t_wst])
                            if hf_ == 0:
                                fw.op("dve", "tensor_copy", [t_wst], [wts[nm_][1]], out=wts[nm_][0][:, :, hf_ * 512:(hf_ + 1) * 512], in_=wst[:])
                            else:
                                ACT(wts[nm_][0][:, :, hf_ * 512:(hf_ + 1) * 512], wst[:], AF.Copy, [t_wst], [wts[nm_][1]])
                    fw.barrier()
                MO_ = Ring(nc, sf, "f_mo", [128, 8, 512], BF16, 2)
                RO_ = Ring(nc, sf, "f_ro", [128, 8, 512], BF16, 2)
                GM_ = Ring(nc, sf, "f_gm", [128, 8, 512], BF16, 1)
                GR_ = Ring(nc, sf, "f_gr", [128, 8, 512], BF16, 1)
                ZT_ = Ring(nc, sf, "f_zt", [128, 512], F32, 2)
                ZB_ = Ring(nc, sf, "f_zb", [128, 8, 512], BF16, 1)
                XT_ = Ring(nc, sf, "f_xt", [128, 8, 512], F32, 1)
                for ti, (t0, n) in enumerate(TILES):
                    if l == DEPTH - 1 and t0 < NCTX:
                        continue
                    j = 1 if t0 < NCTX else 0
                    mo_, t_mo_ = MO_.next(); ro_, t_ro_ = RO_.next(); gm_, t_gm_ = GM_.next(); gr_, t_gr_ = GR_.next(); zb_, t_zb_ = ZB_.next(); xt_, t_xt_ = XT_.next()
                    fw.dma("sp", mo_[:, :, :n], S["moT"].ap[:, t0:t0 + n].rearrange("(c p) t -> p c t", p=128), reads=S["moT"].tu(t0, n), writes=[t_mo_])
                    fw.dma("sp", ro_[:, :, :n], S["roT"].ap[:, t0:t0 + n].rearrange("(c p) t -> p c t", p=128), reads=S["roT"].tu(t0, n), writes=[t_ro_])
                    fw.dma("sp", gm_[:, :, :n], S["mgT"].ap[0:D, t0:t0 + n].rearrange("(c p) t -> p c t", p=128), reads=[x for r0 in range(0, 2048, 128) for x in S["mgT"].tu(t0, n, key=r0)], writes=[t_gm_])
                    fw.dma("sp", gr_[:, :, :n], S["mgT"].ap[D:2 * D, t0:t0 + n].rearrange("(c p) t -> p c t", p=128), reads=[x for r0 in range(0, 2048, 128) for x in S["mgT"].tu(t0, n, key=r0)], writes=[t_gr_])
                    fw.dma("sp", xt_[:, :, :n], xsrc.ap[:, t0:t0 + n].rearrange("(c p) t -> p c t", p=128), reads=xsrc.tu(t0, n), writes=[t_xt_])
                    for m in range(8):
                        pm, t_pm = PS.next(); pr, t_pr = PS.next()
                        for c in range(8):
                            MM(pm[:, :n], wts["w_proj_mlstm"][0][:, c, m * 128:(m + 1) * 128], mo_[:, c, :n], [wts["w_proj_mlstm"][1], t_mo_], [t_pm], start=(c == 0), stop=(c == 7))
                        for c in range(8):
                            MM(pr[:, :n], wts["w_proj_rwkv"][0][:, c, m * 128:(m + 1) * 128], ro_[:, c, :n], [wts["w_proj_rwkv"][1], t_ro_], [t_pr], start=(c == 0), stop=(c == 7))
                        zt_, t_zt_ = ZT_.next()
                        TT("dve", zt_[:, :n], pm[:, :n], gm_[:, m, :n], ALU.mult, [t_pm, t_gm_], [t_zt_])
                        TT("dve", zb_[:, m, :n], pr[:, :n], gr_[:, m, :n], ALU.mult, [t_pr, t_gr_], [t_zb_])
                        TT("pool", zb_[:, m, :n], zb_[:, m, :n], zt_[:, :n], ALU.add, [t_zb_, t_zt_], [t_zb_])
                    for m in range(8):
                        py_, t_py_ = PS.next()
                        for c in range(8):
                            MM(py_[:, :n], wts["w_out"][0][:, c, m * 128:(m + 1) * 128], zb_[:, c, :n], [wts["w_out"][1], t_zb_], [t_py_], start=(c == 0), stop=(c == 7))
                        STT("dve", xt_[:, m, :n], py_[:, :n], prm[:, l, 2, m, j:j + 1], xt_[:, m, :n], ALU.mult, ALU.add, [t_py_, t_xt_, t_prm], [t_xt_])
                    fw.dma("pool", S["xres"].ap[:, t0:t0 + n].rearrange("(c p) t -> p c t", p=128), xt_[:, :, :n], reads=[t_xt_], writes=S["xres"].tu(t0, n))
                fw.barrier()
            if stop == "F":
                break
            moe = (l % 2 == 1)
            with ExitStack() as sg:
                work = {"x": Ring(nc, sg, "gx", [128, 8, 512], F32, 1 if moe else 2), "sq": Ring(nc, sg, "gsq", [128, 8, 512], BF16, 1), "rs": Ring(nc, sg, "grs", [128, 512], F32, 2)}
                FP = 256
                H2 = Ring(nc, sg, "gh2", [128, 8, 512], BF16, 1)
                YA = Ring(nc, sg, "gya", [128, 8, 512], F32, 1)
                WGS = Ring(nc, sg, "gwgs", [128, 8, FP], F32, 3)
                WG = Ring(nc, sg, "gwg", [128, 8, FP], BF16, 2)
                WU = Ring(nc, sg, "gwu", [128, 8, FP], BF16, 2)
                WD = Ring(nc, sg, "gwd", [128, FP // 128, D], BF16, 2)
                ACTB = Ring(nc, sg, "gact", [128, FP // 128, 512], BF16, 2)
                SIL = Ring(nc, sg, "gsil", [128, 512], F32, 3)
                XB = Ring(nc, sg, "gxb", [128, 8, 512], F32, 1)
                if moe:
                    H32 = Ring(nc, sg, "gh32", [128, 8, 512], F32, 1)
                    wr = sg.enter_context(SBT("wr_sb", [128, 8, NEXP], F32)); t_wr = TU("wr")
                    fw.dma("sp", wr[:], I["w_router"].ap[0].rearrange("(c p) e -> p c e", p=128), writes=[t_wr])
                    SEL = sg.enter_context(SBT("SEL", [8, NEXP, 128], F32)); t_SEL = TU("SEL")
                    fw.op("dve", "tensor_copy", [t_cst], [t_SEL], out=SEL[:], in_=ident_f[0:8, 0:8].unsqueeze(2).to_broadcast([8, NEXP, 128]))
                    GTM = Ring(nc, sg, "gtm", [128, 4, 24], F32, 2)
                    GTT = Ring(nc, sg, "gtt", [8, 512], F32, 2)
                    GBC = Ring(nc, sg, "gbc", [128, 512], F32, 2)
                    gfin = sg.enter_context(SBT("gfin", [128, 8], F32)); t_gfin = TU("gfin")
                    fw.dma("sp", gfin[:], I["g_final"].ap.rearrange("(c p) -> p c", p=128), writes=[t_gfin], allow_slow_non_contiguous=True)
                if moe:
                    tiles_g = [(256 + 512 * i, 512, i) for i in range(4)]
                else:
                    tiles_g = [(t0, n, None) for (t0, n) in TILES]
                dff = D_FFE if moe else D_FF
                for (t0, n, hi) in tiles_g:
                    h2, t_h2 = H2.next(); ya, t_ya = YA.next()
                    xkeep = {}
                    if moe:
                        xb, t_xb = XB.next()
                        t1 = t0 + 2048

                        def loader(xt, t_xt, t0=t0, t1=t1, n=n, xb=xb, t_xb=t_xb):
                            fw.dma("sp", xt[:, :, :n], S["xres"].ap[:, t0:t0 + n].rearrange("(c p) t -> p c t", p=128), reads=S["xres"].tu(t0, n), writes=[t_xt])
                            fw.dma("sp", xb[:, :, :n], S["xres"].ap[:, t1:t1 + n].rearrange("(c p) t -> p c t", p=128), reads=S["xres"].tu(t1, n), writes=[t_xb])
                            TS("pool", xt[:, :, :n], xt[:, :, :n], sel_sb[:, 0:1], None, ALU.mult, None, [t_xt, t_cst], [t_xt])
                            STT("dve", xt[:, :, :n], xb[:, :, :n], sel_sb[:, 1:2], xt[:, :, :n], ALU.mult, ALU.add, [t_xb, t_xt, t_cst], [t_xt])
                            fw.op("pool", "tensor_copy", [t_xt], [t_xb], out=xb[:, :, :n], in_=xt[:, :, :n])
                        h32, t_h32 = H32.next()
                        norm_mod(sg, loader, None, t0, n, l, 1, h2[:, :, :n], [t_h2], work, h32=(h32, t_h32))
                    else:
                        norm_mod(sg, S["xres"].ap, S["xres"].tu(t0, n), t0, n, l, 1, h2[:, :, :n], [t_h2], work)
                    fw.op("pool", "memset", [], [t_ya], ap=ya[:, :, :n], constant=0.0)
                    gbcs = [None] * NEXP
                    if moe:
                        gtm, t_gtm = GTM.next(); gtt, t_gtt = GTT.next()
                        pl_, t_pl = PS.next()
                        for sub in range(4):
                            for c in range(8):
                                MM(pl_[:, sub * 8:sub * 8 + 8], h32[:, c, sub * 128:(sub + 1) * 128], wr[:, c, :], [t_h32, t_wr], [t_pl], start=(c == 0), stop=(c == 7))
                        lg = gtm[:, :, 0:8]; eq1 = gtm[:, :, 8:16]; eq2 = gtm[:, :, 16:24]
                        fw.op("dve", "tensor_copy", [t_pl], [t_gtm], out=lg, in_=pl_[:, 0:32].rearrange("p (s e) -> p s e", e=8))
                        mx, t_mx = SIL.next()
                        m1 = mx[:, 0:4]; m2 = mx[:, 4:8]; p1 = mx[:, 8:12]; p2 = mx[:, 12:16]
                        fw.op("dve", "tensor_reduce", [t_gtm], [t_mx], out=m1, in_=lg, axis=AX.X, op=ALU.max)
                        TT("dve", eq1, lg, m1.unsqueeze(2).to_broadcast([128, 4, 8]), ALU.is_equal, [t_gtm, t_mx], [t_gtm])
                        STT("dve", eq2, eq1, -1e30, lg, ALU.mult, ALU.add, [t_gtm], [t_gtm])
                        fw.op("dve", "tensor_reduce", [t_gtm], [t_mx], out=m2, in_=eq2, axis=AX.X, op=ALU.max)
                        TT("dve", eq2, eq2, m2.unsqueeze(2).to_broadcast([128, 4, 8]), ALU.is_equal, [t_gtm, t_mx], [t_gtm])
                        TT("dve", p1, m2, m1, ALU.subtract, [t_mx], [t_mx])
                        ACT(p1, p1, AF.Exp, [t_mx], [t_mx])
                        TS("dve", p1, p1, 1.0, None, ALU.add, None, [t_mx], [t_mx])
                        fw.op("dve", "reciprocal", [t_mx], [t_mx], out=p1, in_=p1)
                        TS("dve", p2, p1, -1.0, 1.0, ALU.mult, ALU.add, [t_mx], [t_mx])
                        TT("dve", eq1, eq1, p1.unsqueeze(2).to_broadcast([128, 4, 8]), ALU.mult, [t_gtm, t_mx], [t_gtm])
                        TT("dve", eq2, eq2, p2.unsqueeze(2).to_broadcast([128, 4, 8]), ALU.mult, [t_gtm, t_mx], [t_gtm])
                        TT("dve", eq1, eq1, eq2, ALU.add, [t_gtm], [t_gtm])
                        pt_, t_pt = PS.next()
                        for sub in range(4):
                            MM(pt_[0:8, sub * 128:(sub + 1) * 128], gtm[:, sub, 8:16], ident_f, [t_gtm, t_cst], [t_pt])
                        fw.op("dve", "tensor_copy", [t_pt], [t_gtt], out=gtt[:], in_=pt_[0:8, :])
                        if "gates" in dbg:
                            fw.dma("pool", dbg["gates"].ap[:, hi * 512:(hi + 1) * 512], gtt[:], reads=[t_gtt], writes=dbg["gates"].tu(hi * 512, 512))
                    for ex in range(NEXP if moe else 1):
                        if moe:
                            gbc, t_gbc = GBC.next()
                            pb_, t_pb = PS.next()
                            MM(pb_[:, :n], SEL[:, ex, :], gtt[:, :n], [t_SEL, t_gtt], [t_pb])
                            fw.op("dve", "tensor_copy", [t_pb], [t_gbc], out=gbc[:, :n], in_=pb_[:, :n])
                            wg_ap = I["w_exp_gate"].ap[0, ex]; wu_ap = I["w_exp_up"].ap[0, ex]; wd_ap = I["w_exp_down"].ap[0, ex]
                        else:
                            wg_ap = I["w_ff_gate"].ap[0]; wu_ap = I["w_ff_up"].ap[0]; wd_ap = I["w_ff_down"].ap[0]
                        for f0 in range(0, dff, FP):
                            fwid = min(FP, dff - f0)
                            nj = fwid // 128
                            wg, t_wg = WG.next(); wu, t_wu = WU.next(); wd, t_wd = WD.next()
                            st1, t_st1 = WGS.next()
                            fw.dma("sp", st1[:, :, :fwid], wg_ap[:, f0:f0 + fwid].rearrange("(c p) m -> p c m", p=128), writes=[t_st1])
                            fw.op("dve", "tensor_copy", [t_st1], [t_wg], out=wg[:, :, :fwid], in_=st1[:, :, :fwid])
                            st2, t_st2 = WGS.next()
                            fw.dma("sp", st2[:, :, :fwid], wu_ap[:, f0:f0 + fwid].rearrange("(c p) m -> p c m", p=128), writes=[t_st2])
                            fw.op("act", "activation", [t_st2], [t_wu], out=wu[:, :, :fwid], in_=st2[:, :, :fwid], func=AF.Copy)
                            st3, t_st3 = WGS.next()
                            st3v = st3[:].rearrange("p c m -> p (c m)").rearrange("p (j m) -> p j m", m=D)
                            fw.dma("sp", st3v[:, :nj, :], wd_ap[f0:f0 + fwid, :].rearrange("(j p) m -> p j m", p=128), writes=[t_st3])
                            ACT(wd[:, :nj, :], st3v[:, :nj, :], AF.Copy, [t_st3], [t_wd])
                            ab_, t_ab_ = ACTB.next()
                            for jb in range(nj):
                                pg_, t_pg_ = PS.next(); pu_, t_pu_ = PS.next()
                                for c in range(8):
                                    MM(pg_[:, :n], wg[:, c, jb * 128:(jb + 1) * 128], h2[:, c, :n], [t_wg, t_h2], [t_pg_], start=(c == 0), stop=(c == 7))
                                for c in range(8):
                                    MM(pu_[:, :n], wu[:, c, jb * 128:(jb + 1) * 128], h2[:, c, :n], [t_wu, t_h2], [t_pu_], start=(c == 0), stop=(c == 7))
                                sil, t_sil = SIL.next()
                                ACT(sil[:, :n], pg_[:, :n], AF.Silu, [t_pg_], [t_sil])
                                if moe:
                                    TT("dve", sil[:, :n], sil[:, :n], gbc[:, :n], ALU.mult, [t_sil, t_gbc], [t_sil])
                                TT("dve", ab_[:, jb, :n], pu_[:, :n], sil[:, :n], ALU.mult, [t_pu_, t_sil], [t_ab_])
                            for m in range(8):
                                pd_, t_pd_ = PS.next()
                                for jb in range(nj):
                                    MM(pd_[:, :n], wd[:, jb, m * 128:(m + 1) * 128], ab_[:, jb, :n], [t_wd, t_ab_], [t_pd_], start=(jb == 0), stop=(jb == nj - 1))
                                TT("dve", ya[:, m, :n], pd_[:, :n], ya[:, m, :n], ALU.add, [t_pd_, t_ya], [t_ya])
                    j = 1 if t0 < NCTX else 0
                    if moe:
                        xs, t_xs = xb, t_xb
                    else:
                        xs, t_xs = work["x"].next()
                        fw.dma("sp", xs[:, :, :n], S["xres"].ap[:, t0:t0 + n].rearrange("(c p) t -> p c t", p=128), reads=S["xres"].tu(t0, n), writes=[t_xs])
                    for m in range(8):
                        STT("dve", xs[:, m, :n], ya[:, m, :n], prm[:, l, 5, m, j:j + 1], xs[:, m, :n], ALU.mult, ALU.add, [t_ya, t_xs, t_prm], [t_xs])
                    if not moe:
                        fw.dma("pool", S["xres"].ap[:, t0:t0 + n].rearrange("(c p) t -> p c t", p=128), xs[:, :, :n], reads=[t_xs], writes=S["xres"].tu(t0, n))
                    else:
                        sq, t_sq = work["sq"].next(); rs, t_rs = work["rs"].next()
                        ACT(sq[:, :, :n], xs[:, :, :n], AF.Square, [t_xs], [t_sq])
                        ps, t_ps = PS.next()
                        for c in range(8):
                            MM(ps[:, :n], ones_b, sq[:, c, :n], [t_sq, t_cst], [t_ps], start=(c == 0), stop=(c == 7))
                        ACT(rs[:, :n], ps[:, :n], AF.Sqrt, [t_ps], [t_rs], scale=1.0 / D, bias=1e-6)
                        fw.op("dve", "reciprocal", [t_rs], [t_rs], out=rs[:, :n], in_=rs[:, :n])
                        TT("dve", xs[:, :, :n], xs[:, :, :n], rs[:, :n].unsqueeze(1).to_broadcast([128, 8, n]), ALU.mult, [t_rs, t_xs], [t_xs])
                        TT("pool", xs[:, :, :n], xs[:, :, :n], gfin[:].unsqueeze(2).to_broadcast([128, 8, n]), ALU.mult, [t_xs, t_gfin], [t_xs])
                        fw.dma("pool", out.ap[:, hi * 512:(hi + 1) * 512].rearrange("(c p) t -> p c t", p=128), xs[:, :, :n], reads=[t_xs], writes=out.tu(hi * 512, 512))
                fw.barrier()

        fw.barrier()
        fw.finish()
        nc._fw_counts = {n: e.cnt for n, e in fw.engs.items()}
        nc._fw_counts["dma"] = {q: sum(u for _, u in lst) for q, (lst, _) in fw.dma_sems.items()}
    return nc


def make_consts():
    c = np.zeros((128, 1024), np.float32)
    p = np.arange(128)[:, None]
    f = np.arange(128)[None, :]
    c[:, 0:128] = (p == f)
    c[:, 128:256] = (p <= f)
    c[:, 256:384] = (p < f)
    c[:, 384:512] = (p >= f)
    c[:, 512:640] = (p > f)
    c[:, 640:768] = (p // 64 == f // 64)
    c[:, 768:896] = 1.0
    c[:, 896:898] = (p // 64 == np.arange(2)[None, :])
    return c


def make_in_maps(inputs):
    f32 = lambda a: np.ascontiguousarray(np.asarray(a, dtype=np.float32))
    shared = {k: f32(inputs[k]) for k in ["w_ada", "b_ada", "g_norm_mix", "g_norm_ffn", "w_in", "b_mlstm_gate", "g_mlstm_norm", "mu_shift", "w0", "w2",
                                          "a0", "a2", "g2", "k_k", "k_a", "ln_w", "ln_b", "w_proj_mlstm", "w_proj_rwkv", "w_out", "w_ff_gate", "w_ff_up",
                                          "w_ff_down", "w_router", "w_exp_gate", "w_exp_up", "w_exp_down", "g_final"]}
    shared["r_k"] = f32(inputs["r_k"]).reshape(DEPTH, D)
    shared["cst"] = make_consts()
    x = f32(inputs["x"]); ctx = f32(inputs["ctx"]); c = f32(inputs["c"]); c_ctx = f32(inputs["c_ctx"])
    maps = []
    for core in range(8):
        b, half = core // 2, core % 2
        m = dict(shared)
        m["xT"] = np.ascontiguousarray(np.concatenate([ctx[b], x[b]], axis=0).T)
        m["cs"] = np.ascontiguousarray(np.stack([c[b], c_ctx], axis=1))
        sel = np.zeros((128, 2), np.float32)
        sel[:, half] = 1.0
        m["sel"] = sel
        maps.append(m)
    return maps


_NC_CACHE = {}


def kernel(**inputs):
    if "nc" not in _NC_CACHE:
        _NC_CACHE["nc"] = build()
    nc = _NC_CACHE["nc"]
    maps = make_in_maps(inputs)
    res = run_bass_kernel_spmd(nc, maps, core_ids=list(range(8)))
    outp = np.zeros((4, NLAT, D), np.float32)
    for core in range(8):
        b, half = core // 2, core % 2
        outp[b, half * 2048:(half + 1) * 2048, :] = res.results[core]["out"].T
    return outp
```
